# Optimizing a Trainium2 kernel written in Bass

```python
import math
import jax, jax.numpy as jnp
from jax import lax
import numpy as np

D_MODEL = 1024
BATCH = 8
SEQ = 2048
DEPTH = 2

GRID_W = 64
CTX_LEN = 256
N_EVEN = (DEPTH + 1) // 2
N_ODD = DEPTH // 2
HEAD_DIM = 64
MIX_WIDTH = D_MODEL
HALF_MIX = MIX_WIDTH // 2
Q_BLOCK = 128
ROPE_THETA = 10000.0
EPS = 1e-6
N_MOD = 6
LRU_WIDTH = HALF_MIX
LRU_BLOCKS = LRU_WIDTH // HEAD_DIM
LRU_BLOCK_SIZE = LRU_WIDTH // LRU_BLOCKS
LRU_CONV = 4
LRU_C = 8.0
GQA_Q_HEADS = HALF_MIX // HEAD_DIM
GQA_KV_HEADS = 2
AB_IN = 2 * LRU_WIDTH + (GQA_Q_HEADS + 2 * GQA_KV_HEADS) * HEAD_DIM
HY_WIDTH = HALF_MIX
HY_ORDER = 2
HY_CONV = 3
HY_BANDS = 16
HY_POS_DIM = 1 + 2 * HY_BANDS
HY_FILTER_HIDDEN = 64
HY_TARGET = 1e-2
HY_FAST_PCT = 0.3
HY_SLOW_PCT = 1.5
HY_MIN_DECAY = math.log(HY_TARGET) / HY_SLOW_PCT
HY_MAX_DECAY = math.log(HY_TARGET) / HY_FAST_PCT
HY_SHIFT = 0.05
MLA_HEADS = HALF_MIX // HEAD_DIM
MLA_Q_RANK = D_MODEL // 4
MLA_KV_RANK = D_MODEL // 8
MLA_NOPE = HEAD_DIM
MLA_ROPE = HEAD_DIM // 2
MLA_V = HEAD_DIM
MLA_QK = MLA_NOPE + MLA_ROPE
CD_IN = (HY_ORDER + 1) * HY_WIDTH + MLA_Q_RANK + MLA_KV_RANK + MLA_ROPE
D_FF = 2816
N_EXPERTS = 8
TOP_K = 2
D_FF_EXPERT = 3584
MOE_BLOCK = 128

kernel_name = 'hybrid_lru_gqa_hyena_mla_moe'

F32 = jnp.float32


def rms_norm(x, g):
    xf = x.astype(F32)
    y = xf * lax.rsqrt(jnp.mean(xf * xf, axis=-1, keepdims=True) + EPS)
    return (y * g.astype(F32)).astype(x.dtype)


def modulate(h, shift, scale):
    return h * (1 + scale) + shift


def grid_angles(rows, rot_dim):
    n_freq = rot_dim // 4
    inv_freq = ROPE_THETA ** (-jnp.arange(n_freq, dtype=F32) / n_freq)
    t = jnp.arange(rows * GRID_W)
    row = (t // GRID_W).astype(F32)
    col = (t % GRID_W).astype(F32)
    ang = jnp.concatenate([row[:, None] * inv_freq, col[:, None] * inv_freq], axis=-1)
    return jnp.cos(ang), jnp.sin(ang)


def apply_rope(x, cos, sin):
    half = x.shape[-1] // 2
    shape = (1, cos.shape[0]) + (1,) * (x.ndim - 3) + (half,)
    cs = cos.reshape(shape).astype(x.dtype)
    sn = sin.reshape(shape).astype(x.dtype)
    x1, x2 = x[..., :half], x[..., half:]
    return jnp.concatenate([x1 * cs - x2 * sn, x1 * sn + x2 * cs], axis=-1)


def depthwise_conv(x, w, b, pad):
    y = lax.conv_general_dilated(x, w[:, None, :].astype(x.dtype), (1,), [pad],
                                 dimension_numbers=('NWC', 'WIO', 'NWC'),
                                 feature_group_count=x.shape[-1])
    return y + b.astype(x.dtype)


def attend_blocked(q, k, v):
    b_, sq, hk, g, dq = q.shape
    nb = sq // Q_BLOCK
    scale = dq ** -0.5
    qb = jnp.moveaxis(q.reshape(b_, nb, Q_BLOCK, hk, g, dq), 1, 0)

    def one_block(qi):
        s = jnp.einsum('bqhgd,bkhd->bhgqk', qi, k).astype(F32) * scale
        p = jax.nn.softmax(s, axis=-1).astype(v.dtype)
        return jnp.einsum('bhgqk,bkhe->bqhge', p, v)

    o = lax.map(one_block, qb)
    return jnp.moveaxis(o, 0, 1).reshape(b_, sq, hk, g, v.shape[-1])


def block_diag_linear(x, w, b):
    b_, l = x.shape[:2]
    xb = x.reshape(b_, l, LRU_BLOCKS, LRU_BLOCK_SIZE)
    y = jnp.einsum('blni,nij->blnj', xb, w.astype(F32)) + b.astype(F32)
    return y.reshape(b_, l, LRU_WIDTH)


def _linear_combine(left, right):
    return left[0] * right[0], right[0] * left[1] + right[1]


def rglru_scan(u, w_a, b_a, w_x, b_x, lam, h0):
    uf = u.astype(F32)
    r = jax.nn.sigmoid(block_diag_linear(uf, w_a, b_a))
    i = jax.nn.sigmoid(block_diag_linear(uf, w_x, b_x))
    log_a = -LRU_C * r * jax.nn.softplus(-lam.astype(F32))
    a = jnp.exp(log_a)
    bterm = jnp.sqrt(-jnp.expm1(2.0 * log_a)) * (i * uf)
    a_cum, b_cum = lax.associative_scan(_linear_combine, (a, bterm), axis=1)
    return a_cum * h0[:, None, :] + b_cum


def bidir_rglru(u, w_a, b_a, w_x, b_x, lam, h0_fwd, h0_bwd):
    h_f = rglru_scan(u, w_a[0], b_a[0], w_x[0], b_x[0], lam[0], h0_fwd)
    h_b = rglru_scan(u[:, ::-1], w_a[1], b_a[1], w_x[1], b_x[1], lam[1], h0_bwd)
    return h_f + h_b[:, ::-1], h_f[:, -1], h_b[:, -1]


def hyena_filters(length, w1, b1, w2, b2, w3, freq):
    t = jnp.arange(length, dtype=F32)[:, None]
    t_norm = t / max(length - 1, 1)
    bands = jnp.linspace(1e-4, HY_BANDS - 1, HY_BANDS, dtype=F32)
    ang = 2.0 * math.pi * t * bands / length
    z = jnp.concatenate([t_norm, jnp.cos(ang), -jnp.sin(ang)], axis=-1)
    fr = freq.astype(F32)
    h = jnp.sin(fr * (z @ w1.astype(F32) + b1.astype(F32)))
    h = jnp.sin(fr * (h @ w2.astype(F32) + b2.astype(F32)))
    h = h @ w3.astype(F32)
    deltas = jnp.abs(jnp.linspace(HY_MIN_DECAY, HY_MAX_DECAY, HY_WIDTH, dtype=F32))
    window = jnp.exp(-t_norm * deltas) + HY_SHIFT
    return h.reshape(length, HY_ORDER, 2, HY_WIDTH) * window[:, None, None, :]


def bidir_long_conv(u, h_fwd, h_bwd, skip):
    l, ch = h_fwd.shape
    uf = u.astype(F32)
    two_sided = jnp.concatenate([h_fwd, jnp.zeros((1, ch), F32), h_bwd[:0:-1]], axis=0)
    spec = jnp.fft.rfft(uf, n=2 * l, axis=1) * jnp.fft.rfft(two_sided, axis=0)[None]
    y = jnp.fft.irfft(spec, n=2 * l, axis=1)[:, :l]
    return (y + uf * skip.astype(F32)).astype(u.dtype)


def swiglu(h, w1, w3, w2):
    return (jax.nn.silu(h @ w1) * (h @ w3)) @ w2


def moe_swiglu(h, router, w1, w3, w2):
    b_, l, d = h.shape
    t = h.reshape(-1, d)
    n_assign = t.shape[0] * TOP_K
    logits = t.astype(F32) @ router.astype(F32)
    top_logit, top_e = lax.top_k(logits, TOP_K)
    gates = jax.nn.softmax(top_logit, axis=-1)
    flat_e = top_e.reshape(-1)
    flat_tok = jnp.arange(n_assign, dtype=jnp.int32) // TOP_K
    flat_g = gates.reshape(-1)
    order = jnp.argsort(flat_e)
    se = flat_e[order]
    counts = jnp.bincount(flat_e, length=N_EXPERTS)
    padded = (counts + MOE_BLOCK - 1) // MOE_BLOCK * MOE_BLOCK
    start = jnp.cumsum(counts) - counts
    pend = jnp.cumsum(padded)
    pstart = pend - padded
    dest = pstart[se] + jnp.arange(n_assign, dtype=jnp.int32) - start[se]
    n_blocks = -(-n_assign // MOE_BLOCK) + N_EXPERTS
    n_slots = n_blocks * MOE_BLOCK
    slot_tok = jnp.zeros((n_slots,), jnp.int32).at[dest].set(flat_tok[order])
    slot_gate = jnp.zeros((n_slots,), F32).at[dest].set(flat_g[order])
    block_expert = jnp.minimum(
        jnp.searchsorted(pend, jnp.arange(n_blocks, dtype=jnp.int32) * MOE_BLOCK, side='right'),
        N_EXPERTS - 1)
    xs = t[slot_tok].reshape(n_blocks, MOE_BLOCK, d)

    def expert_block(args):
        xb, e = args
        return swiglu(xb, w1[e], w3[e], w2[e])

    ys = lax.map(expert_block, (xs, block_expert)).reshape(n_slots, d)
    out = jnp.zeros_like(t).at[slot_tok].add(ys * slot_gate[:, None].astype(t.dtype))
    return out.reshape(b_, l, d)


def ab_mixer(hc, hl, w_in, w_out, conv_w, conv_b, w_a, b_a, w_x, b_x, lam, q_norm, k_norm,
             cos, sin, need_ctx):
    g = GQA_Q_HEADS // GQA_KV_HEADS
    cuts = [LRU_WIDTH, 2 * LRU_WIDTH, 2 * LRU_WIDTH + GQA_Q_HEADS * HEAD_DIM,
            2 * LRU_WIDTH + (GQA_Q_HEADS + GQA_KV_HEADS) * HEAD_DIM]

    def project(h):
        b_, l = h.shape[:2]
        xr, gate, q, k, v = jnp.split(h @ w_in, cuts, axis=-1)
        u = depthwise_conv(xr, conv_w, conv_b, (LRU_CONV // 2, LRU_CONV - 1 - LRU_CONV // 2))
        q = rms_norm(q.reshape(b_, l, GQA_KV_HEADS, g, HEAD_DIM), q_norm)
        k = rms_norm(k.reshape(b_, l, GQA_KV_HEADS, HEAD_DIM), k_norm)
        return u, gate, q, k, v.reshape(b_, l, GQA_KV_HEADS, HEAD_DIM)

    def merge(rec, gate, att):
        b_, l = rec.shape[:2]
        y = jnp.concatenate([rec.astype(gate.dtype) * jax.nn.gelu(gate),
                             att.reshape(b_, l, GQA_Q_HEADS * HEAD_DIM)], axis=-1)
        return y @ w_out

    uc, gc, qc, kc, vc = project(hc)
    h0 = jnp.zeros((hc.shape[0], LRU_WIDTH), F32)
    rc, s_fwd, s_bwd = bidir_rglru(uc, w_a, b_a, w_x, b_x, lam, h0, h0)
    ul, gl, ql, kl, vl = project(hl)
    rl, _, _ = bidir_rglru(ul, w_a, b_a, w_x, b_x, lam, s_fwd, s_bwd)
    ql = apply_rope(ql, cos, sin)
    kl = apply_rope(kl, cos, sin)
    al = attend_blocked(ql, jnp.concatenate([kc, kl], axis=1), jnp.concatenate([vc, vl], axis=1))
    out_l = merge(rl, gl, al)
    out_c = merge(rc, gc, attend_blocked(qc, kc, vc)) if need_ctx else None
    return out_l, out_c


def cd_mixer(hc, hl, w_in, w_out, conv_w, conv_b, fw1, fb1, fw2, fb2, fw3, freq, skip,
             q_a_norm, q_b, kv_a_norm, kv_b, q_norm, k_norm, cos, sin, need_ctx):
    hy_in = (HY_ORDER + 1) * HY_WIDTH
    col_q = hy_in
    col_kv = hy_in + MLA_Q_RANK

    def hyena(z):
        l = z.shape[1]
        z = depthwise_conv(z, conv_w, conv_b, (HY_CONV // 2, HY_CONV // 2))
        parts = jnp.split(z, HY_ORDER + 1, axis=-1)
        filt = hyena_filters(l, fw1, fb1, fw2, fb2, fw3, freq)
        y = parts[0]
        for o in range(HY_ORDER):
            y = parts[o + 1] * bidir_long_conv(y, filt[:, o, 0], filt[:, o, 1], skip[o])
        return y

    def queries(q_a):
        b_, l = q_a.shape[:2]
        q = (rms_norm(q_a, q_a_norm) @ q_b).reshape(b_, l, MLA_HEADS, 1, MLA_QK)
        return rms_norm(q, q_norm)

    def keys_values(kv_a, k_rope):
        b_, l = kv_a.shape[:2]
        kv = (rms_norm(kv_a, kv_a_norm) @ kv_b).reshape(b_, l, MLA_HEADS, MLA_NOPE + MLA_V)
        k_nope, v = jnp.split(kv, [MLA_NOPE], axis=-1)
        k_r = jnp.broadcast_to(k_rope[:, :, None, :], (b_, l, MLA_HEADS, MLA_ROPE))
        return rms_norm(jnp.concatenate([k_nope, k_r], axis=-1), k_norm), v

    def rope_tail(t):
        return jnp.concatenate([t[..., :MLA_NOPE], apply_rope(t[..., MLA_NOPE:], cos, sin)], axis=-1)

    def merge(hy, att):
        b_, l = hy.shape[:2]
        return jnp.concatenate([hy, att.reshape(b_, l, MLA_HEADS * MLA_V)], axis=-1) @ w_out

    if need_ctx:
        zc = hc @ w_in
        kc, vc = keys_values(zc[..., col_kv:col_kv + MLA_KV_RANK], zc[..., col_kv + MLA_KV_RANK:])
        out_c = merge(hyena(zc[..., :hy_in]), attend_blocked(queries(zc[..., col_q:col_kv]), kc, vc))
    else:
        zc = hc @ w_in[:, col_kv:]
        kc, vc = keys_values(zc[..., :MLA_KV_RANK], zc[..., MLA_KV_RANK:])
        out_c = None
    zl = hl @ w_in
    ql = rope_tail(queries(zl[..., col_q:col_kv]))
    kl, vl = keys_values(zl[..., col_kv:col_kv + MLA_KV_RANK], zl[..., col_kv + MLA_KV_RANK:])
    kl = rope_tail(kl)
    att = attend_blocked(ql, jnp.concatenate([kc, kl], axis=1), jnp.concatenate([vc, vl], axis=1))
    out_l = merge(hyena(zl[..., :hy_in]), att)
    return out_l, out_c


def setup_inputs(seed: int = 0) -> dict:
    key = jax.random.key(seed)
    ks = iter(jax.random.split(key, 64))

    def nrm(shape, scale):
        return scale * jax.random.normal(next(ks), shape, F32)

    def gain(shape):
        return 1.0 + nrm(shape, 0.02)

    d, ne, no = D_MODEL, N_EVEN, N_ODD
    a0 = jax.random.uniform(next(ks), (ne, 2, LRU_WIDTH), F32, 0.9, 0.999) ** (1.0 / LRU_C)
    inp = {}
    inp['x'] = nrm((BATCH, SEQ, d), 1.0)
    inp['c'] = nrm((BATCH, d), 1.0)
    inp['ctx'] = nrm((BATCH, CTX_LEN, d), 1.0)
    inp['c_ctx'] = nrm((d,), 1.0)
    inp['mod_w'] = nrm((DEPTH, d, N_MOD * d), 0.5 * d ** -0.5)
    inp['mod_b'] = nrm((DEPTH, N_MOD * d), 0.02)
    inp['norm1_g'] = gain((DEPTH, d))
    inp['norm2_g'] = gain((DEPTH, d))
    inp['ab_w_in'] = nrm((ne, d, AB_IN), d ** -0.5)
    inp['ab_w_out'] = nrm((ne, MIX_WIDTH, d), MIX_WIDTH ** -0.5)
    inp['lru_conv_w'] = nrm((ne, LRU_CONV, LRU_WIDTH), LRU_CONV ** -0.5)
    inp['lru_conv_b'] = nrm((ne, LRU_WIDTH), 0.02)
    inp['lru_w_a'] = nrm((ne, 2, LRU_BLOCKS, LRU_BLOCK_SIZE, LRU_BLOCK_SIZE), LRU_BLOCK_SIZE ** -0.5)
    inp['lru_b_a'] = nrm((ne, 2, LRU_BLOCKS, LRU_BLOCK_SIZE), 0.02)
    inp['lru_w_x'] = nrm((ne, 2, LRU_BLOCKS, LRU_BLOCK_SIZE, LRU_BLOCK_SIZE), LRU_BLOCK_SIZE ** -0.5)
    inp['lru_b_x'] = nrm((ne, 2, LRU_BLOCKS, LRU_BLOCK_SIZE), 0.02)
    inp['lru_lambda'] = jnp.log(a0) - jnp.log1p(-a0)
    inp['gqa_q_norm'] = gain((ne, HEAD_DIM))
    inp['gqa_k_norm'] = gain((ne, HEAD_DIM))
    inp['ffn_w1'] = nrm((ne, d, D_FF), d ** -0.5)
    inp['ffn_w3'] = nrm((ne, d, D_FF), d ** -0.5)
    inp['ffn_w2'] = nrm((ne, D_FF, d), D_FF ** -0.5)
    inp['cd_w_in'] = nrm((no, d, CD_IN), d ** -0.5)
    inp['cd_w_out'] = nrm((no, MIX_WIDTH, d), MIX_WIDTH ** -0.5)
    inp['hy_conv_w'] = nrm((no, HY_CONV, (HY_ORDER + 1) * HY_WIDTH), HY_CONV ** -0.5)
    inp['hy_conv_b'] = nrm((no, (HY_ORDER + 1) * HY_WIDTH), 0.02)
    inp['hy_filt_w1'] = nrm((no, HY_POS_DIM, HY_FILTER_HIDDEN), HY_POS_DIM ** -0.5)
    inp['hy_filt_b1'] = nrm((no, HY_FILTER_HIDDEN), 0.02)
    inp['hy_filt_w2'] = nrm((no, HY_FILTER_HIDDEN, HY_FILTER_HIDDEN), HY_FILTER_HIDDEN ** -0.5)
    inp['hy_filt_b2'] = nrm((no, HY_FILTER_HIDDEN), 0.02)
    inp['hy_filt_w3'] = nrm((no, HY_FILTER_HIDDEN, HY_ORDER * 2 * HY_WIDTH), 0.05 * HY_FILTER_HIDDEN ** -0.5)
    inp['hy_sin_freq'] = gain((no, HY_FILTER_HIDDEN))
    inp['hy_skip'] = nrm((no, HY_ORDER, HY_WIDTH), 0.5)
    inp['mla_q_a_norm'] = gain((no, MLA_Q_RANK))
    inp['mla_q_b'] = nrm((no, MLA_Q_RANK, MLA_HEADS * MLA_QK), MLA_Q_RANK ** -0.5)
    inp['mla_kv_a_norm'] = gain((no, MLA_KV_RANK))
    inp['mla_kv_b'] = nrm((no, MLA_KV_RANK, MLA_HEADS * (MLA_NOPE + MLA_V)), MLA_KV_RANK ** -0.5)
    inp['mla_q_norm'] = gain((no, MLA_QK))
    inp['mla_k_norm'] = gain((no, MLA_QK))
    inp['moe_router'] = nrm((no, d, N_EXPERTS), d ** -0.5)
    inp['moe_w1'] = nrm((no, N_EXPERTS, d, D_FF_EXPERT), d ** -0.5)
    inp['moe_w3'] = nrm((no, N_EXPERTS, d, D_FF_EXPERT), d ** -0.5)
    inp['moe_w2'] = nrm((no, N_EXPERTS, D_FF_EXPERT, d), D_FF_EXPERT ** -0.5)
    return inp


def reference(x, c, ctx, c_ctx, mod_w, mod_b, norm1_g, norm2_g,
              ab_w_in, ab_w_out, lru_conv_w, lru_conv_b, lru_w_a, lru_b_a, lru_w_x, lru_b_x, lru_lambda,
              gqa_q_norm, gqa_k_norm, ffn_w1, ffn_w3, ffn_w2,
              cd_w_in, cd_w_out, hy_conv_w, hy_conv_b, hy_filt_w1, hy_filt_b1, hy_filt_w2, hy_filt_b2,
              hy_filt_w3, hy_sin_freq, hy_skip, mla_q_a_norm, mla_q_b, mla_kv_a_norm, mla_kv_b,
              mla_q_norm, mla_k_norm, moe_router, moe_w1, moe_w3, moe_w2):
    rows = x.shape[1] // GRID_W
    cos_g, sin_g = grid_angles(rows, HEAD_DIM)
    cos_m, sin_m = grid_angles(rows, MLA_ROPE)
    silu_l = jax.nn.silu(c)
    silu_c = jax.nn.silu(c_ctx)[None, :]
    xl, xc = x, ctx
    for layer in range(DEPTH):
        j = layer // 2
        even = layer % 2 == 0
        need_ctx = layer < DEPTH - 1
        mod_l = [m[:, None, :] for m in jnp.split(silu_l @ mod_w[layer] + mod_b[layer], N_MOD, axis=-1)]
        mod_c = [m[:, None, :] for m in jnp.split(silu_c @ mod_w[layer] + mod_b[layer], N_MOD, axis=-1)]
        hl = modulate(rms_norm(xl, norm1_g[layer]), mod_l[0], mod_l[1])
        hc = modulate(rms_norm(xc, norm1_g[layer]), mod_c[0], mod_c[1])
        if even:
            mix_l, mix_c = ab_mixer(hc, hl, ab_w_in[j], ab_w_out[j], lru_conv_w[j], lru_conv_b[j],
                                    lru_w_a[j], lru_b_a[j], lru_w_x[j], lru_b_x[j], lru_lambda[j],
                                    gqa_q_norm[j], gqa_k_norm[j], cos_g, sin_g, need_ctx)
        else:
            mix_l, mix_c = cd_mixer(hc, hl, cd_w_in[j], cd_w_out[j], hy_conv_w[j], hy_conv_b[j],
                                    hy_filt_w1[j], hy_filt_b1[j], hy_filt_w2[j], hy_filt_b2[j],
                                    hy_filt_w3[j], hy_sin_freq[j], hy_skip[j],
                                    mla_q_a_norm[j], mla_q_b[j], mla_kv_a_norm[j], mla_kv_b[j],
                                    mla_q_norm[j], mla_k_norm[j], cos_m, sin_m, need_ctx)

        def channel_mix(h):
            if even:
                return swiglu(h, ffn_w1[j], ffn_w3[j], ffn_w2[j])
            return moe_swiglu(h, moe_router[j], moe_w1[j], moe_w3[j], moe_w2[j])

        xl = xl + mod_l[2] * mix_l
        xl = xl + mod_l[5] * channel_mix(modulate(rms_norm(xl, norm2_g[layer]), mod_l[3], mod_l[4]))
        if need_ctx:
            xc = xc + mod_c[2] * mix_c
            xc = xc + mod_c[5] * channel_mix(modulate(rms_norm(xc, norm2_g[layer]), mod_c[3], mod_c[4]))
    return xl
```

```python
import contextlib
import math
import numpy as np
import ml_dtypes
import concourse.bass as bass
import concourse.mybir as mybir
from concourse.bass_utils import run_bass_kernel_spmd

F32 = mybir.dt.float32
BF16 = mybir.dt.bfloat16
U8 = mybir.dt.uint8
AF = mybir.ActivationFunctionType
ALU = mybir.AluOpType

ENGS = ("pe", "act", "dve", "pool", "sp")
N_DMA_SEMS = 16
DMA_POOLS = {"sp": list(range(0, 7)), "pool": list(range(7, 14)), "act": [14, 15]}
PAGE = 256
ARENA = 207 * 1024
PSUM_PAGE0 = 100000
DRAM_PAGE0 = 200000

D = 1024
T = 2304
NCTX = 256
NLAT = 2048
EPS = 1e-6


class Op:
    __slots__ = ("eng", "fn", "dma", "preds", "sig", "sigval", "dsem", "dval", "idx")


class Prog:
    def __init__(self, nc):
        self.nc = nc
        self.ops = []
        self.pw = {}
        self.pr = {}

    def add(self, eng, fn, reads=(), writes=(), dma=False):
        op = Op()
        op.eng = eng
        op.fn = fn
        op.dma = dma
        op.idx = len(self.ops)
        op.sig = False
        preds = set()
        pw, pr = self.pw, self.pr
        for (lo, hi) in reads:
            for p in range(lo, hi):
                w = pw.get(p)
                if w is not None:
                    preds.add(w)
        for (lo, hi) in writes:
            for p in range(lo, hi):
                w = pw.get(p)
                if w is not None:
                    preds.add(w)
                r = pr.get(p)
                if r:
                    preds.update(r.values())
        rkey = ("d", op.idx) if dma else eng
        for (lo, hi) in reads:
            for p in range(lo, hi):
                r = pr.get(p)
                if r is None:
                    pr[p] = {rkey: op.idx}
                else:
                    r[rkey] = op.idx
        for (lo, hi) in writes:
            for p in range(lo, hi):
                pw[p] = op.idx
                pr[p] = None
        preds.discard(op.idx)
        op.preds = preds
        self.ops.append(op)
        return op

    def emit(self):
        nc = self.nc
        ops = self.ops
        with contextlib.ExitStack() as es:
            esem = {e: es.enter_context(nc.semaphore(f"s_{e}")) for e in ENGS}
            dsems = [es.enter_context(nc.semaphore(f"s_dma{i}")) for i in range(N_DMA_SEMS)]
            waits = [None] * len(ops)
            for op in ops:
                per_eng = {}
                dma_w = []
                for p in op.preds:
                    po = ops[p]
                    if po.dma:
                        dma_w.append(p)
                    else:
                        if po.eng == "pe" and op.eng == "pe" and not op.dma:
                            continue
                        if po.eng not in per_eng or per_eng[po.eng] < p:
                            per_eng[po.eng] = p
                for p in per_eng.values():
                    ops[p].sig = True
                waits[op.idx] = (list(per_eng.values()), dma_w)
            cnt = {e: 0 for e in ENGS}
            dcnt = [0] * N_DMA_SEMS
            dlast = [None] * N_DMA_SEMS
            dpos = {e: 0 for e in ENGS}
            dma_prev = {}
            for op in ops:
                if op.dma:
                    rng = DMA_POOLS[op.eng]
                    k = rng[dpos[op.eng] % len(rng)]
                    dpos[op.eng] += 1
                    dcnt[k] += 16
                    op.dsem = k
                    op.dval = dcnt[k]
                    dma_prev[op.idx] = dlast[k]
                    dlast[k] = op.idx
                elif op.sig:
                    cnt[op.eng] += 1
                    op.sigval = cnt[op.eng]
            streams = {e: [o for o in ops if o.eng == e] for e in ENGS}
            self.stats = {e: len(streams[e]) for e in ENGS}
            self.stats["sig"] = dict(cnt)

            def run_stream(e, eng):
                known = {}

                def wait(key, sem, val):
                    if known.get(key, 0) >= val:
                        return
                    known[key] = val
                    eng.wait_ge(sem, val)

                for op in streams[e]:
                    cw, dw = waits[op.idx]
                    for p in cw:
                        po = ops[p]
                        wait(po.eng, esem[po.eng], po.sigval)
                    for p in dw:
                        po = ops[p]
                        wait(("d", po.dsem), dsems[po.dsem], po.dval)
                    if op.dma:
                        pp = dma_prev[op.idx]
                        if pp is not None:
                            po = ops[pp]
                            wait(("d", po.dsem), dsems[po.dsem], po.dval)
                        ins = op.fn(eng)
                        ins.then_inc(dsems[op.dsem], 16)
                    else:
                        ins = op.fn(eng)
                        if op.sig:
                            ins.then_inc(esem[e], 1)
                if e == "sp":
                    for k in range(N_DMA_SEMS):
                        if dcnt[k]:
                            eng.wait_ge(dsems[k], dcnt[k])

            with nc.Block() as block:
                @block.tensor
                def _(eng):
                    run_stream("pe", eng)

                @block.scalar
                def _(eng):
                    run_stream("act", eng)

                @block.vector
                def _(eng):
                    run_stream("dve", eng)

                @block.gpsimd
                def _(eng):
                    run_stream("pool", eng)

                @block.sync
                def _(eng):
                    run_stream("sp", eng)


class V:
    __slots__ = ("ap", "pg")

    def __init__(self, ap, pg):
        self.ap = ap
        self.pg = pg

    def m(self, f):
        return V(f(self.ap), self.pg)


class Tile:
    def __init__(self, k, off, shape, dt, name):
        self.k = k
        self.off = off
        self.shape = list(shape)
        self.dt = dt
        self.es = 4 if dt == F32 else (2 if dt == BF16 else 1)
        n = 1
        for s in shape[1:]:
            n *= s
        self.n = n
        self.nbytes = n * self.es
        base = k.arena[0:shape[0], off:off + self.nbytes]
        if dt != U8:
            base = base.bitcast(dt)
        if len(shape) == 3:
            base = base.rearrange("p (a b) -> p a b", a=shape[1])
        elif len(shape) == 4:
            base = base.rearrange("p (a b c) -> p a b c", a=shape[1], b=shape[2])
        self.ap = base
        self.name = name

    def _pages(self, lo_e, hi_e):
        lo = self.off + lo_e * self.es
        hi = self.off + hi_e * self.es
        return (lo // PAGE, (hi + PAGE - 1) // PAGE)

    @property
    def all(self):
        return V(self.ap, [self._pages(0, self.n)])

    def __getitem__(self, idx):
        if not isinstance(idx, tuple):
            idx = (idx,)
        ap = self.ap[idx]
        fidx = list(idx[1:]) + [slice(None)] * (len(self.shape) - len(idx))
        dims = self.shape[1:]
        rng = []
        for dim, ix in zip(dims, fidx):
            if isinstance(ix, int):
                rng.append((ix, ix + 1))
            else:
                a = 0 if ix.start is None else ix.start
                b = dim if ix.stop is None else ix.stop
                rng.append((a, b))
        strides = []
        st = 1
        for dim in reversed(dims):
            strides.insert(0, st)
            st *= dim
        lead = rng[:-1]
        cnt = 1
        for a, b in lead:
            cnt *= (b - a)
        pages = []
        if cnt <= 64:
            import itertools
            for combo in itertools.product(*[range(a, b) for a, b in lead]):
                base = sum(c * s_ for c, s_ in zip(combo, strides[:-1]))
                pages.append(self._pages(base + rng[-1][0], base + rng[-1][1]))
        else:
            lo = sum(a * s_ for (a, b), s_ in zip(rng, strides))
            hi = sum((b - 1) * s_ for (a, b), s_ in zip(rng, strides)) + 1
            pages.append(self._pages(lo, hi))
        return V(ap, pages)


def pgs(vs):
    out = []
    for v in vs:
        out.extend(v.pg)
    return out


class K:
    def __init__(self, nc, es):
        self.nc = nc
        self.P = Prog(nc)
        self.arena_t = es.enter_context(nc.sbuf_tensor("arena", [128, ARENA], U8))
        self.arena = self.arena_t
        self.free_list = [(0, ARENA)]
        self.banks = [es.enter_context(nc.psum_tensor(f"bank{i}", [128, 512], F32)) for i in range(8)]
        self.bank_i = 0
        self.dram_pg = DRAM_PAGE0

    def tile(self, shape, dt, name="t"):
        es_ = 4 if dt == F32 else (2 if dt == BF16 else 1)
        n = 1
        for s in shape[1:]:
            n *= s
        nb = ((n * es_ + PAGE - 1) // PAGE) * PAGE
        for i, (o, sz) in enumerate(self.free_list):
            if sz >= nb:
                if sz == nb:
                    self.free_list.pop(i)
                else:
                    self.free_list[i] = (o + nb, sz - nb)
                t = Tile(self, o, shape, dt, name)
                t.alloc = nb
                return t
        raise RuntimeError(f"SBUF arena full allocating {name} {shape} ({nb}B); free={self.free_list}")

    def free(self, *tiles):
        for t in tiles:
            self.free_list.append((t.off, t.alloc))
        self.free_list.sort()
        merged = []
        for o, sz in self.free_list:
            if merged and merged[-1][0] + merged[-1][1] == o:
                merged[-1] = (merged[-1][0], merged[-1][1] + sz)
            else:
                merged.append((o, sz))
        self.free_list = merged

    def ps(self, i, rows=128, c0=0, c1=512, r0=0):
        b = self.banks[i]
        return V(b[r0:r0 + rows, c0:c1], [(PSUM_PAGE0 + i, PSUM_PAGE0 + i + 1)])

    def ps3(self, i, a, rows=128):
        b = self.banks[i]
        return V(b[0:rows, :].rearrange("p (a b) -> p a b", a=a), [(PSUM_PAGE0 + i, PSUM_PAGE0 + i + 1)])

    def dram(self, ap):
        self.dram_pg += 1
        return V(ap, [(self.dram_pg, self.dram_pg + 1)])

    def op(self, eng, fn, R=(), W=()):
        self.P.add(eng, fn, pgs(R), pgs(W))

    def dma(self, eng, out, in_):
        self.P.add(eng, lambda e: e.dma_start(out=out.ap, in_=in_.ap), pgs([in_]), pgs([out]), dma=True)

    def mm(self, out, lhsT, rhs, start=True, stop=True):
        self.P.add("pe", lambda e: e.matmul(out.ap, lhsT=lhsT.ap, rhs=rhs.ap, start=start, stop=stop),
                   pgs([lhsT, rhs]), pgs([out]))

    def transpose(self, out, in_, ident):
        self.P.add("pe", lambda e: e.transpose(out.ap, in_.ap, ident.ap), pgs([in_, ident]), pgs([out]))

    def act(self, out, in_, func, bias=None, scale=None):
        R = [in_]
        kw = {}
        if bias is not None:
            if isinstance(bias, V):
                R.append(bias)
                kw["bias"] = bias.ap
            else:
                kw["bias"] = float(bias)
        if scale is not None:
            if isinstance(scale, V):
                R.append(scale)
                kw["scale"] = scale.ap
            else:
                kw["scale"] = float(scale)
        self.P.add("act", lambda e: e.activation(out=out.ap, in_=in_.ap, func=func, **kw),
                   pgs(R), pgs([out]))

    def copy(self, eng, out, in_):
        if eng == "act":
            self.P.add("act", lambda e: e.copy(out=out.ap, in_=in_.ap), pgs([in_]), pgs([out]))
        else:
            self.P.add(eng, lambda e: e.tensor_copy(out=out.ap, in_=in_.ap), pgs([in_]), pgs([out]))

    def tt(self, eng, out, in0, in1, op):
        self.P.add(eng, lambda e: e.tensor_tensor(out=out.ap, in0=in0.ap, in1=in1.ap, op=op),
                   pgs([in0, in1]), pgs([out]))

    def ts(self, eng, out, in0, s1, s2, op0, op1=None):
        R = [in0]
        a1 = s1.ap if isinstance(s1, V) else float(s1)
        if isinstance(s1, V):
            R.append(s1)
        if s2 is None:
            self.P.add(eng, lambda e: e.tensor_scalar(out=out.ap, in0=in0.ap, scalar1=a1, scalar2=None, op0=op0),
                       pgs(R), pgs([out]))
            return
        a2 = s2.ap if isinstance(s2, V) else float(s2)
        if isinstance(s2, V):
            R.append(s2)
        self.P.add(eng, lambda e: e.tensor_scalar(out=out.ap, in0=in0.ap, scalar1=a1, scalar2=a2, op0=op0, op1=op1),
                   pgs(R), pgs([out]))

    def stt(self, eng, out, in0, scalar, in1, op0, op1):
        R = [in0, in1]
        a = scalar.ap if isinstance(scalar, V) else float(scalar)
        if isinstance(scalar, V):
            R.append(scalar)
        self.P.add(eng, lambda e: e.scalar_tensor_tensor(out=out.ap, in0=in0.ap, scalar=a, in1=in1.ap, op0=op0, op1=op1),
                   pgs(R), pgs([out]))

    def memset(self, eng, out, val):
        self.P.add(eng, lambda e: e.memset(out.ap, val), [], pgs([out]))

    def recip(self, out, in_):
        self.P.add("dve", lambda e: e.reciprocal(out=out.ap, in_=in_.ap), pgs([in_]), pgs([out]))

    def scan(self, out, d0, d1, initial):
        R = [d0, d1]
        a = initial.ap if isinstance(initial, V) else float(initial)
        if isinstance(initial, V):
            R.append(initial)
        self.P.add("dve", lambda e: e.tensor_tensor_scan(out=out.ap, data0=d0.ap, data1=d1.ap, initial=a,
                                                       op0=ALU.mult, op1=ALU.add),
                   pgs(R), pgs([out]))


def col_blocks(ctx=True, lat=True):
    b = []
    if ctx:
        b.append((0, NCTX, 1))
    if lat:
        for i in range(4):
            b.append((NCTX + 512 * i, 512, 0))
    return b


def fm(v):
    v = np.asarray(v)
    n = v.shape[-1] // 128
    return np.ascontiguousarray(v.reshape(v.shape[:-1] + (n, 128)).swapaxes(-1, -2))


def rope_tables(rot_dim, nfeat, rot_off):
    n_freq = rot_dim // 4
    inv_freq = (10000.0 ** (-np.arange(n_freq, dtype=np.float32) / n_freq)).astype(np.float32)
    t = np.arange(NLAT)
    row = (t // 64).astype(np.float32)
    col = (t % 64).astype(np.float32)
    ang = np.concatenate([row[:, None] * inv_freq, col[:, None] * inv_freq], axis=-1).astype(np.float32)
    cos = np.cos(ang).astype(np.float32)
    sin = np.sin(ang).astype(np.float32)
    C = np.ones((nfeat, T), np.float32)
    S = np.zeros((nfeat, T), np.float32)
    half = rot_dim // 2
    for f in range(rot_off, nfeat):
        i = (f - rot_off) % half
        C[f, NCTX:] = cos[:, i]
        S[f, NCTX:] = sin[:, i]
    R = np.zeros((nfeat, nfeat), np.float32)
    for i in range(half):
        R[rot_off + i + half, rot_off + i] = -1.0
        R[rot_off + i, rot_off + i + half] = 1.0
    return C, S, R


def host_consts():
    c = {}
    c["ident"] = np.eye(128, dtype=np.float32)
    cg, sg, rg = rope_tables(64, 64, 0)
    c["rope_g_cos"], c["rope_g_sin"], c["rope_g_R"] = cg, sg, rg
    cm, sm, rm = rope_tables(32, 96, 64)
    c["rope_m_cos"], c["rope_m_sin"], c["rope_m_R"] = cm, sm, rm
    L = NLAT
    N = 2 * L
    s = np.arange(L, dtype=np.float64)[:, None]
    f = np.arange(L, dtype=np.float64)[None, :]
    th = np.pi * (2 * f + 1) * s / N
    Cm = np.cos(th)
    Sm = np.sin(th)
    bf = ml_dtypes.bfloat16
    def tile_(M):
        return np.ascontiguousarray(M.reshape(16, 128, 16, 128).transpose(2, 1, 0, 3).reshape(16, 128, 2048)).astype(bf)
    c["dft_cf"] = tile_(Cm)
    c["dft_sf"] = tile_(Sm)
    c["dft_ci"] = tile_(Cm.T * (2.0 / N))
    c["dft_si"] = tile_(-Sm.T * (2.0 / N))
    t = np.arange(L, dtype=np.float32)[:, None]
    t_norm = t / max(L - 1, 1)
    bands = np.linspace(1e-4, 16 - 1, 16, dtype=np.float32)
    ang = (2.0 * math.pi * t * bands / L).astype(np.float32)
    z = np.concatenate([t_norm, np.cos(ang), -np.sin(ang)], axis=-1).astype(np.float32)
    c["hy_zT"] = np.ascontiguousarray(z.T)
    HY_MIN = math.log(1e-2) / 1.5
    HY_MAX = math.log(1e-2) / 0.3
    deltas = np.abs(np.linspace(HY_MIN, HY_MAX, 512, dtype=np.float32))
    window = (np.exp(-t_norm * deltas) + 0.05).astype(np.float32)
    c["hy_window"] = window
    return c


def build(shapes, stage=99):
    nc = bass.Bass("TRN2", target_bir_lowering=False)
    dr = {}
    for name, shp in shapes.items():
        dt_ = F32
        if isinstance(shp, tuple) and len(shp) == 2 and isinstance(shp[1], str):
            shp, dts = shp
            dt_ = BF16 if dts == "bfloat16" else F32
        dr[name] = nc.dram_tensor(name, list(shp), dt_, kind="ExternalInput").ap()
    out_d = nc.dram_tensor("out", [NLAT, D], F32, kind="ExternalOutput").ap()
    outc_d = nc.dram_tensor("outc", [NCTX, D], F32, kind="ExternalOutput").ap()
    with contextlib.ExitStack() as es:
        k = K(nc, es)
        _emit_program(k, dr, out_d, outc_d, stage)
        k.P.emit()
    return nc


def _emit_program(k, dr, out_d, outc_d, stage):
    dm = k.dram
    SW = "pool"

    xs = k.tile([128, 8, T], F32, "xs")
    hbf = k.tile([128, 8, T], BF16, "hbf")
    ident_f = k.tile([128, 128], F32, "ident_f")
    ident_b = k.tile([128, 128], BF16, "ident_b")
    ones_b = k.tile([128, 128], BF16, "ones_b")
    mods = k.tile([128, 2, 48, 2], F32, "mods")
    affA = k.tile([128, 2, 2, 16], F32, "affA")
    gdup = k.tile([128, 2, 2, 16], F32, "gdup")
    modb = k.tile([128, 2, 48], F32, "modb")

    k.dma("sp", ident_f.all, dm(dr["ident"]))
    k.copy("dve", ident_b.all, ident_f.all)
    k.memset("dve", ones_b.all, 1.0)
    k.dma("sp", gdup.all, dm(dr["gdup"]))
    k.dma("sp", modb.all, dm(dr["modb"]))

    xts = [k.tile([128, 1024], F32, f"xt{i}") for i in range(2)]
    for tc in range(18):
        src = dr["ctx"][tc * 128:(tc + 1) * 128, :] if tc < 2 else dr["x"][(tc - 2) * 128:(tc - 1) * 128, :]
        xt = xts[tc % 2]
        k.dma("sp", xt.all, dm(src))
        for half in range(2):
            b = (tc * 2 + half) % 4
            for kk in range(4):
                kf = half * 4 + kk
                k.transpose(k.ps(b, c0=kk * 128, c1=(kk + 1) * 128), xt[:, kf * 128:(kf + 1) * 128], ident_f.all)
            k.copy("dve" if half == 0 else "act", xs[:, half * 4:(half + 1) * 4, tc * 128:(tc + 1) * 128], k.ps3(b, 4))
    k.free(*xts)

    cl = k.tile([128, 8], F32, "cl")
    cc = k.tile([128, 8], F32, "cc")
    s_bf = k.tile([128, 8, 2], BF16, "s_bf")
    k.dma("sp", cl.all, dm(dr["c_fm"]))
    k.dma("sp", cc.all, dm(dr["cctx_fm"]))
    k.act(s_bf[:, :, 0], cl.all, AF.Silu)
    k.act(s_bf[:, :, 1], cc.all, AF.Silu)
    mwt = [k.tile([128, 8, 512], BF16, f"mw{i}") for i in range(2)]
    gi = 0
    for l in range(2):
        mw = dr["mod_w"][l].rearrange("(kc p) n -> p kc n", p=128)
        for g in range(12):
            wt = mwt[gi % 2]
            k.dma(SW, wt.all, dm(mw[:, :, g * 512:(g + 1) * 512]))
            b = 4 + gi % 2
            for j in range(4):
                for kk in range(8):
                    k.mm(k.ps(b, c0=2 * j, c1=2 * j + 2), wt[:, kk, j * 128:(j + 1) * 128], s_bf[:, kk, :],
                         start=(kk == 0), stop=(kk == 7))
            for j in range(4):
                ch = g * 4 + j
                k.ts("dve", mods[:, l, ch, :], k.ps(b, c0=2 * j, c1=2 * j + 2), modb[:, l, ch:ch + 1], None, ALU.add)
            gi += 1
    k.free(*mwt, cl, cc, s_bf)
    for l in range(2):
        for n, mi in ((0, 1), (1, 4)):
            src = V(mods.ap[:, l, mi * 8:(mi + 1) * 8, :].rearrange("p a b -> p (a b)"), mods[:, l, mi * 8:(mi + 1) * 8, :].pg)
            k.stt("dve", affA[:, l, n, :], src, 1.0, gdup[:, l, n, :], ALU.add, ALU.mult)

    def A_(l, n, kk, s):
        return affA[:, l, n, kk * 2 + s:kk * 2 + s + 1]

    def M_(l, mi, kk, s):
        return mods[:, l, mi * 8 + kk, s:s + 1]

    def norm_mod(l, n, blocks, f32_hook=None):
        sqs = [k.tile([128, 8, 512], BF16, f"sq{i}") for i in range(2)]
        rstds = [k.tile([128, 512], F32, f"rstd{i}") for i in range(2)]
        tmps = [k.tile([128, 512], F32, f"nt{i}") for i in range(3)]
        ti = 0
        for bi, (c0, ncl, s) in enumerate(blocks):
            sq = sqs[bi % 2]
            rstd = rstds[bi % 2]
            for kk in range(8):
                k.act(sq[:, kk, 0:ncl], xs[:, kk, c0:c0 + ncl], AF.Square)
            b = 4 + bi % 2
            for kk in range(8):
                k.mm(k.ps(b, c1=ncl), ones_b.all, sq[:, kk, 0:ncl], start=(kk == 0), stop=(kk == 7))
            k.act(rstd[:, 0:ncl], k.ps(b, c1=ncl), AF.Sqrt, bias=EPS, scale=1.0 / D)
            k.recip(rstd[:, 0:ncl], rstd[:, 0:ncl])
            for kk in range(8):
                tmp = tmps[ti % 3]
                ti += 1
                k.tt("dve" if kk % 2 == 0 else "pool", tmp[:, 0:ncl], xs[:, kk, c0:c0 + ncl], rstd[:, 0:ncl], ALU.mult)
                k.act(hbf[:, kk, c0:c0 + ncl], tmp[:, 0:ncl], AF.Identity,
                      bias=M_(l, 0 if n == 0 else 3, kk, s), scale=A_(l, n, kk, s))
                if f32_hook is not None:
                    f32_hook(bi, c0, ncl, kk, tmp, s)
        k.free(*sqs, *rstds, *tmps)

    def write_out():
        ots = [k.tile([128, 1024], F32, f"ot{i}") for i in range(2)]
        for tc in range(18):
            ot = ots[tc % 2]
            for half in range(2):
                b = (tc * 2 + half) % 4
                for kk in range(4):
                    kf = half * 4 + kk
                    k.transpose(k.ps(b, c0=kk * 128, c1=(kk + 1) * 128), xs[:, kf, tc * 128:(tc + 1) * 128], ident_f.all)
                k.copy("dve" if half == 0 else "act", ot[:, half * 512:(half + 1) * 512], k.ps(b))
            dst = outc_d[tc * 128:(tc + 1) * 128, :] if tc < 2 else out_d[(tc - 2) * 128:(tc - 1) * 128, :]
            k.dma("sp", dm(dst), ot.all)
        k.free(*ots)

    if stage == 0:
        write_out()
        return

    def qk_norm_rope(psv, nrows, gain, onesv, cosT, sinT, Rm, outv, c0, ncl, tl, bi, banks):
        sq, rstd, qn, t1 = tl
        k.act(sq[0:nrows, 0:ncl], psv, AF.Square)
        b1 = banks[0]
        k.mm(k.ps(b1, rows=nrows, c1=ncl), onesv, sq[0:nrows, 0:ncl])
        k.act(rstd[0:nrows, 0:ncl], k.ps(b1, rows=nrows, c1=ncl), AF.Sqrt, bias=EPS, scale=1.0 / nrows)
        k.recip(rstd[0:nrows, 0:ncl], rstd[0:nrows, 0:ncl])
        k.stt("dve", qn[0:nrows, 0:ncl], psv, gain, rstd[0:nrows, 0:ncl], ALU.mult, ALU.mult)
        b2 = banks[1]
        k.copy("act", sq[0:nrows, 0:ncl], qn[0:nrows, 0:ncl])
        k.mm(k.ps(b2, rows=nrows, c1=ncl), Rm, sq[0:nrows, 0:ncl])
        k.tt("pool", t1[0:nrows, 0:ncl], qn[0:nrows, 0:ncl], cosT[0:nrows, c0:c0 + ncl], ALU.mult)
        k.tt("dve", qn[0:nrows, 0:ncl], k.ps(b2, rows=nrows, c1=ncl), sinT[0:nrows, c0:c0 + ncl], ALU.mult)
        k.tt("pool", outv, t1[0:nrows, 0:ncl], qn[0:nrows, 0:ncl], ALU.add)

    def attend(qh, kh, vaug, Dh, q_blocks, n_kc, scale, ycat, yrow0, ychunk, etl, rdt):
        for qi, (c0, ncl) in enumerate(q_blocks):
            ob = qi % 2
            for kc in range(n_kc):
                sb = 2 + kc % 3
                e = etl[kc % 3]
                k.mm(k.ps(sb, c1=ncl), kh[0:Dh, kc * 128:(kc + 1) * 128], qh[0:Dh, c0:c0 + ncl])
                k.act(e[:, 0:ncl], k.ps(sb, c1=ncl), AF.Exp, scale=scale)
                k.mm(k.ps(ob, c1=ncl), vaug[:, kc, 64:192], e[:, 0:ncl], start=(kc == 0), stop=(kc == n_kc - 1))
            rd, rs = rdt[qi % len(rdt)]
            k.recip(rd[64:128, 0:ncl], k.ps(ob, rows=64, r0=64, c1=ncl))
            k.copy("pool", rs[0:64, 0:ncl], rd[64:128, 0:ncl])
            k.tt("dve", ycat[yrow0:yrow0 + 64, ychunk, c0:c0 + ncl], k.ps(ob, rows=64, c1=ncl), rs[0:64, 0:ncl], ALU.mult)

    def project_out(l, ycat, w_d, krow0, blocks):
        wo = k.tile([128, 4, 1024], BF16, "wo")
        k.dma(SW, wo.all, dm(w_d[krow0:krow0 + 512, :].rearrange("(kc p) n -> p kc n", p=128)))
        i = 0
        for (c0, ncl, s) in blocks:
            for d in range(8):
                b = 4 + i % 4
                i += 1
                for kk in range(4):
                    k.mm(k.ps(b, c1=ncl), wo[:, kk, d * 128:(d + 1) * 128], ycat[:, kk, c0:c0 + ncl],
                         start=(kk == 0), stop=(kk == 3))
                k.stt("dve", xs[:, d, c0:c0 + ncl], k.ps(b, c1=ncl), M_(l, 2, d, s),
                      xs[:, d, c0:c0 + ncl], ALU.mult, ALU.add)
        k.free(wo)

    def ffn(l, w1_d, w3_d, w2_d, dff, blocks, hid, gate_bc=None):
        ngroups = (dff + 511) // 512
        w1t = [k.tile([128, 8, 512], BF16, f"w1_{i}") for i in range(2)]
        w3t = [k.tile([128, 8, 512], BF16, f"w3_{i}") for i in range(2)]
        w2t = [k.tile([128, 4, 1024], BF16, f"w2_{i}") for i in range(2)]
        sts = [k.tile([128, 512], F32, f"st{i}") for i in range(3)]
        w1v = w1_d.rearrange("(kc p) n -> p kc n", p=128)
        w3v = w3_d.rearrange("(kc p) n -> p kc n", p=128)
        si = 0
        ai = 0
        for g in range(ngroups):
            f0 = g * 512
            fw = min(512, dff - f0)
            nch = fw // 128
            w1, w3, w2 = w1t[g % 2], w3t[g % 2], w2t[g % 2]
            k.dma(SW, w1[:, :, 0:fw], dm(w1v[:, :, f0:f0 + fw]))
            k.dma(SW, w3[:, :, 0:fw], dm(w3v[:, :, f0:f0 + fw]))
            k.dma(SW, w2[:, 0:nch, :], dm(w2_d[f0:f0 + fw, :].rearrange("(kc p) n -> p kc n", p=128)))
            for m in range(nch):
                for (c0, ncl, s) in blocks:
                    bg = si % 2
                    bu = 2 + si % 2
                    st = sts[si % 3]
                    si += 1
                    for kk in range(8):
                        k.mm(k.ps(bg, c1=ncl), w1[:, kk, m * 128:(m + 1) * 128], hbf[:, kk, c0:c0 + ncl],
                             start=(kk == 0), stop=(kk == 7))
                    for kk in range(8):
                        k.mm(k.ps(bu, c1=ncl), w3[:, kk, m * 128:(m + 1) * 128], hbf[:, kk, c0:c0 + ncl],
                             start=(kk == 0), stop=(kk == 7))
                    k.act(st[:, 0:ncl], k.ps(bg, c1=ncl), AF.Silu)
                    if gate_bc is None:
                        k.tt("dve", hid[:, m, c0:c0 + ncl], st[:, 0:ncl], k.ps(bu, c1=ncl), ALU.mult)
                    else:
                        k.tt("dve", st[:, 0:ncl], st[:, 0:ncl], k.ps(bu, c1=ncl), ALU.mult)
                        k.tt("pool", hid[:, m, c0:c0 + ncl], st[:, 0:ncl], gate_bc[:, c0 - NCTX:c0 - NCTX + ncl], ALU.mult)
            for (c0, ncl, s) in blocks:
                for d in range(8):
                    b = 4 + ai % 4
                    ai += 1
                    for kk in range(nch):
                        k.mm(k.ps(b, c1=ncl), w2[:, kk, d * 128:(d + 1) * 128], hid[:, kk, c0:c0 + ncl],
                             start=(kk == 0), stop=(kk == nch - 1))
                    k.stt("dve", xs[:, d, c0:c0 + ncl], k.ps(b, c1=ncl), M_(l, 5, d, s),
                          xs[:, d, c0:c0 + ncl], ALU.mult, ALU.add)
        k.free(*w1t, *w3t, *w2t, *sts)

    ALLB = col_blocks(True, True)
    LATB = col_blocks(False, True)

    norm_mod(0, 0, ALLB)
    ycat = k.tile([128, 4, T], BF16, "ycat")
    layer0_mixer(k, dr, xs, hbf, ycat, ident_f, ones_b, M_, project_out, qk_norm_rope, attend, ALLB)
    if stage == 1:
        write_out()
        return
    norm_mod(0, 1, ALLB)
    ffn(0, dr["ffn_w1"], dr["ffn_w3"], dr["ffn_w2"], 2816, ALLB, ycat)
    if stage == 2:
        write_out()
        return
    import os
    SUB = int(os.environ.get("SUB1", "99"))
    norm_mod(1, 0, ALLB)
    layer1_mixer(k, dr, xs, hbf, ycat, ident_b, ones_b, M_, project_out, qk_norm_rope, attend, ALLB, LATB, SUB)
    if stage == 3:
        write_out()
        return
    moe_layer(k, dr, xs, hbf, ycat, ident_f, ones_b, M_, A_, norm_mod, ffn, LATB)
    write_out()


def layer0_mixer(k, dr, xs, hbf, ycat, ident_f, ones_b, M_, project_out, qk_norm_rope, attend, ALLB):
    import os
    SUB = int(os.environ.get("SUBSTAGE", "99"))
    dm = k.dram
    SW = "pool"
    win = dr["ab_w_in"].rearrange("(kc p) n -> p kc n", p=128)
    cosT = k.tile([64, T], F32, "cosT")
    sinT = k.tile([64, T], F32, "sinT")
    Rm = k.tile([64, 64], BF16, "Rm")
    gq = k.tile([64, 2], F32, "gqk")
    k.dma("sp", cosT.all, dm(dr["rope_g_cos"]))
    k.dma("sp", sinT.all, dm(dr["rope_g_sin"]))
    k.dma(SW, Rm.all, dm(dr["rope_g_R"]))
    k.dma("sp", gq.all, dm(dr["gqa_norms"]))
    wqkv = k.tile([128, 8, 768], BF16, "wqkv")
    k.dma(SW, wqkv.all, dm(win[:, :, 1024:1792]))
    kh = [k.tile([64, T], BF16, f"kh{i}") for i in range(2)]
    vaug = [k.tile([128, 18, 192], BF16, f"vaug{i}") for i in range(2)]
    tl = (k.tile([64, 512], BF16, "n_sq"), k.tile([64, 512], F32, "n_rstd"), k.tile([64, 512], F32, "n_qn"),
          k.tile([64, 512], F32, "n_t1"))
    onesv = ones_b[0:64, 0:64]
    bi = 0
    for h in range(2):
        for (c0, ncl, s) in ALLB:
            b = 4 + bi % 2
            bi += 1
            for kk in range(8):
                k.mm(k.ps(b, rows=64, c1=ncl), wqkv[:, kk, 512 + h * 64:512 + (h + 1) * 64], hbf[:, kk, c0:c0 + ncl],
                     start=(kk == 0), stop=(kk == 7))
            qk_norm_rope(k.ps(b, rows=64, c1=ncl), 64, gq[:, 1:2], onesv, cosT, sinT, Rm.all,
                         kh[h][0:64, c0:c0 + ncl], c0, ncl, tl, bi, (6, 7))
    if SUB == 1:
        return
    BIS = os.environ.get("BIS", "")
    for h in range(2):
        if "m" not in BIS:
            k.memset("pool", vaug[h].all, 1.0)
    for tc in range(18 if "t" not in BIS else 1):
        b = 4 + tc % 2
        if "x" not in BIS:
            for kk in range(8):
                k.mm(k.ps(b, c1=128), hbf[:, kk, tc * 128:(tc + 1) * 128], wqkv[:, kk, 640:768],
                     start=(kk == 0), stop=(kk == 7))
        if "c" not in BIS:
            for h in range(2):
                ce = "dve"
                if "d" in BIS:
                    ce = "dve"
                if "a" in BIS:
                    ce = "act"
                k.copy(ce, vaug[h][:, tc, 64:128], k.ps(b, c0=h * 64, c1=(h + 1) * 64))
    if SUB == 2:
        return
    qhs = [k.tile([64, T], BF16, f"qh{i}") for i in range(2)]
    etl = [k.tile([128, 512], BF16, f"e{i}") for i in range(3)]
    rdt = [(k.tile([128, 512], F32, f"rd{i}"), k.tile([64, 512], F32, f"rs{i}")) for i in range(1)]
    scale = 64 ** -0.5
    for h in range(8 if SUB != 3 else 1):
        qh = qhs[h % 2]
        for (c0, ncl, s) in ALLB:
            b = 4 + bi % 2
            bi += 1
            for kk in range(8):
                k.mm(k.ps(b, rows=64, c1=ncl), wqkv[:, kk, h * 64:(h + 1) * 64], hbf[:, kk, c0:c0 + ncl],
                     start=(kk == 0), stop=(kk == 7))
            qk_norm_rope(k.ps(b, rows=64, c1=ncl), 64, gq[:, 0:1], onesv, cosT, sinT, Rm.all,
                         qh[0:64, c0:c0 + ncl], c0, ncl, tl, bi, (6, 7))
        kv = h // 4
        attend(qh, kh[kv], vaug[kv], 64, [(0, NCTX)], 2, scale, ycat, (h % 2) * 64, h // 2, etl, rdt)
        attend(qh, kh[kv], vaug[kv], 64, [(NCTX + 512 * i, 512) for i in range(4)], 18, scale, ycat,
               (h % 2) * 64, h // 2, etl, rdt)
    k.free(cosT, sinT, Rm, gq, wqkv, *kh, *vaug, *tl, *qhs, *etl, *[t for p in rdt for t in p])
    if SUB == 3:
        return
    project_out(0, ycat, dr["ab_w_out"], 512, ALLB)
    if SUB == 4:
        return

    cw = k.tile([128, 4, 4], F32, "convw")
    cb = k.tile([128, 4], F32, "convb")
    lb = k.tile([128, 2, 2, 4], F32, "lru_b")
    lam = k.tile([128, 2, 4], F32, "lam")
    cneg = k.tile([128, 2, 4], F32, "cneg")
    k.dma("sp", cw.all, dm(dr["lru_convw_fm"]))
    k.dma("sp", cb.all, dm(dr["lru_convb_fm"]))
    k.dma("sp", lb.all, dm(dr["lru_b_fm"]))
    k.dma("sp", lam.all, dm(dr["lru_lam_fm"]))
    lamf = V(lam.ap.rearrange("p a b -> p (a b)"), lam.all.pg)
    cnegf = V(cneg.ap.rearrange("p a b -> p (a b)"), cneg.all.pg)
    k.act(cnegf, lamf, AF.Exp, scale=-1.0)
    k.act(cnegf, cnegf, AF.Ln, bias=1.0)
    k.ts("dve", cnegf, cnegf, -8.0, None, ALU.mult)
    xr = k.tile([128, T], F32, "xr")
    u = k.tile([128, T], F32, "u")
    ubf = k.tile([128, T], BF16, "ubf")
    rec = k.tile([128, T], F32, "rec")
    at = k.tile([128, T], F32, "a")
    bt = k.tile([128, T], F32, "b")
    ht = k.tile([128, T], F32, "h")
    wx = k.tile([128, 8, 256], BF16, "wxg")
    bd = [k.tile([128, 128], BF16, f"bd{i}") for i in range(4)]
    tmp = [k.tile([128, 512], F32, f"lt{i}") for i in range(4)]
    seqs = [(0, NCTX), (NCTX, NLAT)]
    for j in range(4):
        k.dma(SW, wx[:, :, 0:128], dm(k_win_slice(dr, j * 128)))
        k.dma(SW, wx[:, :, 128:256], dm(k_win_slice(dr, 512 + j * 128)))
        for d_ in range(2):
            for gi_, nm in enumerate(("lru_w_a", "lru_w_x")):
                t_ = bd[d_ * 2 + gi_]
                k.memset("pool", t_.all, 0.0)
                for hb_ in range(2):
                    k.dma(SW, t_[hb_ * 64:(hb_ + 1) * 64, hb_ * 64:(hb_ + 1) * 64], dm(dr[nm][d_, 2 * j + hb_]))
        for bi_, (c0, ncl, s) in enumerate(ALLB):
            b = 4 + bi_ % 2
            for kk in range(8):
                k.mm(k.ps(b, c1=ncl), wx[:, kk, 0:128], hbf[:, kk, c0:c0 + ncl], start=(kk == 0), stop=(kk == 7))
            k.copy("act", xr[:, c0:c0 + ncl], k.ps(b, c1=ncl))
        k.ts("dve", u.all, xr.all, cw[:, j, 2:3], cb[:, j:j + 1], ALU.mult, ALU.add)
        for tap in (0, 1, 3):
            sh = tap - 2
            for (s0, L_) in seqs:
                lo = s0 + max(0, -sh)
                hi = s0 + L_ - max(0, sh)
                k.stt("dve", u[:, lo:hi], xr[:, lo + sh:hi + sh], cw[:, j, tap:tap + 1], u[:, lo:hi], ALU.mult, ALU.add)
        k.copy("pool", ubf.all, u.all)
        for d_ in range(2):
            for bi_, (c0, ncl, s) in enumerate(ALLB):
                ba, bx = 4 + bi_ % 2, 6 + bi_ % 2
                k.mm(k.ps(ba, c1=ncl), bd[d_ * 2].all, ubf[:, c0:c0 + ncl])
                k.mm(k.ps(bx, c1=ncl), bd[d_ * 2 + 1].all, ubf[:, c0:c0 + ncl])
                r_, i_, q_, _ = tmp
                k.act(r_[:, 0:ncl], k.ps(ba, c1=ncl), AF.Sigmoid, bias=lb[:, 0, d_, j:j + 1])
                k.act(i_[:, 0:ncl], k.ps(bx, c1=ncl), AF.Sigmoid, bias=lb[:, 1, d_, j:j + 1])
                k.act(at[:, c0:c0 + ncl], r_[:, 0:ncl], AF.Exp, scale=cneg[:, d_, j:j + 1])
                k.tt("pool", q_[:, 0:ncl], at[:, c0:c0 + ncl], at[:, c0:c0 + ncl], ALU.mult)
                k.act(q_[:, 0:ncl], q_[:, 0:ncl], AF.Sqrt, bias=1.0, scale=-1.0)
                k.tt("dve", i_[:, 0:ncl], i_[:, 0:ncl], u[:, c0:c0 + ncl], ALU.mult)
                k.tt("dve", bt[:, c0:c0 + ncl], q_[:, 0:ncl], i_[:, 0:ncl], ALU.mult)
            if d_ == 0:
                k.scan(rec.all, at.all, bt.all, 0.0)
            else:
                rv = lambda v: v.m(lambda ap: ap[:, ::-1])
                k.scan(rv(ht[:, 0:NCTX]), rv(at[:, 0:NCTX]), rv(bt[:, 0:NCTX]), 0.0)
                k.scan(rv(ht[:, NCTX:T]), rv(at[:, NCTX:T]), rv(bt[:, NCTX:T]), ht[:, 0:1])
                k.tt("pool", rec.all, rec.all, ht.all, ALU.add)
        for bi_, (c0, ncl, s) in enumerate(ALLB):
            b = 4 + bi_ % 2
            for kk in range(8):
                k.mm(k.ps(b, c1=ncl), wx[:, kk, 128:256], hbf[:, kk, c0:c0 + ncl], start=(kk == 0), stop=(kk == 7))
            g_, t_, w_, _ = tmp
            k.copy("act", g_[:, 0:ncl], k.ps(b, c1=ncl))
            k.tt("pool", t_[:, 0:ncl], g_[:, 0:ncl], g_[:, 0:ncl], ALU.mult)
            k.ts("dve", t_[:, 0:ncl], t_[:, 0:ncl], 0.044715, 1.0, ALU.mult, ALU.add)
            k.tt("dve", t_[:, 0:ncl], t_[:, 0:ncl], g_[:, 0:ncl], ALU.mult)
            k.act(t_[:, 0:ncl], t_[:, 0:ncl], AF.Sigmoid, scale=2.0 * math.sqrt(2.0 / math.pi))
            k.tt("pool", t_[:, 0:ncl], t_[:, 0:ncl], g_[:, 0:ncl], ALU.mult)
            k.tt("dve", ycat[:, j, c0:c0 + ncl], t_[:, 0:ncl], rec[:, c0:c0 + ncl], ALU.mult)
    k.free(cw, cb, lb, lam, cneg, xr, u, ubf, rec, at, bt, ht, wx, *bd, *tmp)
    project_out(0, ycat, dr["ab_w_out"], 0, ALLB)


def k_win_slice(dr, c0):
    return dr["ab_w_in"].rearrange("(kc p) n -> p kc n", p=128)[:, :, c0:c0 + 128]


_CONSTS = None
_NC_CACHE = {}


def prep_inputs(inp, b):
    global _CONSTS
    if _CONSTS is None:
        _CONSTS = host_consts()
    f = lambda a: np.ascontiguousarray(np.asarray(a, dtype=np.float32))
    m = dict(_CONSTS)
    m.pop("rope_g_R"); m.pop("rope_m_R")
    m["rope_g_R"] = _CONSTS["rope_g_R"]; m["rope_m_R"] = _CONSTS["rope_m_R"]
    m["x"] = f(inp["x"][b])
    m["ctx"] = f(inp["ctx"][b])
    m["c_fm"] = f(fm(inp["c"][b]))
    m["cctx_fm"] = f(fm(inp["c_ctx"]))
    m["mod_w"] = f(inp["mod_w"])
    m["modb"] = f(fm(inp["mod_b"]).transpose(1, 0, 2))
    g = np.stack([fm(inp["norm1_g"]), fm(inp["norm2_g"])], axis=1)
    g = np.repeat(g[..., None], 2, axis=-1).reshape(2, 2, 128, 16)
    m["gdup"] = f(g.transpose(2, 0, 1, 3))
    m["ab_w_in"] = f(inp["ab_w_in"][0])
    m["ab_w_out"] = f(inp["ab_w_out"][0])
    m["gqa_norms"] = f(np.stack([inp["gqa_q_norm"][0], inp["gqa_k_norm"][0]], axis=1))
    m["lru_convw_fm"] = f(fm(inp["lru_conv_w"][0]).transpose(1, 2, 0))
    m["lru_convb_fm"] = f(fm(inp["lru_conv_b"][0]))
    lb = np.stack([fm(inp["lru_b_a"][0].reshape(2, 512)), fm(inp["lru_b_x"][0].reshape(2, 512))], axis=0)
    m["lru_b_fm"] = f(lb.transpose(2, 0, 1, 3))
    m["lru_lam_fm"] = f(fm(inp["lru_lambda"][0]).transpose(1, 0, 2))
    m["lru_w_a"] = f(inp["lru_w_a"][0])
    m["lru_w_x"] = f(inp["lru_w_x"][0])
    m["ffn_w1"] = f(inp["ffn_w1"][0])
    m["ffn_w3"] = f(inp["ffn_w3"][0])
    m["ffn_w2"] = f(inp["ffn_w2"][0])
    m["cd_w_in"] = f(inp["cd_w_in"][0])
    m["cd_w_out"] = f(inp["cd_w_out"][0])
    mg = np.zeros((128, 5), np.float32)
    mg[:, 0:2] = fm(inp["mla_q_a_norm"][0])
    mg[:, 2] = inp["mla_kv_a_norm"][0]
    mg[0:96, 3] = inp["mla_q_norm"][0]
    mg[0:96, 4] = inp["mla_k_norm"][0]
    m["mla_gains"] = mg
    m["mla_q_b"] = f(inp["mla_q_b"][0])
    m["mla_kv_b"] = f(inp["mla_kv_b"][0])
    m["hy_p"] = f(np.stack([inp["hy_filt_b1"][0], inp["hy_filt_b2"][0], inp["hy_sin_freq"][0]], axis=1))
    m["hy_filt_w1"] = f(inp["hy_filt_w1"][0])
    m["hy_filt_w2"] = f(inp["hy_filt_w2"][0])
    m["hy_filt_w3"] = f(inp["hy_filt_w3"][0])
    m["hy_conv_w"] = f(inp["hy_conv_w"][0])
    m["hy_conv_b"] = f(inp["hy_conv_b"][0])
    m["hy_skip"] = f(inp["hy_skip"][0])
    m["moe_router"] = f(inp["moe_router"][0])
    m["moe_w1"] = f(inp["moe_w1"][0])
    m["moe_w3"] = f(inp["moe_w3"][0])
    m["moe_w2"] = f(inp["moe_w2"][0])
    return m


def shapes_of(m):
    return {k_: (tuple(v.shape), str(v.dtype)) for k_, v in m.items()}


def kernel(**inputs):
    maps = [prep_inputs(inputs, b) for b in range(8)]
    shapes = shapes_of(maps[0])
    key = tuple(sorted(shapes.items()))
    if key not in _NC_CACHE:
        _NC_CACHE[key] = build(shapes)
    nc = _NC_CACHE[key]
    res = run_bass_kernel_spmd(nc, maps, core_ids=list(range(8)))
    return np.stack([np.asarray(r["out"], dtype=np.float32) for r in res.results], axis=0)


def ps_bf(k, i, rows, c0, c1):
    b = k.banks[i]
    return V(b[0:rows, :].bitcast(BF16)[:, c0:c1], [(PSUM_PAGE0 + i, PSUM_PAGE0 + i + 1)])


def layer1_mixer(k, dr, xs, hbf, ycat, ident_b, ones_b, M_, project_out, qk_norm_rope, attend, ALLB, LATB, SUB):
    dm = k.dram
    SW = "pool"
    win = dr["cd_w_in"].rearrange("(kc p) n -> p kc n", p=128)
    cosT = k.tile([96, T], F32, "cosM")
    sinT = k.tile([96, T], F32, "sinM")
    Rm = k.tile([96, 96], BF16, "RmM")
    gn = k.tile([128, 5], F32, "mla_g")
    k.dma("sp", cosT.all, dm(dr["rope_m_cos"]))
    k.dma("sp", sinT.all, dm(dr["rope_m_sin"]))
    k.dma(SW, Rm.all, dm(dr["rope_m_R"]))
    k.dma("sp", gn.all, dm(dr["mla_gains"]))
    wqa = k.tile([128, 8, 256], BF16, "wqa")
    wkva = k.tile([128, 8, 128], BF16, "wkva")
    wrp = k.tile([128, 8, 96], BF16, "wrope_pad")
    qb = k.tile([128, 2, 768], BF16, "q_b")
    kvbn = k.tile([128, 8, 96], BF16, "kvb_nope_pad")
    kvbv = k.tile([128, 8, 64], BF16, "kvb_v")
    k.dma(SW, wqa.all, dm(win[:, :, 1536:1792]))
    k.dma(SW, wkva.all, dm(win[:, :, 1792:1920]))
    k.memset("dve", wrp.all, 0.0)
    k.dma(SW, wrp[:, :, 64:96], dm(win[:, :, 1920:1952]))
    k.dma(SW, qb.all, dm(dr["mla_q_b"].rearrange("(kc p) n -> p kc n", p=128)))
    kvb3 = dr["mla_kv_b"].rearrange("k (h c) -> k h c", h=8)
    k.memset("dve", kvbn.all, 0.0)
    k.dma(SW, kvbn[:, :, 0:64], dm(kvb3[:, :, 0:64]))
    k.dma(SW, kvbv.all, dm(kvb3[:, :, 64:128]))
    qa = k.tile([128, 2, NLAT], BF16, "qa")
    kva = k.tile([128, T], BF16, "kva")
    tl = (k.tile([128, 512], BF16, "n_sq"), k.tile([128, 512], F32, "n_rstd"), k.tile([128, 512], F32, "n_qn"),
          k.tile([128, 512], F32, "n_t1"))
    sq2 = k.tile([128, 2, 512], BF16, "sq2")
    raw = k.tile([128, 2, 512], F32, "qa_raw")
    for bi, (c0, ncl, s) in enumerate(LATB):
        for m in range(2):
            b = 4 + m
            for kk in range(8):
                k.mm(k.ps(b, c1=ncl), wqa[:, kk, m * 128:(m + 1) * 128], hbf[:, kk, c0:c0 + ncl],
                     start=(kk == 0), stop=(kk == 7))
            k.act(sq2[:, m, 0:ncl], k.ps(b, c1=ncl), AF.Square)
            k.copy("act", raw[:, m, 0:ncl], k.ps(b, c1=ncl))
        for m in range(2):
            k.mm(k.ps(6, c1=ncl), ones_b.all, sq2[:, m, 0:ncl], start=(m == 0), stop=(m == 1))
        rstd = tl[1]
        k.act(rstd[:, 0:ncl], k.ps(6, c1=ncl), AF.Sqrt, bias=EPS, scale=1.0 / 256)
        k.recip(rstd[:, 0:ncl], rstd[:, 0:ncl])
        for m in range(2):
            k.stt("dve", qa[:, m, c0 - NCTX:c0 - NCTX + ncl], raw[:, m, 0:ncl], gn[:, m:m + 1], rstd[:, 0:ncl],
                  ALU.mult, ALU.mult)
    for bi, (c0, ncl, s) in enumerate(ALLB):
        b = 4 + bi % 2
        for kk in range(8):
            k.mm(k.ps(b, c1=ncl), wkva[:, kk, :], hbf[:, kk, c0:c0 + ncl], start=(kk == 0), stop=(kk == 7))
        sq, rstd = tl[0], tl[1]
        k.act(sq[:, 0:ncl], k.ps(b, c1=ncl), AF.Square)
        k.mm(k.ps(6, c1=ncl), ones_b.all, sq[:, 0:ncl])
        k.act(rstd[:, 0:ncl], k.ps(6, c1=ncl), AF.Sqrt, bias=EPS, scale=1.0 / 128)
        k.recip(rstd[:, 0:ncl], rstd[:, 0:ncl])
        k.stt("dve", kva[:, c0:c0 + ncl], k.ps(b, c1=ncl), gn[:, 2:3], rstd[:, 0:ncl], ALU.mult, ALU.mult)
    k.free(sq2, raw)
    kh = k.tile([96, T], BF16, "kh")
    qh = k.tile([96, T], BF16, "qh")
    vaug = k.tile([128, 18, 192], BF16, "vaug")
    etl = [k.tile([128, 512], BF16, f"e{i}") for i in range(3)]
    rdt = [(k.tile([128, 512], F32, "rd"), k.tile([64, 512], F32, "rs"))]
    k.memset("pool", vaug.all, 1.0)
    onesv = ones_b[0:96, 0:96]
    scale = 96 ** -0.5
    bi = 0
    for h in range(8):
        for (c0, ncl, s) in ALLB:
            b = 4 + bi % 2
            bi += 1
            for kk in range(8):
                k.mm(k.ps(b, rows=96, c1=ncl), wrp[:, kk, :], hbf[:, kk, c0:c0 + ncl], start=(kk == 0), stop=False)
            k.mm(k.ps(b, rows=96, c1=ncl), kvbn[:, h, :], kva[:, c0:c0 + ncl], start=False, stop=True)
            qk_norm_rope(k.ps(b, rows=96, c1=ncl), 96, gn[0:96, 4:5], onesv, cosT, sinT, Rm.all,
                         kh[0:96, c0:c0 + ncl], c0, ncl, tl, bi, (6, 7))
        for (c0, ncl, s) in LATB:
            b = 4 + bi % 2
            bi += 1
            for m in range(2):
                k.mm(k.ps(b, rows=96, c1=ncl), qb[:, m, h * 96:(h + 1) * 96], qa[:, m, c0 - NCTX:c0 - NCTX + ncl],
                     start=(m == 0), stop=(m == 1))
            qk_norm_rope(k.ps(b, rows=96, c1=ncl), 96, gn[0:96, 3:4], onesv, cosT, sinT, Rm.all,
                         qh[0:96, c0:c0 + ncl], c0, ncl, tl, bi, (6, 7))
        for tc in range(18):
            b = 4 + tc % 2
            k.mm(k.ps(b, c1=64), kva[:, tc * 128:(tc + 1) * 128], kvbv[:, h, :])
            k.copy("dve", vaug[:, tc, 64:128], k.ps(b, c1=64))
        attend(qh, kh, vaug, 96, [(NCTX + 512 * i, 512) for i in range(4)], 18, scale, ycat,
               (h % 2) * 64, h // 2, etl, rdt)
    k.free(cosT, sinT, Rm, gn, wqa, wkva, wrp, qb, kvbn, kvbv, qa, kva, *tl, kh, qh, vaug, *etl, *rdt[0])
    project_out(1, ycat, dr["cd_w_out"], 512, LATB)
    if SUB == 1:
        return

    hp = k.tile([64, 3], F32, "hy_p")
    k.dma("sp", hp.all, dm(dr["hy_p"]))
    zT = k.tile([33, NLAT], BF16, "zT")
    k.dma(SW, zT.all, dm(dr["hy_zT"]))
    fw1 = k.tile([33, 64], BF16, "fw1")
    fw2 = k.tile([64, 64], BF16, "fw2")
    k.dma(SW, fw1.all, dm(dr["hy_filt_w1"]))
    k.dma(SW, fw2.all, dm(dr["hy_filt_w2"]))
    h1 = k.tile([64, NLAT], BF16, "h1T")
    h2 = k.tile([64, NLAT], BF16, "h2T")
    ft = [k.tile([64, 512], F32, f"ft{i}") for i in range(2)]
    MAGIC = 12582912.0
    TWO_PI = 2.0 * math.pi

    def sin_layer(dst, lhsT, src, bcol):
        for i in range(4):
            cs = slice(i * 512, (i + 1) * 512)
            b = 4 + i % 2
            k.mm(k.ps(b, rows=64), lhsT, src[:, cs])
            y, t_ = ft
            k.ts("dve", y.all, k.ps(b, rows=64), hp[:, bcol:bcol + 1], hp[:, 2:3], ALU.add, ALU.mult)
            k.ts("dve", t_.all, y.all, 1.0 / TWO_PI, MAGIC, ALU.mult, ALU.add)
            k.ts("dve", t_.all, t_.all, -MAGIC, -TWO_PI, ALU.add, ALU.mult)
            k.tt("dve", y.all, y.all, t_.all, ALU.add)
            k.act(dst[:, cs], y.all, AF.Sin)

    sin_layer(h1, fw1.all, zT, 0)
    sin_layer(h2, fw2.all, h1, 1)
    k.free(zT, fw1, fw2, h1, *ft, hp)

    CH = 256
    vt = k.tile([128, 16, 512], BF16, "hy_v")
    x1 = k.tile([128, 16, 512], BF16, "hy_x1")
    x2 = k.tile([128, 16, 512], BF16, "hy_x2")
    hpadL = k.tile([128, 8, 129], BF16, "hpadL")
    hpadR = k.tile([128, 8, 129], BF16, "hpadR")
    k.memset("dve", hpadL.all, 0.0)
    k.memset("dve", hpadR.all, 0.0)
    k.copy("pool", hpadL[:, :, 1:129], hbf[:, :, NCTX:NCTX + 128])
    k.copy("pool", hpadR[:, :, 0:127], hbf[:, :, T - 127:T])
    wf = k.tile([128, 8, CH], BF16, "hy_wf")
    wj = [k.tile([128, 8, CH], BF16, f"hy_wj{j}") for j in range(3)]
    cwb = k.tile([128, 3, CH], F32, "hy_cwb")
    bb = k.tile([128, CH], F32, "hy_bias")
    convw = dr["hy_conv_w"]
    for half in range(2):
        hs = slice(half * CH, (half + 1) * CH)
        for part, dst in enumerate((vt, x1, x2)):
            cbase = part * 512 + half * CH
            k.dma(SW, wf.all, dm(win[:, :, cbase:cbase + CH]))
            k.dma("sp", cwb.all, dm(convw[:, cbase:cbase + CH].partition_broadcast(128)))
            k.dma("sp", bb.all, dm(dr["hy_conv_b"][cbase:cbase + CH].partition_broadcast(128)))
            for j in range(3):
                for kk in range(8):
                    k.tt("dve" if kk % 2 else "pool", wj[j][:, kk, :], wf[:, kk, :], cwb[:, j, :], ALU.mult)
            for tcn in range(16):
                b = 4 + tcn % 2
                n_mm = 0
                for j in range(3):
                    for kk in range(8):
                        sh = j - 1
                        if tcn == 0 and sh == -1:
                            lhs = hpadL[:, kk, 0:128]
                        elif tcn == 15 and sh == 1:
                            lhs = hpadR[:, kk, 0:128]
                        else:
                            c0 = NCTX + tcn * 128 + sh
                            lhs = hbf[:, kk, c0:c0 + 128]
                        k.mm(k.ps(b, c1=CH), lhs, wj[j][:, kk, :], start=(n_mm == 0), stop=(n_mm == 23))
                        n_mm += 1
                k.tt("dve", dst[:, tcn, hs], k.ps(b, c1=CH), bb.all, ALU.add)
    k.free(hpadL, hpadR, wf, *wj, cwb, bb)
    carve = [hbf.off]

    def ctile(shape, dt, name):
        t_ = Tile(k, carve[0], shape, dt, name)
        carve[0] += ((t_.nbytes + PAGE - 1) // PAGE) * PAGE
        assert carve[0] <= hbf.off + hbf.alloc
        return t_

    hsum = ctile([128, 16, CH], BF16, "hsum")
    hdif = ctile([128, 16, CH], BF16, "hdif")
    Yre = ctile([128, 16, CH], BF16, "Yre")
    Yim = ctile([128, 16, CH], BF16, "Yim")
    tmps = [ctile([128, 512], F32, "hyt0"), ctile([128, 512], F32, "hyt1"),
            k.tile([128, 512], F32, "hyt2"), k.tile([128, 512], F32, "hyt3")]
    dftA = [k.tile([128, 16, 128], BF16, f"dftA{i}") for i in range(2)]
    dftB = [k.tile([128, 16, 128], BF16, f"dftB{i}") for i in range(2)]
    w3 = k.tile([64, 512], BF16, "fw3")
    skb = k.tile([128, CH], F32, "hy_skipb")
    winr = [k.tile([128, CH], F32, f"hywin{i}") for i in range(2)]
    for half in range(2):
        hs = slice(half * CH, (half + 1) * CH)
        for o in range(2):
            xo = x1 if o == 0 else x2
            for dr_ in range(2):
                cb_ = o * 1024 + dr_ * 512 + half * CH
                k.dma(SW, w3[:, dr_ * CH:(dr_ + 1) * CH], dm(dr["hy_filt_w3"][:, cb_:cb_ + CH]))
            k.dma("sp", skb.all, dm(dr["hy_skip"][o, half * CH:(half + 1) * CH].partition_broadcast(128)))
            for tcn in range(16):
                b = 4 + tcn % 2
                wn = winr[tcn % 2]
                k.dma("sp", wn.all, dm(dr["hy_window"][tcn * 128:(tcn + 1) * 128, half * CH:(half + 1) * CH]))
                k.mm(k.ps(b), h2[0:64, tcn * 128:(tcn + 1) * 128], w3.all)
                hf_, hb_ = tmps[0], tmps[1]
                k.tt("dve", hf_[:, 0:CH], k.ps(b, c0=0, c1=CH), wn.all, ALU.mult)
                k.tt("dve", hb_[:, 0:CH], k.ps(b, c0=CH, c1=2 * CH), wn.all, ALU.mult)
                k.tt("pool", hsum[:, tcn, :], hf_[:, 0:CH], hb_[:, 0:CH], ALU.add)
                k.tt("pool", hdif[:, tcn, :], hb_[:, 0:CH], hf_[:, 0:CH], ALU.subtract)
                if tcn == 0:
                    k.copy("pool", hsum[0:1, 0, :], hf_[0:1, 0:CH])
            for fc in range(16):
                ca, sa = dftA[fc % 2], dftB[fc % 2]
                k.dma("sp", V(ca.ap.rearrange("p a b -> p (a b)"), ca.all.pg), dm(dr["dft_cf"][fc]))
                k.dma("sp", V(sa.ap.rearrange("p a b -> p (a b)"), sa.all.pg), dm(dr["dft_sf"][fc]))
                bx, by = 4 + (fc % 2) * 2, 5 + (fc % 2) * 2
                for tcn in range(16):
                    k.mm(k.ps(bx, c0=0, c1=CH), ca[:, tcn, :], vt[:, tcn, hs], start=(tcn == 0), stop=(tcn == 15))
                for tcn in range(16):
                    k.mm(k.ps(bx, c0=CH, c1=2 * CH), sa[:, tcn, :], vt[:, tcn, hs], start=(tcn == 0), stop=(tcn == 15))
                for tcn in range(16):
                    k.mm(k.ps(by, c0=0, c1=CH), ca[:, tcn, :], hsum[:, tcn, :], start=(tcn == 0), stop=(tcn == 15))
                for tcn in range(16):
                    k.mm(k.ps(by, c0=CH, c1=2 * CH), sa[:, tcn, :], hdif[:, tcn, :], start=(tcn == 0), stop=(tcn == 15))
                kt, t1, t2 = tmps[2], tmps[0], tmps[1]
                k.copy("act", kt.all, k.ps(by))
                A_ = k.ps(bx, c0=0, c1=CH)
                B_ = k.ps(bx, c0=CH, c1=2 * CH)
                k.tt("dve", t1[:, 0:CH], A_, kt[:, 0:CH], ALU.mult)
                k.tt("dve", t1[:, CH:2 * CH], B_, kt[:, CH:2 * CH], ALU.mult)
                k.tt("pool", Yre[:, fc, :], t1[:, 0:CH], t1[:, CH:2 * CH], ALU.add)
                k.tt("dve", t2[:, 0:CH], A_, kt[:, CH:2 * CH], ALU.mult)
                k.tt("dve", t2[:, CH:2 * CH], B_, kt[:, 0:CH], ALU.mult)
                k.tt("pool", Yim[:, fc, :], t2[:, 0:CH], t2[:, CH:2 * CH], ALU.subtract)
            for tcn in range(16):
                ca, sa = dftA[tcn % 2], dftB[tcn % 2]
                k.dma("sp", V(ca.ap.rearrange("p a b -> p (a b)"), ca.all.pg), dm(dr["dft_ci"][tcn]))
                k.dma("sp", V(sa.ap.rearrange("p a b -> p (a b)"), sa.all.pg), dm(dr["dft_si"][tcn]))
                b = 4 + tcn % 2
                for fc in range(16):
                    k.mm(k.ps(b, c1=CH), ca[:, fc, :], Yre[:, fc, :], start=(fc == 0), stop=False)
                for fc in range(16):
                    k.mm(k.ps(b, c1=CH), sa[:, fc, :], Yim[:, fc, :], start=False, stop=(fc == 15))
                t1 = tmps[3]
                k.tt("pool", t1[:, 0:CH], vt[:, tcn, hs], skb.all, ALU.mult)
                k.tt("dve", t1[:, 0:CH], k.ps(b, c1=CH), t1[:, 0:CH], ALU.add)
                k.tt("dve", vt[:, tcn, hs], t1[:, 0:CH], xo[:, tcn, hs], ALU.mult)
    for tcn in range(16):
        b = 4 + tcn % 2
        for cc in range(4):
            k.transpose(ps_bf(k, b, 128, cc * 128, (cc + 1) * 128), vt[:, tcn, cc * 128:(cc + 1) * 128], ident_b.all)
        for cc in range(4):
            k.copy("dve", ycat[:, cc, NCTX + tcn * 128:NCTX + (tcn + 1) * 128],
                   ps_bf(k, b, 128, cc * 128, (cc + 1) * 128))
    k.free(vt, x1, x2, *dftA, *dftB, w3, skb, tmps[2], tmps[3], *winr, h2)
    project_out(1, ycat, dr["cd_w_out"], 0, LATB)


def moe_layer(k, dr, xs, hbf, ycat, ident_f, ones_b, M_, A_, norm_mod, ffn, LATB):
    dm = k.dram
    AX = mybir.AxisListType.X
    rw = k.tile([128, 8, 8], F32, "rw")
    r_hi = k.tile([128, 8, 8], BF16, "r_hi")
    r_lo = k.tile([128, 8, 8], BF16, "r_lo")
    k.dma("sp", rw.all, dm(dr["moe_router"].rearrange("(kc p) e -> p kc e", p=128)))
    k.copy("dve", r_hi.all, rw.all)
    k.tt("dve", r_lo.all, rw.all, r_hi.all, ALU.subtract)
    logits = k.tile([128, 16, 8], F32, "logits")
    gates = k.tile([128, 16, 8], F32, "gates")
    hlo = k.tile([128, 8, 512], BF16, "hlo")
    hf32 = [k.tile([128, 512], F32, f"hf32_{i}") for i in range(2)]

    def hook(bi, c0, ncl, kk, tmp, s):
        hf = hf32[kk % 2]
        k.act(hf[:, 0:ncl], tmp[:, 0:ncl], AF.Identity, bias=M_(1, 3, kk, s), scale=A_(1, 1, kk, s))
        k.tt("pool", hlo[:, kk, 0:ncl], hf[:, 0:ncl], hbf[:, kk, c0:c0 + ncl], ALU.subtract)
        if kk == 7:
            for q in range(ncl // 128):
                tcn = (c0 - NCTX) // 128 + q
                b = 6 + tcn % 2
                n = 0
                for which in range(3):
                    for k2 in range(8):
                        if which == 1:
                            lhs = hlo[:, k2, q * 128:(q + 1) * 128]
                        else:
                            lhs = hbf[:, k2, c0 + q * 128:c0 + (q + 1) * 128]
                        rr = r_lo if which == 2 else r_hi
                        k.mm(k.ps(b, c1=8), lhs, rr[:, k2, :], start=(n == 0), stop=(n == 23))
                        n += 1
                k.copy("dve", logits[:, tcn, :], k.ps(b, c1=8))

    norm_mod(1, 1, LATB, f32_hook=hook)
    k.free(hlo, *hf32, rw, r_hi, r_lo)
    mx = k.tile([128, 8], F32, "mx")
    sel = k.tile([128, 8], F32, "sel")
    ex = k.tile([128, 8], F32, "ex")
    sm = k.tile([128, 4], F32, "sm")
    for tcn in range(16):
        lg = logits[:, tcn, :]
        k.op("dve", (lambda o_, i_: (lambda e_: e_.max(out=o_, in_=i_)))(mx.ap, lg.ap), R=[lg], W=[mx.all])
        k.ts("dve", sel.all, lg, mx[:, 1:2], None, ALU.is_ge)
        k.ts("dve", sm[:, 0:1], mx[:, 0:1], -1.0, None, ALU.mult)
        k.act(ex.all, lg, AF.Exp, bias=sm[:, 0:1])
        k.tt("dve", ex.all, ex.all, sel.all, ALU.mult)
        k.op("dve", (lambda o_, i_: (lambda e_: e_.reduce_sum(out=o_, in_=i_, axis=AX)))(sm[:, 1:2].ap, ex.ap),
             R=[ex.all], W=[sm[:, 1:2]])
        k.recip(sm[:, 2:3], sm[:, 1:2])
        k.ts("dve", gates[:, tcn, :], ex.all, sm[:, 2:3], None, ALU.mult)
    k.free(mx, sel, ex, sm)
    Gbs = [k.tile([128, NLAT], F32, f"Gb{i}") for i in range(2)]
    Dt = k.tile([128, 128], F32, "Dt")
    Dhi = k.tile([128, 128], BF16, "Dhi")
    Dlo = k.tile([128, 128], BF16, "Dlo")
    for e in range(8):
        Gb = Gbs[e % 2]
        for tcn in range(16):
            b = 6 + tcn % 2
            k.ts("dve", Dt.all, ident_f.all, gates[:, tcn, e:e + 1], None, ALU.mult)
            k.copy("dve", Dhi.all, Dt.all)
            k.tt("dve", Dlo.all, Dt.all, Dhi.all, ALU.subtract)
            k.mm(k.ps(b, c1=128), ones_b.all, Dhi.all, start=True, stop=False)
            k.mm(k.ps(b, c1=128), ones_b.all, Dlo.all, start=False, stop=True)
            k.copy("act", Gb[:, tcn * 128:(tcn + 1) * 128], k.ps(b, c1=128))
        ffn(1, dr["moe_w1"][e], dr["moe_w3"][e], dr["moe_w2"][e], 3584, LATB, ycat, gate_bc=Gb)
    k.free(*Gbs, Dt, Dhi, Dlo, logits, gates)
```

```python
import contextlib
import math
import numpy as np
import ml_dtypes
import concourse.bass as bass
import concourse.mybir as mybir
from concourse.bass_utils import run_bass_kernel_spmd

F32 = mybir.dt.float32
BF16 = mybir.dt.bfloat16
U8 = mybir.dt.uint8
AF = mybir.ActivationFunctionType
ALU = mybir.AluOpType

ENGS = ("pe", "act", "dve", "pool", "sp")
N_DMA_SEMS = 16
DMA_POOLS = {"sp": list(range(0, 7)), "pool": list(range(7, 14)), "act": [14, 15]}
PAGE = 256
ARENA = 207 * 1024
PSUM_PAGE0 = 100000
DRAM_PAGE0 = 200000

D = 1024
T = 2304
NCTX = 256
NLAT = 2048
EPS = 1e-6


class Op:
    __slots__ = ("eng", "fn", "dma", "preds", "sig", "sigval", "dsem", "dval", "idx")


class Prog:
    def __init__(self, nc):
        self.nc = nc
        self.ops = []
        self.pw = {}
        self.pr = {}

    def add(self, eng, fn, reads=(), writes=(), dma=False):
        op = Op()
        op.eng = eng
        op.fn = fn
        op.dma = dma
        op.idx = len(self.ops)
        op.sig = False
        preds = set()
        pw, pr = self.pw, self.pr
        for (lo, hi) in reads:
            for p in range(lo, hi):
                w = pw.get(p)
                if w is not None:
                    preds.add(w)
        for (lo, hi) in writes:
            for p in range(lo, hi):
                w = pw.get(p)
                if w is not None:
                    preds.add(w)
                r = pr.get(p)
                if r:
                    preds.update(r.values())
        rkey = ("d", op.idx) if dma else eng
        for (lo, hi) in reads:
            for p in range(lo, hi):
                r = pr.get(p)
                if r is None:
                    pr[p] = {rkey: op.idx}
                else:
                    r[rkey] = op.idx
        for (lo, hi) in writes:
            for p in range(lo, hi):
                pw[p] = op.idx
                pr[p] = None
        preds.discard(op.idx)
        op.preds = preds
        self.ops.append(op)
        return op

    def emit(self):
        nc = self.nc
        ops = self.ops
        with contextlib.ExitStack() as es:
            esem = {e: es.enter_context(nc.semaphore(f"s_{e}")) for e in ENGS}
            dsems = [es.enter_context(nc.semaphore(f"s_dma{i}")) for i in range(N_DMA_SEMS)]
            waits = [None] * len(ops)
            for op in ops:
                per_eng = {}
                dma_w = []
                for p in op.preds:
                    po = ops[p]
                    if po.dma:
                        dma_w.append(p)
                    else:
                        if po.eng == "pe" and op.eng == "pe" and not op.dma:
                            continue
                        if po.eng not in per_eng or per_eng[po.eng] < p:
                            per_eng[po.eng] = p
                for p in per_eng.values():
                    ops[p].sig = True
                waits[op.idx] = (list(per_eng.values()), dma_w)
            cnt = {e: 0 for e in ENGS}
            dcnt = [0] * N_DMA_SEMS
            dlast = [None] * N_DMA_SEMS
            dpos = {e: 0 for e in ENGS}
            dma_prev = {}
            for op in ops:
                if op.dma:
                    rng = DMA_POOLS[op.eng]
                    k = rng[dpos[op.eng] % len(rng)]
                    dpos[op.eng] += 1
                    dcnt[k] += 16
                    op.dsem = k
                    op.dval = dcnt[k]
                    dma_prev[op.idx] = dlast[k]
                    dlast[k] = op.idx
                elif op.sig:
                    cnt[op.eng] += 1
                    op.sigval = cnt[op.eng]
            streams = {e: [o for o in ops if o.eng == e] for e in ENGS}
            self.stats = {e: len(streams[e]) for e in ENGS}
            self.stats["sig"] = dict(cnt)

            def run_stream(e, eng):
                known = {}

                def wait(key, sem, val):
                    if known.get(key, 0) >= val:
                        return
                    known[key] = val
                    eng.wait_ge(sem, val)

                for op in streams[e]:
                    cw, dw = waits[op.idx]
                    for p in cw:
                        po = ops[p]
                        wait(po.eng, esem[po.eng], po.sigval)
                    for p in dw:
                        po = ops[p]
                        wait(("d", po.dsem), dsems[po.dsem], po.dval)
                    if op.dma:
                        pp = dma_prev[op.idx]
                        if pp is not None:
                            po = ops[pp]
                            wait(("d", po.dsem), dsems[po.dsem], po.dval)
                        ins = op.fn(eng)
                        ins.then_inc(dsems[op.dsem], 16)
                    else:
                        ins = op.fn(eng)
                        if op.sig:
                            ins.then_inc(esem[e], 1)
                if e == "sp":
                    for k in range(N_DMA_SEMS):
                        if dcnt[k]:
                            eng.wait_ge(dsems[k], dcnt[k])

            with nc.Block() as block:
                @block.tensor
                def _(eng):
                    run_stream("pe", eng)

                @block.scalar
                def _(eng):
                    run_stream("act", eng)

                @block.vector
                def _(eng):
                    run_stream("dve", eng)

                @block.gpsimd
                def _(eng):
                    run_stream("pool", eng)

                @block.sync
                def _(eng):
                    run_stream("sp", eng)


class V:
    __slots__ = ("ap", "pg")

    def __init__(self, ap, pg):
        self.ap = ap
        self.pg = pg

    def m(self, f):
        return V(f(self.ap), self.pg)


class Tile:
    def __init__(self, k, off, shape, dt, name):
        self.k = k
        self.off = off
        self.shape = list(shape)
        self.dt = dt
        self.es = 4 if dt == F32 else (2 if dt == BF16 else 1)
        n = 1
        for s in shape[1:]:
            n *= s
        self.n = n
        self.nbytes = n * self.es
        base = k.arena[0:shape[0], off:off + self.nbytes]
        if dt != U8:
            base = base.bitcast(dt)
        if len(shape) == 3:
            base = base.rearrange("p (a b) -> p a b", a=shape[1])
        elif len(shape) == 4:
            base = base.rearrange("p (a b c) -> p a b c", a=shape[1], b=shape[2])
        self.ap = base
        self.name = name

    def _pages(self, lo_e, hi_e):
        lo = self.off + lo_e * self.es
        hi = self.off + hi_e * self.es
        return (lo // PAGE, (hi + PAGE - 1) // PAGE)

    @property
    def all(self):
        return V(self.ap, [self._pages(0, self.n)])

    def __getitem__(self, idx):
        if not isinstance(idx, tuple):
            idx = (idx,)
        ap = self.ap[idx]
        fidx = list(idx[1:]) + [slice(None)] * (len(self.shape) - len(idx))
        dims = self.shape[1:]
        rng = []
        for dim, ix in zip(dims, fidx):
            if isinstance(ix, int):
                rng.append((ix, ix + 1))
            else:
                a = 0 if ix.start is None else ix.start
                b = dim if ix.stop is None else ix.stop
                rng.append((a, b))
        strides = []
        st = 1
        for dim in reversed(dims):
            strides.insert(0, st)
            st *= dim
        lead = rng[:-1]
        cnt = 1
        for a, b in lead:
            cnt *= (b - a)
        pages = []
        if cnt <= 64:
            import itertools
            for combo in itertools.product(*[range(a, b) for a, b in lead]):
                base = sum(c * s_ for c, s_ in zip(combo, strides[:-1]))
                pages.append(self._pages(base + rng[-1][0], base + rng[-1][1]))
        else:
            lo = sum(a * s_ for (a, b), s_ in zip(rng, strides))
            hi = sum((b - 1) * s_ for (a, b), s_ in zip(rng, strides)) + 1
            pages.append(self._pages(lo, hi))
        return V(ap, pages)


def pgs(vs):
    out = []
    for v in vs:
        out.extend(v.pg)
    return out


class K:
    def __init__(self, nc, es):
        self.nc = nc
        self.P = Prog(nc)
        self.arena_t = es.enter_context(nc.sbuf_tensor("arena", [128, ARENA], U8))
        self.arena = self.arena_t
        self.free_list = [(0, ARENA)]
        self.banks = [es.enter_context(nc.psum_tensor(f"bank{i}", [128, 512], F32)) for i in range(8)]
        self.bank_i = 0
        self.dram_pg = DRAM_PAGE0

    def tile(self, shape, dt, name="t"):
        es_ = 4 if dt == F32 else (2 if dt == BF16 else 1)
        n = 1
        for s in shape[1:]:
            n *= s
        nb = ((n * es_ + PAGE - 1) // PAGE) * PAGE
        for i, (o, sz) in enumerate(self.free_list):
            if sz >= nb:
                if sz == nb:
                    self.free_list.pop(i)
                else:
                    self.free_list[i] = (o + nb, sz - nb)
                t = Tile(self, o, shape, dt, name)
                t.alloc = nb
                return t
        raise RuntimeError(f"SBUF arena full allocating {name} {shape} ({nb}B); free={self.free_list}")

    def free(self, *tiles):
        for t in tiles:
            self.free_list.append((t.off, t.alloc))
        self.free_list.sort()
        merged = []
        for o, sz in self.free_list:
            if merged and merged[-1][0] + merged[-1][1] == o:
                merged[-1] = (merged[-1][0], merged[-1][1] + sz)
            else:
                merged.append((o, sz))
        self.free_list = merged

    def ps(self, i, rows=128, c0=0, c1=512, r0=0):
        b = self.banks[i]
        return V(b[r0:r0 + rows, c0:c1], [(PSUM_PAGE0 + i, PSUM_PAGE0 + i + 1)])

    def ps3(self, i, a, rows=128):
        b = self.banks[i]
        return V(b[0:rows, :].rearrange("p (a b) -> p a b", a=a), [(PSUM_PAGE0 + i, PSUM_PAGE0 + i + 1)])

    def dram(self, ap):
        self.dram_pg += 1
        return V(ap, [(self.dram_pg, self.dram_pg + 1)])

    def op(self, eng, fn, R=(), W=()):
        self.P.add(eng, fn, pgs(R), pgs(W))

    def dma(self, eng, out, in_):
        self.P.add(eng, lambda e: e.dma_start(out=out.ap, in_=in_.ap), pgs([in_]), pgs([out]), dma=True)

    def mm(self, out, lhsT, rhs, start=True, stop=True):
        self.P.add("pe", lambda e: e.matmul(out.ap, lhsT=lhsT.ap, rhs=rhs.ap, start=start, stop=stop),
                   pgs([lhsT, rhs]), pgs([out]))

    def transpose(self, out, in_, ident):
        self.P.add("pe", lambda e: e.transpose(out.ap, in_.ap, ident.ap), pgs([in_, ident]), pgs([out]))

    def act(self, out, in_, func, bias=None, scale=None):
        R = [in_]
        kw = {}
        if bias is not None:
            if isinstance(bias, V):
                R.append(bias)
                kw["bias"] = bias.ap
            else:
                kw["bias"] = float(bias)
        if scale is not None:
            if isinstance(scale, V):
                R.append(scale)
                kw["scale"] = scale.ap
            else:
                kw["scale"] = float(scale)
        self.P.add("act", lambda e: e.activation(out=out.ap, in_=in_.ap, func=func, **kw),
                   pgs(R), pgs([out]))

    def copy(self, eng, out, in_):
        if eng == "act":
            self.P.add("act", lambda e: e.copy(out=out.ap, in_=in_.ap), pgs([in_]), pgs([out]))
        else:
            self.P.add(eng, lambda e: e.tensor_copy(out=out.ap, in_=in_.ap), pgs([in_]), pgs([out]))

    def tt(self, eng, out, in0, in1, op):
        self.P.add(eng, lambda e: e.tensor_tensor(out=out.ap, in0=in0.ap, in1=in1.ap, op=op),
                   pgs([in0, in1]), pgs([out]))

    def ts(self, eng, out, in0, s1, s2, op0, op1=None):
        R = [in0]
        a1 = s1.ap if isinstance(s1, V) else float(s1)
        if isinstance(s1, V):
            R.append(s1)
        if s2 is None:
            self.P.add(eng, lambda e: e.tensor_scalar(out=out.ap, in0=in0.ap, scalar1=a1, scalar2=None, op0=op0),
                       pgs(R), pgs([out]))
            return
        a2 = s2.ap if isinstance(s2, V) else float(s2)
        if isinstance(s2, V):
            R.append(s2)
        self.P.add(eng, lambda e: e.tensor_scalar(out=out.ap, in0=in0.ap, scalar1=a1, scalar2=a2, op0=op0, op1=op1),
                   pgs(R), pgs([out]))

    def stt(self, eng, out, in0, scalar, in1, op0, op1):
        R = [in0, in1]
        a = scalar.ap if isinstance(scalar, V) else float(scalar)
        if isinstance(scalar, V):
            R.append(scalar)
        self.P.add(eng, lambda e: e.scalar_tensor_tensor(out=out.ap, in0=in0.ap, scalar=a, in1=in1.ap, op0=op0, op1=op1),
                   pgs(R), pgs([out]))

    def memset(self, eng, out, val):
        self.P.add(eng, lambda e: e.memset(out.ap, val), [], pgs([out]))

    def recip(self, out, in_):
        self.P.add("dve", lambda e: e.reciprocal(out=out.ap, in_=in_.ap), pgs([in_]), pgs([out]))

    def scan(self, out, d0, d1, initial):
        R = [d0, d1]
        a = initial.ap if isinstance(initial, V) else float(initial)
        if isinstance(initial, V):
            R.append(initial)
        self.P.add("dve", lambda e: e.tensor_tensor_scan(out=out.ap, data0=d0.ap, data1=d1.ap, initial=a,
                                                       op0=ALU.mult, op1=ALU.add),
                   pgs(R), pgs([out]))


def col_blocks(ctx=True, lat=True):
    b = []
    if ctx:
        b.append((0, NCTX, 1))
    if lat:
        for i in range(4):
            b.append((NCTX + 512 * i, 512, 0))
    return b


def fm(v):
    v = np.asarray(v)
    n = v.shape[-1] // 128
    return np.ascontiguousarray(v.reshape(v.shape[:-1] + (n, 128)).swapaxes(-1, -2))


def rope_tables(rot_dim, nfeat, rot_off):
    n_freq = rot_dim // 4
    inv_freq = (10000.0 ** (-np.arange(n_freq, dtype=np.float32) / n_freq)).astype(np.float32)
    t = np.arange(NLAT)
    row = (t // 64).astype(np.float32)
    col = (t % 64).astype(np.float32)
    ang = np.concatenate([row[:, None] * inv_freq, col[:, None] * inv_freq], axis=-1).astype(np.float32)
    cos = np.cos(ang).astype(np.float32)
    sin = np.sin(ang).astype(np.float32)
    C = np.ones((nfeat, T), np.float32)
    S = np.zeros((nfeat, T), np.float32)
    half = rot_dim // 2
    for f in range(rot_off, nfeat):
        i = (f - rot_off) % half
        C[f, NCTX:] = cos[:, i]
        S[f, NCTX:] = sin[:, i]
    R = np.zeros((nfeat, nfeat), np.float32)
    for i in range(half):
        R[rot_off + i + half, rot_off + i] = -1.0
        R[rot_off + i, rot_off + i + half] = 1.0
    return C, S, R


def host_consts():
    c = {}
    c["ident"] = np.eye(128, dtype=np.float32)
    cg, sg, rg = rope_tables(64, 64, 0)
    c["rope_g_cos"], c["rope_g_sin"], c["rope_g_R"] = cg, sg, rg
    cm, sm, rm = rope_tables(32, 96, 64)
    c["rope_m_cos"], c["rope_m_sin"], c["rope_m_R"] = cm, sm, rm
    L = NLAT
    N = 2 * L
    s = np.arange(L, dtype=np.float64)[:, None]
    f = np.arange(L, dtype=np.float64)[None, :]
    th = np.pi * (2 * f + 1) * s / N
    Cm = np.cos(th)
    Sm = np.sin(th)
    bf = ml_dtypes.bfloat16
    def tile_(M):
        return np.ascontiguousarray(M.reshape(16, 128, 16, 128).transpose(2, 1, 0, 3).reshape(16, 128, 2048)).astype(bf)
    c["dft_cf"] = tile_(Cm)
    c["dft_sf"] = tile_(Sm)
    c["dft_ci"] = tile_(Cm.T * (2.0 / N))
    c["dft_si"] = tile_(-Sm.T * (2.0 / N))
    t = np.arange(L, dtype=np.float32)[:, None]
    t_norm = t / max(L - 1, 1)
    bands = np.linspace(1e-4, 16 - 1, 16, dtype=np.float32)
    ang = (2.0 * math.pi * t * bands / L).astype(np.float32)
    z = np.concatenate([t_norm, np.cos(ang), -np.sin(ang)], axis=-1).astype(np.float32)
    c["hy_zT"] = np.ascontiguousarray(z.T)
    HY_MIN = math.log(1e-2) / 1.5
    HY_MAX = math.log(1e-2) / 0.3
    deltas = np.abs(np.linspace(HY_MIN, HY_MAX, 512, dtype=np.float32))
    window = (np.exp(-t_norm * deltas) + 0.05).astype(np.float32)
    c["hy_window"] = window
    return c


def build(shapes, stage=99):
    nc = bass.Bass("TRN2", target_bir_lowering=False)
    dr = {}
    for name, shp in shapes.items():
        dt_ = F32
        if isinstance(shp, tuple) and len(shp) == 2 and isinstance(shp[1], str):
            shp, dts = shp
            dt_ = BF16 if dts == "bfloat16" else F32
        dr[name] = nc.dram_tensor(name, list(shp), dt_, kind="ExternalInput").ap()
    out_d = nc.dram_tensor("out", [NLAT, D], F32, kind="ExternalOutput").ap()
    outc_d = nc.dram_tensor("outc", [NCTX, D], F32, kind="ExternalOutput").ap()
    with contextlib.ExitStack() as es:
        k = K(nc, es)
        _emit_program(k, dr, out_d, outc_d, stage)
        k.P.emit()
    return nc


def _emit_program(k, dr, out_d, outc_d, stage):
    dm = k.dram
    SW = "pool"

    xs = k.tile([128, 8, T], F32, "xs")
    hbf = k.tile([128, 8, T], BF16, "hbf")
    ident_f = k.tile([128, 128], F32, "ident_f")
    ident_b = k.tile([128, 128], BF16, "ident_b")
    ones_b = k.tile([128, 128], BF16, "ones_b")
    mods = k.tile([128, 2, 48, 2], F32, "mods")
    affA = k.tile([128, 2, 2, 16], F32, "affA")
    gdup = k.tile([128, 2, 2, 16], F32, "gdup")
    modb = k.tile([128, 2, 48], F32, "modb")

    k.dma("sp", ident_f.all, dm(dr["ident"]))
    k.copy("dve", ident_b.all, ident_f.all)
    k.memset("dve", ones_b.all, 1.0)
    k.dma("sp", gdup.all, dm(dr["gdup"]))
    k.dma("sp", modb.all, dm(dr["modb"]))

    xts = [k.tile([128, 1024], F32, f"xt{i}") for i in range(2)]
    for tc in range(18):
        src = dr["ctx"][tc * 128:(tc + 1) * 128, :] if tc < 2 else dr["x"][(tc - 2) * 128:(tc - 1) * 128, :]
        xt = xts[tc % 2]
        k.dma("sp", xt.all, dm(src))
        for half in range(2):
            b = (tc * 2 + half) % 4
            for kk in range(4):
                kf = half * 4 + kk
                k.transpose(k.ps(b, c0=kk * 128, c1=(kk + 1) * 128), xt[:, kf * 128:(kf + 1) * 128], ident_f.all)
            k.copy("dve" if half == 0 else "act", xs[:, half * 4:(half + 1) * 4, tc * 128:(tc + 1) * 128], k.ps3(b, 4))
    k.free(*xts)

    cl = k.tile([128, 8], F32, "cl")
    cc = k.tile([128, 8], F32, "cc")
    s_bf = k.tile([128, 8, 2], BF16, "s_bf")
    k.dma("sp", cl.all, dm(dr["c_fm"]))
    k.dma("sp", cc.all, dm(dr["cctx_fm"]))
    k.act(s_bf[:, :, 0], cl.all, AF.Silu)
    k.act(s_bf[:, :, 1], cc.all, AF.Silu)
    mwt = [k.tile([128, 8, 512], BF16, f"mw{i}") for i in range(2)]
    gi = 0
    for l in range(2):
        mw = dr["mod_w"][l].rearrange("(kc p) n -> p kc n", p=128)
        for g in range(12):
            wt = mwt[gi % 2]
            k.dma(SW, wt.all, dm(mw[:, :, g * 512:(g + 1) * 512]))
            b = 4 + gi % 2
            for j in range(4):
                for kk in range(8):
                    k.mm(k.ps(b, c0=2 * j, c1=2 * j + 2), wt[:, kk, j * 128:(j + 1) * 128], s_bf[:, kk, :],
                         start=(kk == 0), stop=(kk == 7))
            for j in range(4):
                ch = g * 4 + j
                k.ts("dve", mods[:, l, ch, :], k.ps(b, c0=2 * j, c1=2 * j + 2), modb[:, l, ch:ch + 1], None, ALU.add)
            gi += 1
    k.free(*mwt, cl, cc, s_bf)
    for l in range(2):
        for n, mi in ((0, 1), (1, 4)):
            src = V(mods.ap[:, l, mi * 8:(mi + 1) * 8, :].rearrange("p a b -> p (a b)"), mods[:, l, mi * 8:(mi + 1) * 8, :].pg)
            k.stt("dve", affA[:, l, n, :], src, 1.0, gdup[:, l, n, :], ALU.add, ALU.mult)

    def A_(l, n, kk, s):
        return affA[:, l, n, kk * 2 + s:kk * 2 + s + 1]

    def M_(l, mi, kk, s):
        return mods[:, l, mi * 8 + kk, s:s + 1]

    def norm_mod(l, n, blocks, f32_hook=None):
        sqs = [k.tile([128, 8, 512], BF16, f"sq{i}") for i in range(2)]
        rstds = [k.tile([128, 512], F32, f"rstd{i}") for i in range(2)]
        tmps = [k.tile([128, 512], F32, f"nt{i}") for i in range(3)]
        ti = 0
        for bi, (c0, ncl, s) in enumerate(blocks):
            sq = sqs[bi % 2]
            rstd = rstds[bi % 2]
            for kk in range(8):
                k.act(sq[:, kk, 0:ncl], xs[:, kk, c0:c0 + ncl], AF.Square)
            b = 4 + bi % 2
            for kk in range(8):
                k.mm(k.ps(b, c1=ncl), ones_b.all, sq[:, kk, 0:ncl], start=(kk == 0), stop=(kk == 7))
            k.act(rstd[:, 0:ncl], k.ps(b, c1=ncl), AF.Sqrt, bias=EPS, scale=1.0 / D)
            k.recip(rstd[:, 0:ncl], rstd[:, 0:ncl])
            for kk in range(8):
                tmp = tmps[ti % 3]
                ti += 1
                k.tt("dve" if kk % 2 == 0 else "pool", tmp[:, 0:ncl], xs[:, kk, c0:c0 + ncl], rstd[:, 0:ncl], ALU.mult)
                k.act(hbf[:, kk, c0:c0 + ncl], tmp[:, 0:ncl], AF.Identity,
                      bias=M_(l, 0 if n == 0 else 3, kk, s), scale=A_(l, n, kk, s))
                if f32_hook is not None:
                    f32_hook(bi, c0, ncl, kk, tmp, s)
        k.free(*sqs, *rstds, *tmps)

    def write_out():
        ots = [k.tile([128, 1024], F32, f"ot{i}") for i in range(2)]
        for tc in range(18):
            ot = ots[tc % 2]
            for half in range(2):
                b = (tc * 2 + half) % 4
                for kk in range(4):
                    kf = half * 4 + kk
                    k.transpose(k.ps(b, c0=kk * 128, c1=(kk + 1) * 128), xs[:, kf, tc * 128:(tc + 1) * 128], ident_f.all)
                k.copy("dve" if half == 0 else "act", ot[:, half * 512:(half + 1) * 512], k.ps(b))
            dst = outc_d[tc * 128:(tc + 1) * 128, :] if tc < 2 else out_d[(tc - 2) * 128:(tc - 1) * 128, :]
            k.dma("sp", dm(dst), ot.all)
        k.free(*ots)

    if stage == 0:
        write_out()
        return

    def qk_norm_rope(psv, nrows, gain, onesv, cosT, sinT, Rm, outv, c0, ncl, tl, bi, banks):
        sq, rstd, qn, t1 = tl
        k.act(sq[0:nrows, 0:ncl], psv, AF.Square)
        b1 = banks[0]
        k.mm(k.ps(b1, rows=nrows, c1=ncl), onesv, sq[0:nrows, 0:ncl])
        k.act(rstd[0:nrows, 0:ncl], k.ps(b1, rows=nrows, c1=ncl), AF.Sqrt, bias=EPS, scale=1.0 / nrows)
        k.recip(rstd[0:nrows, 0:ncl], rstd[0:nrows, 0:ncl])
        k.stt("dve", qn[0:nrows, 0:ncl], psv, gain, rstd[0:nrows, 0:ncl], ALU.mult, ALU.mult)
        b2 = banks[1]
        k.copy("act", sq[0:nrows, 0:ncl], qn[0:nrows, 0:ncl])
        k.mm(k.ps(b2, rows=nrows, c1=ncl), Rm, sq[0:nrows, 0:ncl])
        k.tt("pool", t1[0:nrows, 0:ncl], qn[0:nrows, 0:ncl], cosT[0:nrows, c0:c0 + ncl], ALU.mult)
        k.tt("dve", qn[0:nrows, 0:ncl], k.ps(b2, rows=nrows, c1=ncl), sinT[0:nrows, c0:c0 + ncl], ALU.mult)
        k.tt("pool", outv, t1[0:nrows, 0:ncl], qn[0:nrows, 0:ncl], ALU.add)

    def attend(qh, kh, vaug, Dh, q_blocks, n_kc, scale, ycat, yrow0, ychunk, etl, rdt):
        for qi, (c0, ncl) in enumerate(q_blocks):
            ob = qi % 2
            LA = 2
            for it in range(n_kc + LA):
                if it < n_kc:
                    kc = it
                    sb = 2 + kc % 3
                    e = etl[kc % len(etl)]
                    k.mm(k.ps(sb, c1=ncl), kh[0:Dh, kc * 128:(kc + 1) * 128], qh[0:Dh, c0:c0 + ncl])
                    k.act(e[:, 0:ncl], k.ps(sb, c1=ncl), AF.Exp, scale=scale)
                if it >= LA:
                    kc = it - LA
                    e = etl[kc % len(etl)]
                    k.mm(k.ps(ob, c1=ncl), vaug[:, kc, 64:192], e[:, 0:ncl], start=(kc == 0), stop=(kc == n_kc - 1))
            rd, rs = rdt[qi % len(rdt)]
            k.recip(rd[64:128, 0:ncl], k.ps(ob, rows=64, r0=64, c1=ncl))
            k.copy("pool", rs[0:64, 0:ncl], rd[64:128, 0:ncl])
            k.tt("dve", ycat[yrow0:yrow0 + 64, ychunk, c0:c0 + ncl], k.ps(ob, rows=64, c1=ncl), rs[0:64, 0:ncl], ALU.mult)

    def project_out(l, ycat, w_d, krow0, blocks):
        wo = k.tile([128, 4, 1024], BF16, "wo")
        k.dma(SW, wo.all, dm(w_d[krow0:krow0 + 512, :].rearrange("(kc p) n -> p kc n", p=128)))
        i = 0
        for (c0, ncl, s) in blocks:
            for d in range(8):
                b = 4 + i % 4
                i += 1
                for kk in range(4):
                    k.mm(k.ps(b, c1=ncl), wo[:, kk, d * 128:(d + 1) * 128], ycat[:, kk, c0:c0 + ncl],
                         start=(kk == 0), stop=(kk == 3))
                k.stt("dve", xs[:, d, c0:c0 + ncl], k.ps(b, c1=ncl), M_(l, 2, d, s),
                      xs[:, d, c0:c0 + ncl], ALU.mult, ALU.add)
        k.free(wo)

    def ffn(l, w1_d, w3_d, w2_d, dff, blocks, hid, gate_bc=None):
        ngroups = (dff + 511) // 512
        w1t = [k.tile([128, 8, 512], BF16, f"w1_{i}") for i in range(2)]
        w3t = [k.tile([128, 8, 512], BF16, f"w3_{i}") for i in range(2)]
        w2t = [k.tile([128, 4, 1024], BF16, f"w2_{i}") for i in range(2)]
        sts = [k.tile([128, 512], F32, f"st{i}") for i in range(3)]
        w1v = w1_d.rearrange("(kc p) n -> p kc n", p=128)
        w3v = w3_d.rearrange("(kc p) n -> p kc n", p=128)
        si = 0
        ai = 0
        for g in range(ngroups):
            f0 = g * 512
            fw = min(512, dff - f0)
            nch = fw // 128
            w1, w3, w2 = w1t[g % 2], w3t[g % 2], w2t[g % 2]
            k.dma(SW, w1[:, :, 0:fw], dm(w1v[:, :, f0:f0 + fw]))
            k.dma(SW, w3[:, :, 0:fw], dm(w3v[:, :, f0:f0 + fw]))
            k.dma(SW, w2[:, 0:nch, :], dm(w2_d[f0:f0 + fw, :].rearrange("(kc p) n -> p kc n", p=128)))
            for m in range(nch):
                for (c0, ncl, s) in blocks:
                    bg = si % 2
                    bu = 2 + si % 2
                    st = sts[si % 3]
                    si += 1
                    for kk in range(8):
                        k.mm(k.ps(bg, c1=ncl), w1[:, kk, m * 128:(m + 1) * 128], hbf[:, kk, c0:c0 + ncl],
                             start=(kk == 0), stop=(kk == 7))
                    for kk in range(8):
                        k.mm(k.ps(bu, c1=ncl), w3[:, kk, m * 128:(m + 1) * 128], hbf[:, kk, c0:c0 + ncl],
                             start=(kk == 0), stop=(kk == 7))
                    k.act(st[:, 0:ncl], k.ps(bg, c1=ncl), AF.Silu)
                    if gate_bc is None:
                        k.tt("dve", hid[:, m, c0:c0 + ncl], st[:, 0:ncl], k.ps(bu, c1=ncl), ALU.mult)
                    else:
                        k.tt("dve", st[:, 0:ncl], st[:, 0:ncl], k.ps(bu, c1=ncl), ALU.mult)
                        k.tt("pool", hid[:, m, c0:c0 + ncl], st[:, 0:ncl], gate_bc[:, c0 - NCTX:c0 - NCTX + ncl], ALU.mult)
            for (c0, ncl, s) in blocks:
                for d in range(8):
                    b = 4 + ai % 4
                    ai += 1
                    for kk in range(nch):
                        k.mm(k.ps(b, c1=ncl), w2[:, kk, d * 128:(d + 1) * 128], hid[:, kk, c0:c0 + ncl],
                             start=(kk == 0), stop=(kk == nch - 1))
                    k.stt("dve", xs[:, d, c0:c0 + ncl], k.ps(b, c1=ncl), M_(l, 5, d, s),
                          xs[:, d, c0:c0 + ncl], ALU.mult, ALU.add)
        k.free(*w1t, *w3t, *w2t, *sts)

    ALLB = col_blocks(True, True)
    LATB = col_blocks(False, True)

    norm_mod(0, 0, ALLB)
    ycat = k.tile([128, 4, T], BF16, "ycat")
    layer0_mixer(k, dr, xs, hbf, ycat, ident_f, ones_b, M_, project_out, qk_norm_rope, attend, ALLB)
    if stage == 1:
        write_out()
        return
    norm_mod(0, 1, ALLB)
    ffn(0, dr["ffn_w1"], dr["ffn_w3"], dr["ffn_w2"], 2816, ALLB, ycat)
    if stage == 2:
        write_out()
        return
    import os
    SUB = int(os.environ.get("SUB1", "99"))
    norm_mod(1, 0, ALLB)
    layer1_mixer(k, dr, xs, hbf, ycat, ident_b, ones_b, M_, project_out, qk_norm_rope, attend, ALLB, LATB, SUB)
    if stage == 3:
        write_out()
        return
    moe_layer(k, dr, xs, hbf, ycat, ident_f, ones_b, M_, A_, norm_mod, ffn, LATB)
    write_out()


def layer0_mixer(k, dr, xs, hbf, ycat, ident_f, ones_b, M_, project_out, qk_norm_rope, attend, ALLB):
    import os
    SUB = int(os.environ.get("SUBSTAGE", "99"))
    dm = k.dram
    SW = "pool"
    win = dr["ab_w_in"].rearrange("(kc p) n -> p kc n", p=128)
    cosT = k.tile([64, T], F32, "cosT")
    sinT = k.tile([64, T], F32, "sinT")
    Rm = k.tile([64, 64], BF16, "Rm")
    gq = k.tile([64, 2], F32, "gqk")
    k.dma("sp", cosT.all, dm(dr["rope_g_cos"]))
    k.dma("sp", sinT.all, dm(dr["rope_g_sin"]))
    k.dma(SW, Rm.all, dm(dr["rope_g_R"]))
    k.dma("sp", gq.all, dm(dr["gqa_norms"]))
    wqkv = k.tile([128, 8, 768], BF16, "wqkv")
    k.dma(SW, wqkv.all, dm(win[:, :, 1024:1792]))
    kh = [k.tile([64, T], BF16, f"kh{i}") for i in range(2)]
    vaug = [k.tile([128, 18, 192], BF16, f"vaug{i}") for i in range(2)]
    tl = (k.tile([64, 512], BF16, "n_sq"), k.tile([64, 512], F32, "n_rstd"), k.tile([64, 512], F32, "n_qn"),
          k.tile([64, 512], F32, "n_t1"))
    onesv = ones_b[0:64, 0:64]
    bi = 0
    for h in range(2):
        for (c0, ncl, s) in ALLB:
            b = 4 + bi % 2
            bi += 1
            for kk in range(8):
                k.mm(k.ps(b, rows=64, c1=ncl), wqkv[:, kk, 512 + h * 64:512 + (h + 1) * 64], hbf[:, kk, c0:c0 + ncl],
                     start=(kk == 0), stop=(kk == 7))
            qk_norm_rope(k.ps(b, rows=64, c1=ncl), 64, gq[:, 1:2], onesv, cosT, sinT, Rm.all,
                         kh[h][0:64, c0:c0 + ncl], c0, ncl, tl, bi, (6, 7))
    if SUB == 1:
        return
    BIS = os.environ.get("BIS", "")
    for h in range(2):
        if "m" not in BIS:
            k.memset("pool", vaug[h].all, 1.0)
    for tc in range(18 if "t" not in BIS else 1):
        b = 4 + tc % 2
        if "x" not in BIS:
            for kk in range(8):
                k.mm(k.ps(b, c1=128), hbf[:, kk, tc * 128:(tc + 1) * 128], wqkv[:, kk, 640:768],
                     start=(kk == 0), stop=(kk == 7))
        if "c" not in BIS:
            for h in range(2):
                ce = "dve"
                if "d" in BIS:
                    ce = "dve"
                if "a" in BIS:
                    ce = "act"
                k.copy(ce, vaug[h][:, tc, 64:128], k.ps(b, c0=h * 64, c1=(h + 1) * 64))
    if SUB == 2:
        return
    qhs = [k.tile([64, T], BF16, f"qh{i}") for i in range(2)]
    etl = [k.tile([128, 512], BF16, f"e{i}") for i in range(4)]
    rdt = [(k.tile([128, 512], F32, f"rd{i}"), k.tile([64, 512], F32, f"rs{i}")) for i in range(1)]
    scale = 64 ** -0.5
    for h in range(8 if SUB != 3 else 1):
        qh = qhs[h % 2]
        for (c0, ncl, s) in ALLB:
            b = 4 + bi % 2
            bi += 1
            for kk in range(8):
                k.mm(k.ps(b, rows=64, c1=ncl), wqkv[:, kk, h * 64:(h + 1) * 64], hbf[:, kk, c0:c0 + ncl],
                     start=(kk == 0), stop=(kk == 7))
            qk_norm_rope(k.ps(b, rows=64, c1=ncl), 64, gq[:, 0:1], onesv, cosT, sinT, Rm.all,
                         qh[0:64, c0:c0 + ncl], c0, ncl, tl, bi, (6, 7))
        kv = h // 4
        attend(qh, kh[kv], vaug[kv], 64, [(0, NCTX)], 2, scale, ycat, (h % 2) * 64, h // 2, etl, rdt)
        attend(qh, kh[kv], vaug[kv], 64, [(NCTX + 512 * i, 512) for i in range(4)], 18, scale, ycat,
               (h % 2) * 64, h // 2, etl, rdt)
    k.free(cosT, sinT, Rm, gq, wqkv, *kh, *vaug, *tl, *qhs, *etl, *[t for p in rdt for t in p])
    if SUB == 3:
        return
    project_out(0, ycat, dr["ab_w_out"], 512, ALLB)
    if SUB == 4:
        return

    cw = k.tile([128, 4, 4], F32, "convw")
    cb = k.tile([128, 4], F32, "convb")
    lb = k.tile([128, 2, 2, 4], F32, "lru_b")
    lam = k.tile([128, 2, 4], F32, "lam")
    cneg = k.tile([128, 2, 4], F32, "cneg")
    k.dma("sp", cw.all, dm(dr["lru_convw_fm"]))
    k.dma("sp", cb.all, dm(dr["lru_convb_fm"]))
    k.dma("sp", lb.all, dm(dr["lru_b_fm"]))
    k.dma("sp", lam.all, dm(dr["lru_lam_fm"]))
    lamf = V(lam.ap.rearrange("p a b -> p (a b)"), lam.all.pg)
    cnegf = V(cneg.ap.rearrange("p a b -> p (a b)"), cneg.all.pg)
    k.act(cnegf, lamf, AF.Exp, scale=-1.0)
    k.act(cnegf, cnegf, AF.Ln, bias=1.0)
    k.ts("dve", cnegf, cnegf, -8.0, None, ALU.mult)
    xr = k.tile([128, T], F32, "xr")
    u = k.tile([128, T], F32, "u")
    ubf = k.tile([128, T], BF16, "ubf")
    rec = k.tile([128, T], F32, "rec")
    at = k.tile([128, T], F32, "a")
    bt = k.tile([128, T], F32, "b")
    ht = k.tile([128, T], F32, "h")
    wx = k.tile([128, 8, 256], BF16, "wxg")
    bd = [k.tile([128, 128], BF16, f"bd{i}") for i in range(4)]
    tmp = [k.tile([128, 512], F32, f"lt{i}") for i in range(4)]
    seqs = [(0, NCTX), (NCTX, NLAT)]
    for j in range(4):
        k.dma(SW, wx[:, :, 0:128], dm(k_win_slice(dr, j * 128)))
        k.dma(SW, wx[:, :, 128:256], dm(k_win_slice(dr, 512 + j * 128)))
        for d_ in range(2):
            for gi_, nm in enumerate(("lru_w_a", "lru_w_x")):
                t_ = bd[d_ * 2 + gi_]
                k.memset("pool", t_.all, 0.0)
                for hb_ in range(2):
                    k.dma(SW, t_[hb_ * 64:(hb_ + 1) * 64, hb_ * 64:(hb_ + 1) * 64], dm(dr[nm][d_, 2 * j + hb_]))
        for bi_, (c0, ncl, s) in enumerate(ALLB):
            b = 4 + bi_ % 2
            for kk in range(8):
                k.mm(k.ps(b, c1=ncl), wx[:, kk, 0:128], hbf[:, kk, c0:c0 + ncl], start=(kk == 0), stop=(kk == 7))
            k.copy("act", xr[:, c0:c0 + ncl], k.ps(b, c1=ncl))
        k.ts("dve", u.all, xr.all, cw[:, j, 2:3], cb[:, j:j + 1], ALU.mult, ALU.add)
        for tap in (0, 1, 3):
            sh = tap - 2
            for (s0, L_) in seqs:
                lo = s0 + max(0, -sh)
                hi = s0 + L_ - max(0, sh)
                k.stt("dve", u[:, lo:hi], xr[:, lo + sh:hi + sh], cw[:, j, tap:tap + 1], u[:, lo:hi], ALU.mult, ALU.add)
        k.copy("pool", ubf.all, u.all)
        for d_ in range(2):
            for bi_, (c0, ncl, s) in enumerate(ALLB):
                ba, bx = 4 + bi_ % 2, 6 + bi_ % 2
                k.mm(k.ps(ba, c1=ncl), bd[d_ * 2].all, ubf[:, c0:c0 + ncl])
                k.mm(k.ps(bx, c1=ncl), bd[d_ * 2 + 1].all, ubf[:, c0:c0 + ncl])
                r_, i_, q_, _ = tmp
                k.act(r_[:, 0:ncl], k.ps(ba, c1=ncl), AF.Sigmoid, bias=lb[:, 0, d_, j:j + 1])
                k.act(i_[:, 0:ncl], k.ps(bx, c1=ncl), AF.Sigmoid, bias=lb[:, 1, d_, j:j + 1])
                k.act(at[:, c0:c0 + ncl], r_[:, 0:ncl], AF.Exp, scale=cneg[:, d_, j:j + 1])
                k.tt("pool", q_[:, 0:ncl], at[:, c0:c0 + ncl], at[:, c0:c0 + ncl], ALU.mult)
                k.act(q_[:, 0:ncl], q_[:, 0:ncl], AF.Sqrt, bias=1.0, scale=-1.0)
                k.tt("dve", i_[:, 0:ncl], i_[:, 0:ncl], u[:, c0:c0 + ncl], ALU.mult)
                k.tt("dve", bt[:, c0:c0 + ncl], q_[:, 0:ncl], i_[:, 0:ncl], ALU.mult)
            if d_ == 0:
                k.scan(rec.all, at.all, bt.all, 0.0)
            else:
                rv = lambda v: v.m(lambda ap: ap[:, ::-1])
                k.scan(rv(ht[:, 0:NCTX]), rv(at[:, 0:NCTX]), rv(bt[:, 0:NCTX]), 0.0)
                k.scan(rv(ht[:, NCTX:T]), rv(at[:, NCTX:T]), rv(bt[:, NCTX:T]), ht[:, 0:1])
                k.tt("pool", rec.all, rec.all, ht.all, ALU.add)
        for bi_, (c0, ncl, s) in enumerate(ALLB):
            b = 4 + bi_ % 2
            for kk in range(8):
                k.mm(k.ps(b, c1=ncl), wx[:, kk, 128:256], hbf[:, kk, c0:c0 + ncl], start=(kk == 0), stop=(kk == 7))
            g_, t_, w_, _ = tmp
            k.copy("act", g_[:, 0:ncl], k.ps(b, c1=ncl))
            k.tt("pool", t_[:, 0:ncl], g_[:, 0:ncl], g_[:, 0:ncl], ALU.mult)
            k.ts("dve", t_[:, 0:ncl], t_[:, 0:ncl], 0.044715, 1.0, ALU.mult, ALU.add)
            k.tt("dve", t_[:, 0:ncl], t_[:, 0:ncl], g_[:, 0:ncl], ALU.mult)
            k.act(t_[:, 0:ncl], t_[:, 0:ncl], AF.Sigmoid, scale=2.0 * math.sqrt(2.0 / math.pi))
            k.tt("pool", t_[:, 0:ncl], t_[:, 0:ncl], g_[:, 0:ncl], ALU.mult)
            k.tt("dve", ycat[:, j, c0:c0 + ncl], t_[:, 0:ncl], rec[:, c0:c0 + ncl], ALU.mult)
    k.free(cw, cb, lb, lam, cneg, xr, u, ubf, rec, at, bt, ht, wx, *bd, *tmp)
    project_out(0, ycat, dr["ab_w_out"], 0, ALLB)


def k_win_slice(dr, c0):
    return dr["ab_w_in"].rearrange("(kc p) n -> p kc n", p=128)[:, :, c0:c0 + 128]


_CONSTS = None
_NC_CACHE = {}


def prep_inputs(inp, b):
    global _CONSTS
    if _CONSTS is None:
        _CONSTS = host_consts()
    f = lambda a: np.ascontiguousarray(np.asarray(a, dtype=np.float32))
    m = dict(_CONSTS)
    m.pop("rope_g_R"); m.pop("rope_m_R")
    m["rope_g_R"] = _CONSTS["rope_g_R"]; m["rope_m_R"] = _CONSTS["rope_m_R"]
    m["x"] = f(inp["x"][b])
    m["ctx"] = f(inp["ctx"][b])
    m["c_fm"] = f(fm(inp["c"][b]))
    m["cctx_fm"] = f(fm(inp["c_ctx"]))
    m["mod_w"] = f(inp["mod_w"])
    m["modb"] = f(fm(inp["mod_b"]).transpose(1, 0, 2))
    g = np.stack([fm(inp["norm1_g"]), fm(inp["norm2_g"])], axis=1)
    g = np.repeat(g[..., None], 2, axis=-1).reshape(2, 2, 128, 16)
    m["gdup"] = f(g.transpose(2, 0, 1, 3))
    m["ab_w_in"] = f(inp["ab_w_in"][0])
    m["ab_w_out"] = f(inp["ab_w_out"][0])
    m["gqa_norms"] = f(np.stack([inp["gqa_q_norm"][0], inp["gqa_k_norm"][0]], axis=1))
    m["lru_convw_fm"] = f(fm(inp["lru_conv_w"][0]).transpose(1, 2, 0))
    m["lru_convb_fm"] = f(fm(inp["lru_conv_b"][0]))
    lb = np.stack([fm(inp["lru_b_a"][0].reshape(2, 512)), fm(inp["lru_b_x"][0].reshape(2, 512))], axis=0)
    m["lru_b_fm"] = f(lb.transpose(2, 0, 1, 3))
    m["lru_lam_fm"] = f(fm(inp["lru_lambda"][0]).transpose(1, 0, 2))
    m["lru_w_a"] = f(inp["lru_w_a"][0])
    m["lru_w_x"] = f(inp["lru_w_x"][0])
    m["ffn_w1"] = f(inp["ffn_w1"][0])
    m["ffn_w3"] = f(inp["ffn_w3"][0])
    m["ffn_w2"] = f(inp["ffn_w2"][0])
    m["cd_w_in"] = f(inp["cd_w_in"][0])
    m["cd_w_out"] = f(inp["cd_w_out"][0])
    mg = np.zeros((128, 5), np.float32)
    mg[:, 0:2] = fm(inp["mla_q_a_norm"][0])
    mg[:, 2] = inp["mla_kv_a_norm"][0]
    mg[0:96, 3] = inp["mla_q_norm"][0]
    mg[0:96, 4] = inp["mla_k_norm"][0]
    m["mla_gains"] = mg
    m["mla_q_b"] = f(inp["mla_q_b"][0])
    m["mla_kv_b"] = f(inp["mla_kv_b"][0])
    m["hy_p"] = f(np.stack([inp["hy_filt_b1"][0], inp["hy_filt_b2"][0], inp["hy_sin_freq"][0]], axis=1))
    m["hy_filt_w1"] = f(inp["hy_filt_w1"][0])
    m["hy_filt_w2"] = f(inp["hy_filt_w2"][0])
    m["hy_filt_w3"] = f(inp["hy_filt_w3"][0])
    m["hy_conv_w"] = f(inp["hy_conv_w"][0])
    m["hy_conv_b"] = f(inp["hy_conv_b"][0])
    m["hy_skip"] = f(inp["hy_skip"][0])
    m["moe_router"] = f(inp["moe_router"][0])
    m["moe_w1"] = f(inp["moe_w1"][0])
    m["moe_w3"] = f(inp["moe_w3"][0])
    m["moe_w2"] = f(inp["moe_w2"][0])
    return m


def shapes_of(m):
    return {k_: (tuple(v.shape), str(v.dtype)) for k_, v in m.items()}


def kernel(**inputs):
    maps = [prep_inputs(inputs, b) for b in range(8)]
    shapes = shapes_of(maps[0])
    key = tuple(sorted(shapes.items()))
    if key not in _NC_CACHE:
        _NC_CACHE[key] = build(shapes)
    nc = _NC_CACHE[key]
    res = run_bass_kernel_spmd(nc, maps, core_ids=list(range(8)))
    return np.stack([np.asarray(r["out"], dtype=np.float32) for r in res.results], axis=0)


def ps_bf(k, i, rows, c0, c1):
    b = k.banks[i]
    return V(b[0:rows, :].bitcast(BF16)[:, c0:c1], [(PSUM_PAGE0 + i, PSUM_PAGE0 + i + 1)])


def layer1_mixer(k, dr, xs, hbf, ycat, ident_b, ones_b, M_, project_out, qk_norm_rope, attend, ALLB, LATB, SUB):
    dm = k.dram
    SW = "pool"
    win = dr["cd_w_in"].rearrange("(kc p) n -> p kc n", p=128)
    cosT = k.tile([96, T], F32, "cosM")
    sinT = k.tile([96, T], F32, "sinM")
    Rm = k.tile([96, 96], BF16, "RmM")
    gn = k.tile([128, 5], F32, "mla_g")
    k.dma("sp", cosT.all, dm(dr["rope_m_cos"]))
    k.dma("sp", sinT.all, dm(dr["rope_m_sin"]))
    k.dma(SW, Rm.all, dm(dr["rope_m_R"]))
    k.dma("sp", gn.all, dm(dr["mla_gains"]))
    wqa = k.tile([128, 8, 256], BF16, "wqa")
    wkva = k.tile([128, 8, 128], BF16, "wkva")
    wrp = k.tile([128, 8, 96], BF16, "wrope_pad")
    qb = k.tile([128, 2, 768], BF16, "q_b")
    kvbn = k.tile([128, 8, 96], BF16, "kvb_nope_pad")
    kvbv = k.tile([128, 8, 64], BF16, "kvb_v")
    k.dma(SW, wqa.all, dm(win[:, :, 1536:1792]))
    k.dma(SW, wkva.all, dm(win[:, :, 1792:1920]))
    k.memset("dve", wrp.all, 0.0)
    k.dma(SW, wrp[:, :, 64:96], dm(win[:, :, 1920:1952]))
    k.dma(SW, qb.all, dm(dr["mla_q_b"].rearrange("(kc p) n -> p kc n", p=128)))
    kvb3 = dr["mla_kv_b"].rearrange("k (h c) -> k h c", h=8)
    k.memset("dve", kvbn.all, 0.0)
    k.dma(SW, kvbn[:, :, 0:64], dm(kvb3[:, :, 0:64]))
    k.dma(SW, kvbv.all, dm(kvb3[:, :, 64:128]))
    qa = k.tile([128, 2, NLAT], BF16, "qa")
    kva = k.tile([128, T], BF16, "kva")
    tl = (k.tile([128, 512], BF16, "n_sq"), k.tile([128, 512], F32, "n_rstd"), k.tile([128, 512], F32, "n_qn"),
          k.tile([128, 512], F32, "n_t1"))
    sq2 = k.tile([128, 2, 512], BF16, "sq2")
    raw = k.tile([128, 2, 512], F32, "qa_raw")
    for bi, (c0, ncl, s) in enumerate(LATB):
        for m in range(2):
            b = 4 + m
            for kk in range(8):
                k.mm(k.ps(b, c1=ncl), wqa[:, kk, m * 128:(m + 1) * 128], hbf[:, kk, c0:c0 + ncl],
                     start=(kk == 0), stop=(kk == 7))
            k.act(sq2[:, m, 0:ncl], k.ps(b, c1=ncl), AF.Square)
            k.copy("act", raw[:, m, 0:ncl], k.ps(b, c1=ncl))
        for m in range(2):
            k.mm(k.ps(6, c1=ncl), ones_b.all, sq2[:, m, 0:ncl], start=(m == 0), stop=(m == 1))
        rstd = tl[1]
        k.act(rstd[:, 0:ncl], k.ps(6, c1=ncl), AF.Sqrt, bias=EPS, scale=1.0 / 256)
        k.recip(rstd[:, 0:ncl], rstd[:, 0:ncl])
        for m in range(2):
            k.stt("dve", qa[:, m, c0 - NCTX:c0 - NCTX + ncl], raw[:, m, 0:ncl], gn[:, m:m + 1], rstd[:, 0:ncl],
                  ALU.mult, ALU.mult)
    for bi, (c0, ncl, s) in enumerate(ALLB):
        b = 4 + bi % 2
        for kk in range(8):
            k.mm(k.ps(b, c1=ncl), wkva[:, kk, :], hbf[:, kk, c0:c0 + ncl], start=(kk == 0), stop=(kk == 7))
        sq, rstd = tl[0], tl[1]
        k.act(sq[:, 0:ncl], k.ps(b, c1=ncl), AF.Square)
        k.mm(k.ps(6, c1=ncl), ones_b.all, sq[:, 0:ncl])
        k.act(rstd[:, 0:ncl], k.ps(6, c1=ncl), AF.Sqrt, bias=EPS, scale=1.0 / 128)
        k.recip(rstd[:, 0:ncl], rstd[:, 0:ncl])
        k.stt("dve", kva[:, c0:c0 + ncl], k.ps(b, c1=ncl), gn[:, 2:3], rstd[:, 0:ncl], ALU.mult, ALU.mult)
    k.free(sq2, raw)
    kh = k.tile([96, T], BF16, "kh")
    qh = k.tile([96, T], BF16, "qh")
    vaug = k.tile([128, 18, 192], BF16, "vaug")
    etl = [k.tile([128, 512], BF16, f"e{i}") for i in range(4)]
    rdt = [(k.tile([128, 512], F32, "rd"), k.tile([64, 512], F32, "rs"))]
    k.memset("pool", vaug.all, 1.0)
    onesv = ones_b[0:96, 0:96]
    scale = 96 ** -0.5
    bi = 0
    for h in range(8):
        for (c0, ncl, s) in ALLB:
            b = 4 + bi % 2
            bi += 1
            for kk in range(8):
                k.mm(k.ps(b, rows=96, c1=ncl), wrp[:, kk, :], hbf[:, kk, c0:c0 + ncl], start=(kk == 0), stop=False)
            k.mm(k.ps(b, rows=96, c1=ncl), kvbn[:, h, :], kva[:, c0:c0 + ncl], start=False, stop=True)
            qk_norm_rope(k.ps(b, rows=96, c1=ncl), 96, gn[0:96, 4:5], onesv, cosT, sinT, Rm.all,
                         kh[0:96, c0:c0 + ncl], c0, ncl, tl, bi, (6, 7))
        for (c0, ncl, s) in LATB:
            b = 4 + bi % 2
            bi += 1
            for m in range(2):
                k.mm(k.ps(b, rows=96, c1=ncl), qb[:, m, h * 96:(h + 1) * 96], qa[:, m, c0 - NCTX:c0 - NCTX + ncl],
                     start=(m == 0), stop=(m == 1))
            qk_norm_rope(k.ps(b, rows=96, c1=ncl), 96, gn[0:96, 3:4], onesv, cosT, sinT, Rm.all,
                         qh[0:96, c0:c0 + ncl], c0, ncl, tl, bi, (6, 7))
        for tc in range(18):
            b = 4 + tc % 2
            k.mm(k.ps(b, c1=64), kva[:, tc * 128:(tc + 1) * 128], kvbv[:, h, :])
            k.copy("dve", vaug[:, tc, 64:128], k.ps(b, c1=64))
        attend(qh, kh, vaug, 96, [(NCTX + 512 * i, 512) for i in range(4)], 18, scale, ycat,
               (h % 2) * 64, h // 2, etl, rdt)
    k.free(cosT, sinT, Rm, gn, wqa, wkva, wrp, qb, kvbn, kvbv, qa, kva, *tl, kh, qh, vaug, *etl, *rdt[0])
    project_out(1, ycat, dr["cd_w_out"], 512, LATB)
    if SUB == 1:
        return

    hp = k.tile([64, 3], F32, "hy_p")
    k.dma("sp", hp.all, dm(dr["hy_p"]))
    zT = k.tile([33, NLAT], BF16, "zT")
    k.dma(SW, zT.all, dm(dr["hy_zT"]))
    fw1 = k.tile([33, 64], BF16, "fw1")
    fw2 = k.tile([64, 64], BF16, "fw2")
    k.dma(SW, fw1.all, dm(dr["hy_filt_w1"]))
    k.dma(SW, fw2.all, dm(dr["hy_filt_w2"]))
    h1 = k.tile([64, NLAT], BF16, "h1T")
    h2 = k.tile([64, NLAT], BF16, "h2T")
    ft = [k.tile([64, 512], F32, f"ft{i}") for i in range(2)]
    MAGIC = 12582912.0
    TWO_PI = 2.0 * math.pi

    def sin_layer(dst, lhsT, src, bcol):
        for i in range(4):
            cs = slice(i * 512, (i + 1) * 512)
            b = 4 + i % 2
            k.mm(k.ps(b, rows=64), lhsT, src[:, cs])
            y, t_ = ft
            k.ts("dve", y.all, k.ps(b, rows=64), hp[:, bcol:bcol + 1], hp[:, 2:3], ALU.add, ALU.mult)
            k.ts("dve", t_.all, y.all, 1.0 / TWO_PI, MAGIC, ALU.mult, ALU.add)
            k.ts("dve", t_.all, t_.all, -MAGIC, -TWO_PI, ALU.add, ALU.mult)
            k.tt("dve", y.all, y.all, t_.all, ALU.add)
            k.act(dst[:, cs], y.all, AF.Sin)

    sin_layer(h1, fw1.all, zT, 0)
    sin_layer(h2, fw2.all, h1, 1)
    k.free(zT, fw1, fw2, h1, *ft, hp)

    CH = 256
    vt = k.tile([128, 16, 512], BF16, "hy_v")
    x1 = k.tile([128, 16, 512], BF16, "hy_x1")
    x2 = k.tile([128, 16, 512], BF16, "hy_x2")
    hpadL = k.tile([128, 8, 129], BF16, "hpadL")
    hpadR = k.tile([128, 8, 129], BF16, "hpadR")
    k.memset("dve", hpadL.all, 0.0)
    k.memset("dve", hpadR.all, 0.0)
    k.copy("pool", hpadL[:, :, 1:129], hbf[:, :, NCTX:NCTX + 128])
    k.copy("pool", hpadR[:, :, 0:127], hbf[:, :, T - 127:T])
    wf = k.tile([128, 8, CH], BF16, "hy_wf")
    wj = [k.tile([128, 8, CH], BF16, f"hy_wj{j}") for j in range(3)]
    cwb = k.tile([128, 3, CH], F32, "hy_cwb")
    bb = k.tile([128, CH], F32, "hy_bias")
    convw = dr["hy_conv_w"]
    for half in range(2):
        hs = slice(half * CH, (half + 1) * CH)
        for part, dst in enumerate((vt, x1, x2)):
            cbase = part * 512 + half * CH
            k.dma(SW, wf.all, dm(win[:, :, cbase:cbase + CH]))
            k.dma("sp", cwb.all, dm(convw[:, cbase:cbase + CH].partition_broadcast(128)))
            k.dma("sp", bb.all, dm(dr["hy_conv_b"][cbase:cbase + CH].partition_broadcast(128)))
            for j in range(3):
                for kk in range(8):
                    k.tt("dve" if kk % 2 else "pool", wj[j][:, kk, :], wf[:, kk, :], cwb[:, j, :], ALU.mult)
            for tcn in range(16):
                b = 4 + tcn % 2
                n_mm = 0
                for j in range(3):
                    for kk in range(8):
                        sh = j - 1
                        if tcn == 0 and sh == -1:
                            lhs = hpadL[:, kk, 0:128]
                        elif tcn == 15 and sh == 1:
                            lhs = hpadR[:, kk, 0:128]
                        else:
                            c0 = NCTX + tcn * 128 + sh
                            lhs = hbf[:, kk, c0:c0 + 128]
                        k.mm(k.ps(b, c1=CH), lhs, wj[j][:, kk, :], start=(n_mm == 0), stop=(n_mm == 23))
                        n_mm += 1
                k.tt("dve", dst[:, tcn, hs], k.ps(b, c1=CH), bb.all, ALU.add)
    k.free(hpadL, hpadR, wf, *wj, cwb, bb)
    carve = [hbf.off]

    def ctile(shape, dt, name):
        t_ = Tile(k, carve[0], shape, dt, name)
        carve[0] += ((t_.nbytes + PAGE - 1) // PAGE) * PAGE
        assert carve[0] <= hbf.off + hbf.alloc
        return t_

    hsum = ctile([128, 16, CH], BF16, "hsum")
    hdif = ctile([128, 16, CH], BF16, "hdif")
    Yre = ctile([128, 16, CH], BF16, "Yre")
    Yim = ctile([128, 16, CH], BF16, "Yim")
    tmps = [ctile([128, 512], F32, "hyt0"), ctile([128, 512], F32, "hyt1"),
            k.tile([128, 512], F32, "hyt2"), k.tile([128, 512], F32, "hyt3")]
    dftA = [k.tile([128, 16, 128], BF16, f"dftA{i}") for i in range(2)]
    dftB = [k.tile([128, 16, 128], BF16, f"dftB{i}") for i in range(2)]
    w3 = k.tile([64, 512], BF16, "fw3")
    skb = k.tile([128, CH], F32, "hy_skipb")
    winr = [k.tile([128, CH], F32, f"hywin{i}") for i in range(2)]
    for half in range(2):
        hs = slice(half * CH, (half + 1) * CH)
        for o in range(2):
            xo = x1 if o == 0 else x2
            for dr_ in range(2):
                cb_ = o * 1024 + dr_ * 512 + half * CH
                k.dma(SW, w3[:, dr_ * CH:(dr_ + 1) * CH], dm(dr["hy_filt_w3"][:, cb_:cb_ + CH]))
            k.dma("sp", skb.all, dm(dr["hy_skip"][o, half * CH:(half + 1) * CH].partition_broadcast(128)))
            for tcn in range(16):
                b = 4 + tcn % 2
                wn = winr[tcn % 2]
                k.dma("sp", wn.all, dm(dr["hy_window"][tcn * 128:(tcn + 1) * 128, half * CH:(half + 1) * CH]))
                k.mm(k.ps(b), h2[0:64, tcn * 128:(tcn + 1) * 128], w3.all)
                hf_, hb_ = tmps[0], tmps[1]
                k.tt("dve", hf_[:, 0:CH], k.ps(b, c0=0, c1=CH), wn.all, ALU.mult)
                k.tt("dve", hb_[:, 0:CH], k.ps(b, c0=CH, c1=2 * CH), wn.all, ALU.mult)
                k.tt("pool", hsum[:, tcn, :], hf_[:, 0:CH], hb_[:, 0:CH], ALU.add)
                k.tt("pool", hdif[:, tcn, :], hb_[:, 0:CH], hf_[:, 0:CH], ALU.subtract)
                if tcn == 0:
                    k.copy("pool", hsum[0:1, 0, :], hf_[0:1, 0:CH])
            for fc in range(16):
                ca, sa = dftA[fc % 2], dftB[fc % 2]
                k.dma("sp", V(ca.ap.rearrange("p a b -> p (a b)"), ca.all.pg), dm(dr["dft_cf"][fc]))
                k.dma("sp", V(sa.ap.rearrange("p a b -> p (a b)"), sa.all.pg), dm(dr["dft_sf"][fc]))
                bx, by = 4 + (fc % 2) * 2, 5 + (fc % 2) * 2
                for tcn in range(16):
                    k.mm(k.ps(bx, c0=0, c1=CH), ca[:, tcn, :], vt[:, tcn, hs], start=(tcn == 0), stop=(tcn == 15))
                for tcn in range(16):
                    k.mm(k.ps(bx, c0=CH, c1=2 * CH), sa[:, tcn, :], vt[:, tcn, hs], start=(tcn == 0), stop=(tcn == 15))
                for tcn in range(16):
                    k.mm(k.ps(by, c0=0, c1=CH), ca[:, tcn, :], hsum[:, tcn, :], start=(tcn == 0), stop=(tcn == 15))
                for tcn in range(16):
                    k.mm(k.ps(by, c0=CH, c1=2 * CH), sa[:, tcn, :], hdif[:, tcn, :], start=(tcn == 0), stop=(tcn == 15))
                kt, t1, t2 = tmps[2], tmps[0], tmps[1]
                k.copy("act", kt.all, k.ps(by))
                A_ = k.ps(bx, c0=0, c1=CH)
                B_ = k.ps(bx, c0=CH, c1=2 * CH)
                k.tt("dve", t1[:, 0:CH], A_, kt[:, 0:CH], ALU.mult)
                k.tt("dve", t1[:, CH:2 * CH], B_, kt[:, CH:2 * CH], ALU.mult)
                k.tt("pool", Yre[:, fc, :], t1[:, 0:CH], t1[:, CH:2 * CH], ALU.add)
                k.tt("dve", t2[:, 0:CH], A_, kt[:, CH:2 * CH], ALU.mult)
                k.tt("dve", t2[:, CH:2 * CH], B_, kt[:, 0:CH], ALU.mult)
                k.tt("pool", Yim[:, fc, :], t2[:, 0:CH], t2[:, CH:2 * CH], ALU.subtract)
            for tcn in range(16):
                ca, sa = dftA[tcn % 2], dftB[tcn % 2]
                k.dma("sp", V(ca.ap.rearrange("p a b -> p (a b)"), ca.all.pg), dm(dr["dft_ci"][tcn]))
                k.dma("sp", V(sa.ap.rearrange("p a b -> p (a b)"), sa.all.pg), dm(dr["dft_si"][tcn]))
                b = 4 + tcn % 2
                for fc in range(16):
                    k.mm(k.ps(b, c1=CH), ca[:, fc, :], Yre[:, fc, :], start=(fc == 0), stop=False)
                for fc in range(16):
                    k.mm(k.ps(b, c1=CH), sa[:, fc, :], Yim[:, fc, :], start=False, stop=(fc == 15))
                t1 = tmps[3]
                k.tt("pool", t1[:, 0:CH], vt[:, tcn, hs], skb.all, ALU.mult)
                k.tt("dve", t1[:, 0:CH], k.ps(b, c1=CH), t1[:, 0:CH], ALU.add)
                k.tt("dve", vt[:, tcn, hs], t1[:, 0:CH], xo[:, tcn, hs], ALU.mult)
    for tcn in range(16):
        b = 4 + tcn % 2
        for cc in range(4):
            k.transpose(ps_bf(k, b, 128, cc * 128, (cc + 1) * 128), vt[:, tcn, cc * 128:(cc + 1) * 128], ident_b.all)
        for cc in range(4):
            k.copy("dve", ycat[:, cc, NCTX + tcn * 128:NCTX + (tcn + 1) * 128],
                   ps_bf(k, b, 128, cc * 128, (cc + 1) * 128))
    k.free(vt, x1, x2, *dftA, *dftB, w3, skb, tmps[2], tmps[3], *winr, h2)
    project_out(1, ycat, dr["cd_w_out"], 0, LATB)


def moe_layer(k, dr, xs, hbf, ycat, ident_f, ones_b, M_, A_, norm_mod, ffn, LATB):
    dm = k.dram
    AX = mybir.AxisListType.X
    rw = k.tile([128, 8, 8], F32, "rw")
    r_hi = k.tile([128, 8, 8], BF16, "r_hi")
    r_lo = k.tile([128, 8, 8], BF16, "r_lo")
    k.dma("sp", rw.all, dm(dr["moe_router"].rearrange("(kc p) e -> p kc e", p=128)))
    k.copy("dve", r_hi.all, rw.all)
    k.tt("dve", r_lo.all, rw.all, r_hi.all, ALU.subtract)
    logits = k.tile([128, 16, 8], F32, "logits")
    gates = k.tile([128, 16, 8], F32, "gates")
    hlo = k.tile([128, 8, 512], BF16, "hlo")
    hf32 = [k.tile([128, 512], F32, f"hf32_{i}") for i in range(2)]

    def hook(bi, c0, ncl, kk, tmp, s):
        hf = hf32[kk % 2]
        k.act(hf[:, 0:ncl], tmp[:, 0:ncl], AF.Identity, bias=M_(1, 3, kk, s), scale=A_(1, 1, kk, s))
        k.tt("pool", hlo[:, kk, 0:ncl], hf[:, 0:ncl], hbf[:, kk, c0:c0 + ncl], ALU.subtract)
        if kk == 7:
            for q in range(ncl // 128):
                tcn = (c0 - NCTX) // 128 + q
                b = 6 + tcn % 2
                n = 0
                for which in range(3):
                    for k2 in range(8):
                        if which == 1:
                            lhs = hlo[:, k2, q * 128:(q + 1) * 128]
                        else:
                            lhs = hbf[:, k2, c0 + q * 128:c0 + (q + 1) * 128]
                        rr = r_lo if which == 2 else r_hi
                        k.mm(k.ps(b, c1=8), lhs, rr[:, k2, :], start=(n == 0), stop=(n == 23))
                        n += 1
                k.copy("dve", logits[:, tcn, :], k.ps(b, c1=8))

    norm_mod(1, 1, LATB, f32_hook=hook)
    k.free(hlo, *hf32, rw, r_hi, r_lo)
    mx = k.tile([128, 8], F32, "mx")
    sel = k.tile([128, 8], F32, "sel")
    ex = k.tile([128, 8], F32, "ex")
    sm = k.tile([128, 4], F32, "sm")
    for tcn in range(16):
        lg = logits[:, tcn, :]
        k.op("dve", (lambda o_, i_: (lambda e_: e_.max(out=o_, in_=i_)))(mx.ap, lg.ap), R=[lg], W=[mx.all])
        k.ts("dve", sel.all, lg, mx[:, 1:2], None, ALU.is_ge)
        k.ts("dve", sm[:, 0:1], mx[:, 0:1], -1.0, None, ALU.mult)
        k.act(ex.all, lg, AF.Exp, bias=sm[:, 0:1])
        k.tt("dve", ex.all, ex.all, sel.all, ALU.mult)
        k.op("dve", (lambda o_, i_: (lambda e_: e_.reduce_sum(out=o_, in_=i_, axis=AX)))(sm[:, 1:2].ap, ex.ap),
             R=[ex.all], W=[sm[:, 1:2]])
        k.recip(sm[:, 2:3], sm[:, 1:2])
        k.ts("dve", gates[:, tcn, :], ex.all, sm[:, 2:3], None, ALU.mult)
    k.free(mx, sel, ex, sm)
    Gbs = [k.tile([128, NLAT], F32, f"Gb{i}") for i in range(2)]
    Dt = k.tile([128, 128], F32, "Dt")
    Dhi = k.tile([128, 128], BF16, "Dhi")
    Dlo = k.tile([128, 128], BF16, "Dlo")
    for e in range(8):
        Gb = Gbs[e % 2]
        for tcn in range(16):
            b = 6 + tcn % 2
            k.ts("dve", Dt.all, ident_f.all, gates[:, tcn, e:e + 1], None, ALU.mult)
            k.copy("dve", Dhi.all, Dt.all)
            k.tt("dve", Dlo.all, Dt.all, Dhi.all, ALU.subtract)
            k.mm(k.ps(b, c1=128), ones_b.all, Dhi.all, start=True, stop=False)
            k.mm(k.ps(b, c1=128), ones_b.all, Dlo.all, start=False, stop=True)
            k.copy("act", Gb[:, tcn * 128:(tcn + 1) * 128], k.ps(b, c1=128))
        ffn(1, dr["moe_w1"][e], dr["moe_w3"][e], dr["moe_w2"][e], 3584, LATB, ycat, gate_bc=Gb)
    k.free(*Gbs, Dt, Dhi, Dlo, logits, gates)
```

```python
import contextlib
import math
import numpy as np
import ml_dtypes
import concourse.bass as bass
import concourse.mybir as mybir
from concourse.bass_utils import run_bass_kernel_spmd

F32 = mybir.dt.float32
BF16 = mybir.dt.bfloat16
U8 = mybir.dt.uint8
AF = mybir.ActivationFunctionType
ALU = mybir.AluOpType

ENGS = ("pe", "act", "dve", "pool", "sp")
N_DMA_SEMS = 16
DMA_POOLS = {"sp": list(range(0, 7)), "pool": list(range(7, 14)), "act": [14, 15]}
PAGE = 256
ARENA = 207 * 1024
PSUM_PAGE0 = 100000
DRAM_PAGE0 = 200000

D = 1024
T = 2304
NCTX = 256
NLAT = 2048
EPS = 1e-6


class Op:
    __slots__ = ("eng", "fn", "dma", "preds", "sig", "sigval", "dsem", "dval", "idx")


class Prog:
    def __init__(self, nc):
        self.nc = nc
        self.ops = []
        self.pw = {}
        self.pr = {}

    def add(self, eng, fn, reads=(), writes=(), dma=False):
        op = Op()
        op.eng = eng
        op.fn = fn
        op.dma = dma
        op.idx = len(self.ops)
        op.sig = False
        preds = set()
        pw, pr = self.pw, self.pr
        for (lo, hi) in reads:
            for p in range(lo, hi):
                w = pw.get(p)
                if w is not None:
                    preds.add(w)
        for (lo, hi) in writes:
            for p in range(lo, hi):
                w = pw.get(p)
                if w is not None:
                    preds.add(w)
                r = pr.get(p)
                if r:
                    preds.update(r.values())
        rkey = ("d", op.idx) if dma else eng
        for (lo, hi) in reads:
            for p in range(lo, hi):
                r = pr.get(p)
                if r is None:
                    pr[p] = {rkey: op.idx}
                else:
                    r[rkey] = op.idx
        for (lo, hi) in writes:
            for p in range(lo, hi):
                pw[p] = op.idx
                pr[p] = None
        preds.discard(op.idx)
        op.preds = preds
        self.ops.append(op)
        return op

    def emit(self):
        nc = self.nc
        ops = self.ops
        with contextlib.ExitStack() as es:
            esem = {e: es.enter_context(nc.semaphore(f"s_{e}")) for e in ENGS}
            dsems = [es.enter_context(nc.semaphore(f"s_dma{i}")) for i in range(N_DMA_SEMS)]
            waits = [None] * len(ops)
            for op in ops:
                per_eng = {}
                dma_w = []
                for p in op.preds:
                    po = ops[p]
                    if po.dma:
                        dma_w.append(p)
                    else:
                        if po.eng == "pe" and op.eng == "pe" and not op.dma:
                            continue
                        if po.eng not in per_eng or per_eng[po.eng] < p:
                            per_eng[po.eng] = p
                for p in per_eng.values():
                    ops[p].sig = True
                waits[op.idx] = (list(per_eng.values()), dma_w)
            cnt = {e: 0 for e in ENGS}
            dcnt = [0] * N_DMA_SEMS
            dlast = [None] * N_DMA_SEMS
            dpos = {e: 0 for e in ENGS}
            dma_prev = {}
            for op in ops:
                if op.dma:
                    rng = DMA_POOLS[op.eng]
                    k = rng[dpos[op.eng] % len(rng)]
                    dpos[op.eng] += 1
                    dcnt[k] += 16
                    op.dsem = k
                    op.dval = dcnt[k]
                    dma_prev[op.idx] = dlast[k]
                    dlast[k] = op.idx
                elif op.sig:
                    cnt[op.eng] += 1
                    op.sigval = cnt[op.eng]
            streams = {e: [o for o in ops if o.eng == e] for e in ENGS}
            self.stats = {e: len(streams[e]) for e in ENGS}
            self.stats["sig"] = dict(cnt)

            def run_stream(e, eng):
                known = {}

                def wait(key, sem, val):
                    if known.get(key, 0) >= val:
                        return
                    known[key] = val
                    eng.wait_ge(sem, val)

                for op in streams[e]:
                    cw, dw = waits[op.idx]
                    for p in cw:
                        po = ops[p]
                        wait(po.eng, esem[po.eng], po.sigval)
                    for p in dw:
                        po = ops[p]
                        wait(("d", po.dsem), dsems[po.dsem], po.dval)
                    if op.dma:
                        pp = dma_prev[op.idx]
                        if pp is not None:
                            po = ops[pp]
                            wait(("d", po.dsem), dsems[po.dsem], po.dval)
                        ins = op.fn(eng)
                        ins.then_inc(dsems[op.dsem], 16)
                    else:
                        ins = op.fn(eng)
                        if op.sig:
                            ins.then_inc(esem[e], 1)
                if e == "sp":
                    for k in range(N_DMA_SEMS):
                        if dcnt[k]:
                            eng.wait_ge(dsems[k], dcnt[k])

            with nc.Block() as block:
                @block.tensor
                def _(eng):
                    run_stream("pe", eng)

                @block.scalar
                def _(eng):
                    run_stream("act", eng)

                @block.vector
                def _(eng):
                    run_stream("dve", eng)

                @block.gpsimd
                def _(eng):
                    run_stream("pool", eng)

                @block.sync
                def _(eng):
                    run_stream("sp", eng)


class V:
    __slots__ = ("ap", "pg")

    def __init__(self, ap, pg):
        self.ap = ap
        self.pg = pg

    def m(self, f):
        return V(f(self.ap), self.pg)


class Tile:
    def __init__(self, k, off, shape, dt, name):
        self.k = k
        self.off = off
        self.shape = list(shape)
        self.dt = dt
        self.es = 4 if dt == F32 else (2 if dt == BF16 else 1)
        n = 1
        for s in shape[1:]:
            n *= s
        self.n = n
        self.nbytes = n * self.es
        base = k.arena[0:shape[0], off:off + self.nbytes]
        if dt != U8:
            base = base.bitcast(dt)
        if len(shape) == 3:
            base = base.rearrange("p (a b) -> p a b", a=shape[1])
        elif len(shape) == 4:
            base = base.rearrange("p (a b c) -> p a b c", a=shape[1], b=shape[2])
        self.ap = base
        self.name = name

    def _pages(self, lo_e, hi_e):
        lo = self.off + lo_e * self.es
        hi = self.off + hi_e * self.es
        return (lo // PAGE, (hi + PAGE - 1) // PAGE)

    @property
    def all(self):
        return V(self.ap, [self._pages(0, self.n)])

    def __getitem__(self, idx):
        if not isinstance(idx, tuple):
            idx = (idx,)
        ap = self.ap[idx]
        fidx = list(idx[1:]) + [slice(None)] * (len(self.shape) - len(idx))
        dims = self.shape[1:]
        rng = []
        for dim, ix in zip(dims, fidx):
            if isinstance(ix, int):
                rng.append((ix, ix + 1))
            else:
                a = 0 if ix.start is None else ix.start
                b = dim if ix.stop is None else ix.stop
                rng.append((a, b))
        strides = []
        st = 1
        for dim in reversed(dims):
            strides.insert(0, st)
            st *= dim
        lead = rng[:-1]
        cnt = 1
        for a, b in lead:
            cnt *= (b - a)
        pages = []
        if cnt <= 64:
            import itertools
            for combo in itertools.product(*[range(a, b) for a, b in lead]):
                base = sum(c * s_ for c, s_ in zip(combo, strides[:-1]))
                pages.append(self._pages(base + rng[-1][0], base + rng[-1][1]))
        else:
            lo = sum(a * s_ for (a, b), s_ in zip(rng, strides))
            hi = sum((b - 1) * s_ for (a, b), s_ in zip(rng, strides)) + 1
            pages.append(self._pages(lo, hi))
        return V(ap, pages)


def pgs(vs):
    out = []
    for v in vs:
        out.extend(v.pg)
    return out


class K:
    def __init__(self, nc, es):
        self.nc = nc
        self.P = Prog(nc)
        self.arena_t = es.enter_context(nc.sbuf_tensor("arena", [128, ARENA], U8))
        self.arena = self.arena_t
        self.free_list = [(0, ARENA)]
        self.banks = [es.enter_context(nc.psum_tensor(f"bank{i}", [128, 512], F32)) for i in range(8)]
        self.bank_i = 0
        self.dram_pg = DRAM_PAGE0

    def tile(self, shape, dt, name="t"):
        es_ = 4 if dt == F32 else (2 if dt == BF16 else 1)
        n = 1
        for s in shape[1:]:
            n *= s
        nb = ((n * es_ + PAGE - 1) // PAGE) * PAGE
        for i, (o, sz) in enumerate(self.free_list):
            if sz >= nb:
                if sz == nb:
                    self.free_list.pop(i)
                else:
                    self.free_list[i] = (o + nb, sz - nb)
                t = Tile(self, o, shape, dt, name)
                t.alloc = nb
                return t
        raise RuntimeError(f"SBUF arena full allocating {name} {shape} ({nb}B); free={self.free_list}")

    def free(self, *tiles):
        for t in tiles:
            self.free_list.append((t.off, t.alloc))
        self.free_list.sort()
        merged = []
        for o, sz in self.free_list:
            if merged and merged[-1][0] + merged[-1][1] == o:
                merged[-1] = (merged[-1][0], merged[-1][1] + sz)
            else:
                merged.append((o, sz))
        self.free_list = merged

    def ps(self, i, rows=128, c0=0, c1=512, r0=0):
        b = self.banks[i]
        return V(b[r0:r0 + rows, c0:c1], [(PSUM_PAGE0 + i, PSUM_PAGE0 + i + 1)])

    def ps3(self, i, a, rows=128):
        b = self.banks[i]
        return V(b[0:rows, :].rearrange("p (a b) -> p a b", a=a), [(PSUM_PAGE0 + i, PSUM_PAGE0 + i + 1)])

    def dram(self, ap):
        self.dram_pg += 1
        return V(ap, [(self.dram_pg, self.dram_pg + 1)])

    def op(self, eng, fn, R=(), W=()):
        self.P.add(eng, fn, pgs(R), pgs(W))

    def dma(self, eng, out, in_):
        self.P.add(eng, lambda e: e.dma_start(out=out.ap, in_=in_.ap), pgs([in_]), pgs([out]), dma=True)

    def mm(self, out, lhsT, rhs, start=True, stop=True):
        self.P.add("pe", lambda e: e.matmul(out.ap, lhsT=lhsT.ap, rhs=rhs.ap, start=start, stop=stop),
                   pgs([lhsT, rhs]), pgs([out]))

    def transpose(self, out, in_, ident):
        self.P.add("pe", lambda e: e.transpose(out.ap, in_.ap, ident.ap), pgs([in_, ident]), pgs([out]))

    def act(self, out, in_, func, bias=None, scale=None):
        R = [in_]
        kw = {}
        if bias is not None:
            if isinstance(bias, V):
                R.append(bias)
                kw["bias"] = bias.ap
            else:
                kw["bias"] = float(bias)
        if scale is not None:
            if isinstance(scale, V):
                R.append(scale)
                kw["scale"] = scale.ap
            else:
                kw["scale"] = float(scale)
        self.P.add("act", lambda e: e.activation(out=out.ap, in_=in_.ap, func=func, **kw),
                   pgs(R), pgs([out]))

    def copy(self, eng, out, in_):
        if eng == "act":
            self.P.add("act", lambda e: e.copy(out=out.ap, in_=in_.ap), pgs([in_]), pgs([out]))
        else:
            self.P.add(eng, lambda e: e.tensor_copy(out=out.ap, in_=in_.ap), pgs([in_]), pgs([out]))

    def tt(self, eng, out, in0, in1, op):
        self.P.add(eng, lambda e: e.tensor_tensor(out=out.ap, in0=in0.ap, in1=in1.ap, op=op),
                   pgs([in0, in1]), pgs([out]))

    def ts(self, eng, out, in0, s1, s2, op0, op1=None):
        R = [in0]
        a1 = s1.ap if isinstance(s1, V) else float(s1)
        if isinstance(s1, V):
            R.append(s1)
        if s2 is None:
            self.P.add(eng, lambda e: e.tensor_scalar(out=out.ap, in0=in0.ap, scalar1=a1, scalar2=None, op0=op0),
                       pgs(R), pgs([out]))
            return
        a2 = s2.ap if isinstance(s2, V) else float(s2)
        if isinstance(s2, V):
            R.append(s2)
        self.P.add(eng, lambda e: e.tensor_scalar(out=out.ap, in0=in0.ap, scalar1=a1, scalar2=a2, op0=op0, op1=op1),
                   pgs(R), pgs([out]))

    def stt(self, eng, out, in0, scalar, in1, op0, op1):
        R = [in0, in1]
        a = scalar.ap if isinstance(scalar, V) else float(scalar)
        if isinstance(scalar, V):
            R.append(scalar)
        self.P.add(eng, lambda e: e.scalar_tensor_tensor(out=out.ap, in0=in0.ap, scalar=a, in1=in1.ap, op0=op0, op1=op1),
                   pgs(R), pgs([out]))

    def memset(self, eng, out, val):
        self.P.add(eng, lambda e: e.memset(out.ap, val), [], pgs([out]))

    def recip(self, out, in_):
        self.P.add("dve", lambda e: e.reciprocal(out=out.ap, in_=in_.ap), pgs([in_]), pgs([out]))

    def scan(self, out, d0, d1, initial):
        R = [d0, d1]
        a = initial.ap if isinstance(initial, V) else float(initial)
        if isinstance(initial, V):
            R.append(initial)
        self.P.add("dve", lambda e: e.tensor_tensor_scan(out=out.ap, data0=d0.ap, data1=d1.ap, initial=a,
                                                       op0=ALU.mult, op1=ALU.add),
                   pgs(R), pgs([out]))


def col_blocks(ctx=True, lat=True):
    b = []
    if ctx:
        b.append((0, NCTX, 1))
    if lat:
        for i in range(4):
            b.append((NCTX + 512 * i, 512, 0))
    return b


def fm(v):
    v = np.asarray(v)
    n = v.shape[-1] // 128
    return np.ascontiguousarray(v.reshape(v.shape[:-1] + (n, 128)).swapaxes(-1, -2))


def rope_tables(rot_dim, nfeat, rot_off):
    n_freq = rot_dim // 4
    inv_freq = (10000.0 ** (-np.arange(n_freq, dtype=np.float32) / n_freq)).astype(np.float32)
    t = np.arange(NLAT)
    row = (t // 64).astype(np.float32)
    col = (t % 64).astype(np.float32)
    ang = np.concatenate([row[:, None] * inv_freq, col[:, None] * inv_freq], axis=-1).astype(np.float32)
    cos = np.cos(ang).astype(np.float32)
    sin = np.sin(ang).astype(np.float32)
    C = np.ones((nfeat, T), np.float32)
    S = np.zeros((nfeat, T), np.float32)
    half = rot_dim // 2
    for f in range(rot_off, nfeat):
        i = (f - rot_off) % half
        C[f, NCTX:] = cos[:, i]
        S[f, NCTX:] = sin[:, i]
    R = np.zeros((nfeat, nfeat), np.float32)
    for i in range(half):
        R[rot_off + i + half, rot_off + i] = -1.0
        R[rot_off + i, rot_off + i + half] = 1.0
    return C, S, R


def host_consts():
    c = {}
    c["ident"] = np.eye(128, dtype=np.float32)
    cg, sg, rg = rope_tables(64, 64, 0)
    c["rope_g_cos"], c["rope_g_sin"], c["rope_g_R"] = cg, sg, rg
    cm, sm, rm = rope_tables(32, 96, 64)
    c["rope_m_cos"], c["rope_m_sin"], c["rope_m_R"] = cm, sm, rm
    L = NLAT
    N = 2 * L
    s = np.arange(L, dtype=np.float64)[:, None]
    f = np.arange(L, dtype=np.float64)[None, :]
    th = np.pi * (2 * f + 1) * s / N
    Cm = np.cos(th)
    Sm = np.sin(th)
    bf = ml_dtypes.bfloat16
    def tile_(M):
        return np.ascontiguousarray(M.reshape(16, 128, 16, 128).transpose(2, 1, 0, 3).reshape(16, 128, 2048)).astype(bf)
    c["dft_cf"] = tile_(Cm)
    c["dft_sf"] = tile_(Sm)
    c["dft_ci"] = tile_(Cm.T * (2.0 / N))
    c["dft_si"] = tile_(-Sm.T * (2.0 / N))
    t = np.arange(L, dtype=np.float32)[:, None]
    t_norm = t / max(L - 1, 1)
    bands = np.linspace(1e-4, 16 - 1, 16, dtype=np.float32)
    ang = (2.0 * math.pi * t * bands / L).astype(np.float32)
    z = np.concatenate([t_norm, np.cos(ang), -np.sin(ang)], axis=-1).astype(np.float32)
    c["hy_zT"] = np.ascontiguousarray(z.T)
    HY_MIN = math.log(1e-2) / 1.5
    HY_MAX = math.log(1e-2) / 0.3
    deltas = np.abs(np.linspace(HY_MIN, HY_MAX, 512, dtype=np.float32))
    window = (np.exp(-t_norm * deltas) + 0.05).astype(np.float32)
    c["hy_window"] = window
    return c


def build(shapes, stage=99):
    nc = bass.Bass("TRN2", target_bir_lowering=False)
    dr = {}
    for name, shp in shapes.items():
        dt_ = F32
        if isinstance(shp, tuple) and len(shp) == 2 and isinstance(shp[1], str):
            shp, dts = shp
            dt_ = BF16 if dts == "bfloat16" else F32
        dr[name] = nc.dram_tensor(name, list(shp), dt_, kind="ExternalInput").ap()
    out_d = nc.dram_tensor("out", [NLAT, D], F32, kind="ExternalOutput").ap()
    outc_d = nc.dram_tensor("outc", [NCTX, D], F32, kind="ExternalOutput").ap()
    with contextlib.ExitStack() as es:
        k = K(nc, es)
        _emit_program(k, dr, out_d, outc_d, stage)
        k.P.emit()
    return nc


def _emit_program(k, dr, out_d, outc_d, stage):
    dm = k.dram
    SW = "pool"

    xs = k.tile([128, 8, T], F32, "xs")
    hbf = k.tile([128, 8, T], BF16, "hbf")
    ident_f = k.tile([128, 128], F32, "ident_f")
    ident_b = k.tile([128, 128], BF16, "ident_b")
    ones_b = k.tile([128, 128], BF16, "ones_b")
    mods = k.tile([128, 2, 48, 2], F32, "mods")
    affA = k.tile([128, 2, 2, 16], F32, "affA")
    gdup = k.tile([128, 2, 2, 16], F32, "gdup")
    modb = k.tile([128, 2, 48], F32, "modb")

    k.dma("sp", ident_f.all, dm(dr["ident"]))
    k.copy("dve", ident_b.all, ident_f.all)
    k.memset("dve", ones_b.all, 1.0)
    k.dma("sp", gdup.all, dm(dr["gdup"]))
    k.dma("sp", modb.all, dm(dr["modb"]))

    xts = [k.tile([128, 1024], F32, f"xt{i}") for i in range(2)]
    for tc in range(18):
        src = dr["ctx"][tc * 128:(tc + 1) * 128, :] if tc < 2 else dr["x"][(tc - 2) * 128:(tc - 1) * 128, :]
        xt = xts[tc % 2]
        k.dma("sp", xt.all, dm(src))
        for half in range(2):
            b = (tc * 2 + half) % 4
            for kk in range(4):
                kf = half * 4 + kk
                k.transpose(k.ps(b, c0=kk * 128, c1=(kk + 1) * 128), xt[:, kf * 128:(kf + 1) * 128], ident_f.all)
            k.copy("dve" if half == 0 else "act", xs[:, half * 4:(half + 1) * 4, tc * 128:(tc + 1) * 128], k.ps3(b, 4))
    k.free(*xts)

    cl = k.tile([128, 8], F32, "cl")
    cc = k.tile([128, 8], F32, "cc")
    s_bf = k.tile([128, 8, 2], BF16, "s_bf")
    k.dma("sp", cl.all, dm(dr["c_fm"]))
    k.dma("sp", cc.all, dm(dr["cctx_fm"]))
    k.act(s_bf[:, :, 0], cl.all, AF.Silu)
    k.act(s_bf[:, :, 1], cc.all, AF.Silu)
    mwt = [k.tile([128, 8, 512], BF16, f"mw{i}") for i in range(2)]
    gi = 0
    for l in range(2):
        mw = dr["mod_w"][l].rearrange("(kc p) n -> p kc n", p=128)
        for g in range(12):
            wt = mwt[gi % 2]
            k.dma(SW, wt.all, dm(mw[:, :, g * 512:(g + 1) * 512]))
            b = 4 + gi % 2
            for j in range(4):
                for kk in range(8):
                    k.mm(k.ps(b, c0=2 * j, c1=2 * j + 2), wt[:, kk, j * 128:(j + 1) * 128], s_bf[:, kk, :],
                         start=(kk == 0), stop=(kk == 7))
            for j in range(4):
                ch = g * 4 + j
                k.ts("dve", mods[:, l, ch, :], k.ps(b, c0=2 * j, c1=2 * j + 2), modb[:, l, ch:ch + 1], None, ALU.add)
            gi += 1
    k.free(*mwt, cl, cc, s_bf)
    for l in range(2):
        for n, mi in ((0, 1), (1, 4)):
            src = V(mods.ap[:, l, mi * 8:(mi + 1) * 8, :].rearrange("p a b -> p (a b)"), mods[:, l, mi * 8:(mi + 1) * 8, :].pg)
            k.stt("dve", affA[:, l, n, :], src, 1.0, gdup[:, l, n, :], ALU.add, ALU.mult)

    def A_(l, n, kk, s):
        return affA[:, l, n, kk * 2 + s:kk * 2 + s + 1]

    def M_(l, mi, kk, s):
        return mods[:, l, mi * 8 + kk, s:s + 1]

    def norm_mod(l, n, blocks, f32_hook=None):
        sqs = [k.tile([128, 8, 512], BF16, f"sq{i}") for i in range(2)]
        rstds = [k.tile([128, 512], F32, f"rstd{i}") for i in range(2)]
        tmps = [k.tile([128, 512], F32, f"nt{i}") for i in range(3)]
        ti = 0
        for bi, (c0, ncl, s) in enumerate(blocks):
            sq = sqs[bi % 2]
            rstd = rstds[bi % 2]
            for kk in range(8):
                k.act(sq[:, kk, 0:ncl], xs[:, kk, c0:c0 + ncl], AF.Square)
            b = 4 + bi % 2
            for kk in range(8):
                k.mm(k.ps(b, c1=ncl), ones_b.all, sq[:, kk, 0:ncl], start=(kk == 0), stop=(kk == 7))
            k.act(rstd[:, 0:ncl], k.ps(b, c1=ncl), AF.Sqrt, bias=EPS, scale=1.0 / D)
            k.recip(rstd[:, 0:ncl], rstd[:, 0:ncl])
            for kk in range(8):
                tmp = tmps[ti % 3]
                ti += 1
                k.tt("dve" if kk % 2 == 0 else "pool", tmp[:, 0:ncl], xs[:, kk, c0:c0 + ncl], rstd[:, 0:ncl], ALU.mult)
                k.act(hbf[:, kk, c0:c0 + ncl], tmp[:, 0:ncl], AF.Identity,
                      bias=M_(l, 0 if n == 0 else 3, kk, s), scale=A_(l, n, kk, s))
                if f32_hook is not None:
                    f32_hook(bi, c0, ncl, kk, tmp, s)
        k.free(*sqs, *rstds, *tmps)

    def write_out():
        ots = [k.tile([128, 1024], F32, f"ot{i}") for i in range(2)]
        for tc in range(18):
            ot = ots[tc % 2]
            for half in range(2):
                b = (tc * 2 + half) % 4
                for kk in range(4):
                    kf = half * 4 + kk
                    k.transpose(k.ps(b, c0=kk * 128, c1=(kk + 1) * 128), xs[:, kf, tc * 128:(tc + 1) * 128], ident_f.all)
                k.copy("dve" if half == 0 else "act", ot[:, half * 512:(half + 1) * 512], k.ps(b))
            dst = outc_d[tc * 128:(tc + 1) * 128, :] if tc < 2 else out_d[(tc - 2) * 128:(tc - 1) * 128, :]
            k.dma("sp", dm(dst), ot.all)
        k.free(*ots)

    if stage == 0:
        write_out()
        return

    def qk_norm_rope(psv, nrows, gain, onesv, cosT, sinT, Rm, outv, c0, ncl, tl, bi, banks):
        sq, rstd, qn, t1 = tl
        k.act(sq[0:nrows, 0:ncl], psv, AF.Square)
        k.act(qn[0:nrows, 0:ncl], psv, AF.Identity, scale=gain)
        b1, b2 = banks
        k.mm(k.ps(b1, rows=nrows, c1=ncl), onesv, sq[0:nrows, 0:ncl])
        k.act(rstd[0:nrows, 0:ncl], k.ps(b1, rows=nrows, c1=ncl), AF.Sqrt, bias=EPS, scale=1.0 / nrows)
        k.copy("pool", sq[0:nrows, 0:ncl], qn[0:nrows, 0:ncl])
        k.mm(k.ps(b2, rows=nrows, c1=ncl), Rm, sq[0:nrows, 0:ncl])
        k.recip(rstd[0:nrows, 0:ncl], rstd[0:nrows, 0:ncl])
        k.tt("pool", t1[0:nrows, 0:ncl], qn[0:nrows, 0:ncl], cosT[0:nrows, c0:c0 + ncl], ALU.mult)
        k.tt("dve", qn[0:nrows, 0:ncl], k.ps(b2, rows=nrows, c1=ncl), sinT[0:nrows, c0:c0 + ncl], ALU.mult)
        k.tt("pool", t1[0:nrows, 0:ncl], t1[0:nrows, 0:ncl], qn[0:nrows, 0:ncl], ALU.add)
        k.tt("dve", outv, t1[0:nrows, 0:ncl], rstd[0:nrows, 0:ncl], ALU.mult)

    def attend(qh, kh, vaug, Dh, q_blocks, n_kc, scale, ycat, yrow0, ychunk, etl, rdt):
        for qi, (c0, ncl) in enumerate(q_blocks):
            ob = qi % 2
            LA = 2
            for it in range(n_kc + LA):
                if it < n_kc:
                    kc = it
                    sb = 2 + kc % 3
                    e = etl[kc % len(etl)]
                    k.mm(k.ps(sb, c1=ncl), kh[0:Dh, kc * 128:(kc + 1) * 128], qh[0:Dh, c0:c0 + ncl])
                    k.act(e[:, 0:ncl], k.ps(sb, c1=ncl), AF.Exp, scale=scale)
                if it >= LA:
                    kc = it - LA
                    e = etl[kc % len(etl)]
                    k.mm(k.ps(ob, c1=ncl), vaug[:, kc, 64:192], e[:, 0:ncl], start=(kc == 0), stop=(kc == n_kc - 1))
            rd, rs = rdt[qi % len(rdt)]
            k.recip(rd[64:128, 0:ncl], k.ps(ob, rows=64, r0=64, c1=ncl))
            k.copy("pool", rs[0:64, 0:ncl], rd[64:128, 0:ncl])
            k.tt("dve", ycat[yrow0:yrow0 + 64, ychunk, c0:c0 + ncl], k.ps(ob, rows=64, c1=ncl), rs[0:64, 0:ncl], ALU.mult)

    def project_out(l, ycat, w_d, krow0, blocks):
        wo = k.tile([128, 4, 1024], BF16, "wo")
        k.dma(SW, wo.all, dm(w_d[krow0:krow0 + 512, :].rearrange("(kc p) n -> p kc n", p=128)))
        i = 0
        for (c0, ncl, s) in blocks:
            for d in range(8):
                b = 4 + i % 4
                i += 1
                for kk in range(4):
                    k.mm(k.ps(b, c1=ncl), wo[:, kk, d * 128:(d + 1) * 128], ycat[:, kk, c0:c0 + ncl],
                         start=(kk == 0), stop=(kk == 3))
                k.stt("dve", xs[:, d, c0:c0 + ncl], k.ps(b, c1=ncl), M_(l, 2, d, s),
                      xs[:, d, c0:c0 + ncl], ALU.mult, ALU.add)
        k.free(wo)

    def ffn(l, w1_d, w3_d, w2_d, dff, blocks, hid, gate_bc=None):
        ngroups = (dff + 511) // 512
        w1t = [k.tile([128, 8, 512], BF16, f"w1_{i}") for i in range(2)]
        w3t = [k.tile([128, 8, 512], BF16, f"w3_{i}") for i in range(2)]
        w2t = [k.tile([128, 4, 1024], BF16, f"w2_{i}") for i in range(2)]
        sts = [k.tile([128, 512], F32, f"st{i}") for i in range(3)]
        w1v = w1_d.rearrange("(kc p) n -> p kc n", p=128)
        w3v = w3_d.rearrange("(kc p) n -> p kc n", p=128)
        si = 0
        ai = 0
        for g in range(ngroups):
            f0 = g * 512
            fw = min(512, dff - f0)
            nch = fw // 128
            w1, w3, w2 = w1t[g % 2], w3t[g % 2], w2t[g % 2]
            k.dma(SW, w1[:, :, 0:fw], dm(w1v[:, :, f0:f0 + fw]))
            k.dma(SW, w3[:, :, 0:fw], dm(w3v[:, :, f0:f0 + fw]))
            k.dma(SW, w2[:, 0:nch, :], dm(w2_d[f0:f0 + fw, :].rearrange("(kc p) n -> p kc n", p=128)))
            for m in range(nch):
                for (c0, ncl, s) in blocks:
                    bg = si % 2
                    bu = 2 + si % 2
                    st = sts[si % 3]
                    si += 1
                    for kk in range(8):
                        k.mm(k.ps(bg, c1=ncl), w1[:, kk, m * 128:(m + 1) * 128], hbf[:, kk, c0:c0 + ncl],
                             start=(kk == 0), stop=(kk == 7))
                    for kk in range(8):
                        k.mm(k.ps(bu, c1=ncl), w3[:, kk, m * 128:(m + 1) * 128], hbf[:, kk, c0:c0 + ncl],
                             start=(kk == 0), stop=(kk == 7))
                    k.act(st[:, 0:ncl], k.ps(bg, c1=ncl), AF.Silu)
                    if gate_bc is None:
                        k.tt("dve", hid[:, m, c0:c0 + ncl], st[:, 0:ncl], k.ps(bu, c1=ncl), ALU.mult)
                    else:
                        k.tt("dve", st[:, 0:ncl], st[:, 0:ncl], k.ps(bu, c1=ncl), ALU.mult)
                        k.tt("pool", hid[:, m, c0:c0 + ncl], st[:, 0:ncl], gate_bc[:, c0 - NCTX:c0 - NCTX + ncl], ALU.mult)
            for (c0, ncl, s) in blocks:
                for d in range(8):
                    b = 4 + ai % 4
                    ai += 1
                    for kk in range(nch):
                        k.mm(k.ps(b, c1=ncl), w2[:, kk, d * 128:(d + 1) * 128], hid[:, kk, c0:c0 + ncl],
                             start=(kk == 0), stop=(kk == nch - 1))
                    k.stt("dve", xs[:, d, c0:c0 + ncl], k.ps(b, c1=ncl), M_(l, 5, d, s),
                          xs[:, d, c0:c0 + ncl], ALU.mult, ALU.add)
        k.free(*w1t, *w3t, *w2t, *sts)

    ALLB = col_blocks(True, True)
    LATB = col_blocks(False, True)

    norm_mod(0, 0, ALLB)
    ycat = k.tile([128, 4, T], BF16, "ycat")
    layer0_mixer(k, dr, xs, hbf, ycat, ident_f, ones_b, M_, project_out, qk_norm_rope, attend, ALLB)
    if stage == 1:
        write_out()
        return
    norm_mod(0, 1, ALLB)
    ffn(0, dr["ffn_w1"], dr["ffn_w3"], dr["ffn_w2"], 2816, ALLB, ycat)
    if stage == 2:
        write_out()
        return
    import os
    SUB = int(os.environ.get("SUB1", "99"))
    norm_mod(1, 0, ALLB)
    layer1_mixer(k, dr, xs, hbf, ycat, ident_b, ones_b, M_, project_out, qk_norm_rope, attend, ALLB, LATB, SUB)
    if stage == 3:
        write_out()
        return
    moe_layer(k, dr, xs, hbf, ycat, ident_f, ones_b, M_, A_, norm_mod, ffn, LATB)
    write_out()


def layer0_mixer(k, dr, xs, hbf, ycat, ident_f, ones_b, M_, project_out, qk_norm_rope, attend, ALLB):
    import os
    SUB = int(os.environ.get("SUBSTAGE", "99"))
    dm = k.dram
    SW = "pool"
    win = dr["ab_w_in"].rearrange("(kc p) n -> p kc n", p=128)
    cosT = k.tile([64, T], F32, "cosT")
    sinT = k.tile([64, T], F32, "sinT")
    Rm = k.tile([64, 64], BF16, "Rm")
    gq = k.tile([64, 2], F32, "gqk")
    k.dma("sp", cosT.all, dm(dr["rope_g_cos"]))
    k.dma("sp", sinT.all, dm(dr["rope_g_sin"]))
    k.dma(SW, Rm.all, dm(dr["rope_g_R"]))
    k.dma("sp", gq.all, dm(dr["gqa_norms"]))
    wqkv = k.tile([128, 8, 768], BF16, "wqkv")
    k.dma(SW, wqkv.all, dm(win[:, :, 1024:1792]))
    kh = [k.tile([64, T], BF16, f"kh{i}") for i in range(2)]
    vaug = [k.tile([128, 18, 192], BF16, f"vaug{i}") for i in range(2)]
    tl = (k.tile([64, 512], BF16, "n_sq"), k.tile([64, 512], F32, "n_rstd"), k.tile([64, 512], F32, "n_qn"),
          k.tile([64, 512], F32, "n_t1"))
    onesv = ones_b[0:64, 0:64]
    bi = 0
    for h in range(2):
        for (c0, ncl, s) in ALLB:
            b = 4 + bi % 2
            bi += 1
            for kk in range(8):
                k.mm(k.ps(b, rows=64, c1=ncl), wqkv[:, kk, 512 + h * 64:512 + (h + 1) * 64], hbf[:, kk, c0:c0 + ncl],
                     start=(kk == 0), stop=(kk == 7))
            qk_norm_rope(k.ps(b, rows=64, c1=ncl), 64, gq[:, 1:2], onesv, cosT, sinT, Rm.all,
                         kh[h][0:64, c0:c0 + ncl], c0, ncl, tl, bi, (6, 7))
    if SUB == 1:
        return
    BIS = os.environ.get("BIS", "")
    for h in range(2):
        if "m" not in BIS:
            k.memset("pool", vaug[h].all, 1.0)
    for tc in range(18 if "t" not in BIS else 1):
        b = 4 + tc % 2
        if "x" not in BIS:
            for kk in range(8):
                k.mm(k.ps(b, c1=128), hbf[:, kk, tc * 128:(tc + 1) * 128], wqkv[:, kk, 640:768],
                     start=(kk == 0), stop=(kk == 7))
        if "c" not in BIS:
            for h in range(2):
                ce = "dve"
                if "d" in BIS:
                    ce = "dve"
                if "a" in BIS:
                    ce = "act"
                k.copy(ce, vaug[h][:, tc, 64:128], k.ps(b, c0=h * 64, c1=(h + 1) * 64))
    if SUB == 2:
        return
    qhs = [k.tile([64, T], BF16, f"qh{i}") for i in range(2)]
    etl = [k.tile([128, 512], BF16, f"e{i}") for i in range(4)]
    rdt = [(k.tile([128, 512], F32, f"rd{i}"), k.tile([64, 512], F32, f"rs{i}")) for i in range(1)]
    scale = 64 ** -0.5
    for h in range(8 if SUB != 3 else 1):
        qh = qhs[h % 2]
        for (c0, ncl, s) in ALLB:
            b = 4 + bi % 2
            bi += 1
            for kk in range(8):
                k.mm(k.ps(b, rows=64, c1=ncl), wqkv[:, kk, h * 64:(h + 1) * 64], hbf[:, kk, c0:c0 + ncl],
                     start=(kk == 0), stop=(kk == 7))
            qk_norm_rope(k.ps(b, rows=64, c1=ncl), 64, gq[:, 0:1], onesv, cosT, sinT, Rm.all,
                         qh[0:64, c0:c0 + ncl], c0, ncl, tl, bi, (6, 7))
        kv = h // 4
        attend(qh, kh[kv], vaug[kv], 64, [(0, NCTX)], 2, scale, ycat, (h % 2) * 64, h // 2, etl, rdt)
        attend(qh, kh[kv], vaug[kv], 64, [(NCTX + 512 * i, 512) for i in range(4)], 18, scale, ycat,
               (h % 2) * 64, h // 2, etl, rdt)
    k.free(cosT, sinT, Rm, gq, wqkv, *kh, *vaug, *tl, *qhs, *etl, *[t for p in rdt for t in p])
    if SUB == 3:
        return
    project_out(0, ycat, dr["ab_w_out"], 512, ALLB)
    if SUB == 4:
        return

    cw = k.tile([128, 4, 4], F32, "convw")
    cb = k.tile([128, 4], F32, "convb")
    lb = k.tile([128, 2, 2, 4], F32, "lru_b")
    lam = k.tile([128, 2, 4], F32, "lam")
    cneg = k.tile([128, 2, 4], F32, "cneg")
    k.dma("sp", cw.all, dm(dr["lru_convw_fm"]))
    k.dma("sp", cb.all, dm(dr["lru_convb_fm"]))
    k.dma("sp", lb.all, dm(dr["lru_b_fm"]))
    k.dma("sp", lam.all, dm(dr["lru_lam_fm"]))
    lamf = V(lam.ap.rearrange("p a b -> p (a b)"), lam.all.pg)
    cnegf = V(cneg.ap.rearrange("p a b -> p (a b)"), cneg.all.pg)
    k.act(cnegf, lamf, AF.Exp, scale=-1.0)
    k.act(cnegf, cnegf, AF.Ln, bias=1.0)
    k.ts("dve", cnegf, cnegf, -8.0, None, ALU.mult)
    xr = k.tile([128, T], F32, "xr")
    u = k.tile([128, T], F32, "u")
    ubf = k.tile([128, T], BF16, "ubf")
    rec = k.tile([128, T], F32, "rec")
    at = k.tile([128, T], F32, "a")
    bt = k.tile([128, T], F32, "b")
    ht = k.tile([128, T], F32, "h")
    wx = k.tile([128, 8, 256], BF16, "wxg")
    bd = [k.tile([128, 128], BF16, f"bd{i}") for i in range(4)]
    tmp = [k.tile([128, 512], F32, f"lt{i}") for i in range(4)]
    seqs = [(0, NCTX), (NCTX, NLAT)]
    for j in range(4):
        k.dma(SW, wx[:, :, 0:128], dm(k_win_slice(dr, j * 128)))
        k.dma(SW, wx[:, :, 128:256], dm(k_win_slice(dr, 512 + j * 128)))
        for d_ in range(2):
            for gi_, nm in enumerate(("lru_w_a", "lru_w_x")):
                t_ = bd[d_ * 2 + gi_]
                k.memset("pool", t_.all, 0.0)
                for hb_ in range(2):
                    k.dma(SW, t_[hb_ * 64:(hb_ + 1) * 64, hb_ * 64:(hb_ + 1) * 64], dm(dr[nm][d_, 2 * j + hb_]))
        for bi_, (c0, ncl, s) in enumerate(ALLB):
            b = 4 + bi_ % 2
            for kk in range(8):
                k.mm(k.ps(b, c1=ncl), wx[:, kk, 0:128], hbf[:, kk, c0:c0 + ncl], start=(kk == 0), stop=(kk == 7))
            k.copy("act", xr[:, c0:c0 + ncl], k.ps(b, c1=ncl))
        k.ts("dve", u.all, xr.all, cw[:, j, 2:3], cb[:, j:j + 1], ALU.mult, ALU.add)
        for tap in (0, 1, 3):
            sh = tap - 2
            for (s0, L_) in seqs:
                lo = s0 + max(0, -sh)
                hi = s0 + L_ - max(0, sh)
                k.stt("dve", u[:, lo:hi], xr[:, lo + sh:hi + sh], cw[:, j, tap:tap + 1], u[:, lo:hi], ALU.mult, ALU.add)
        k.copy("pool", ubf.all, u.all)
        for d_ in range(2):
            for bi_, (c0, ncl, s) in enumerate(ALLB):
                ba, bx = 4 + bi_ % 2, 6 + bi_ % 2
                k.mm(k.ps(ba, c1=ncl), bd[d_ * 2].all, ubf[:, c0:c0 + ncl])
                k.mm(k.ps(bx, c1=ncl), bd[d_ * 2 + 1].all, ubf[:, c0:c0 + ncl])
                r_, i_, q_, _ = tmp
                k.act(r_[:, 0:ncl], k.ps(ba, c1=ncl), AF.Sigmoid, bias=lb[:, 0, d_, j:j + 1])
                k.act(i_[:, 0:ncl], k.ps(bx, c1=ncl), AF.Sigmoid, bias=lb[:, 1, d_, j:j + 1])
                k.act(at[:, c0:c0 + ncl], r_[:, 0:ncl], AF.Exp, scale=cneg[:, d_, j:j + 1])
                k.tt("pool", q_[:, 0:ncl], at[:, c0:c0 + ncl], at[:, c0:c0 + ncl], ALU.mult)
                k.act(q_[:, 0:ncl], q_[:, 0:ncl], AF.Sqrt, bias=1.0, scale=-1.0)
                k.tt("dve", i_[:, 0:ncl], i_[:, 0:ncl], u[:, c0:c0 + ncl], ALU.mult)
                k.tt("dve", bt[:, c0:c0 + ncl], q_[:, 0:ncl], i_[:, 0:ncl], ALU.mult)
            if d_ == 0:
                k.scan(rec.all, at.all, bt.all, 0.0)
            else:
                rv = lambda v: v.m(lambda ap: ap[:, ::-1])
                k.scan(rv(ht[:, 0:NCTX]), rv(at[:, 0:NCTX]), rv(bt[:, 0:NCTX]), 0.0)
                k.scan(rv(ht[:, NCTX:T]), rv(at[:, NCTX:T]), rv(bt[:, NCTX:T]), ht[:, 0:1])
                k.tt("pool", rec.all, rec.all, ht.all, ALU.add)
        for bi_, (c0, ncl, s) in enumerate(ALLB):
            b = 4 + bi_ % 2
            for kk in range(8):
                k.mm(k.ps(b, c1=ncl), wx[:, kk, 128:256], hbf[:, kk, c0:c0 + ncl], start=(kk == 0), stop=(kk == 7))
            g_, t_, w_, _ = tmp
            k.copy("act", g_[:, 0:ncl], k.ps(b, c1=ncl))
            k.tt("pool", t_[:, 0:ncl], g_[:, 0:ncl], g_[:, 0:ncl], ALU.mult)
            k.ts("dve", t_[:, 0:ncl], t_[:, 0:ncl], 0.044715, 1.0, ALU.mult, ALU.add)
            k.tt("dve", t_[:, 0:ncl], t_[:, 0:ncl], g_[:, 0:ncl], ALU.mult)
            k.act(t_[:, 0:ncl], t_[:, 0:ncl], AF.Sigmoid, scale=2.0 * math.sqrt(2.0 / math.pi))
            k.tt("pool", t_[:, 0:ncl], t_[:, 0:ncl], g_[:, 0:ncl], ALU.mult)
            k.tt("dve", ycat[:, j, c0:c0 + ncl], t_[:, 0:ncl], rec[:, c0:c0 + ncl], ALU.mult)
    k.free(cw, cb, lb, lam, cneg, xr, u, ubf, rec, at, bt, ht, wx, *bd, *tmp)
    project_out(0, ycat, dr["ab_w_out"], 0, ALLB)


def k_win_slice(dr, c0):
    return dr["ab_w_in"].rearrange("(kc p) n -> p kc n", p=128)[:, :, c0:c0 + 128]


_CONSTS = None
_NC_CACHE = {}


def prep_inputs(inp, b):
    global _CONSTS
    if _CONSTS is None:
        _CONSTS = host_consts()
    f = lambda a: np.ascontiguousarray(np.asarray(a, dtype=np.float32))
    m = dict(_CONSTS)
    m.pop("rope_g_R"); m.pop("rope_m_R")
    m["rope_g_R"] = _CONSTS["rope_g_R"]; m["rope_m_R"] = _CONSTS["rope_m_R"]
    m["x"] = f(inp["x"][b])
    m["ctx"] = f(inp["ctx"][b])
    m["c_fm"] = f(fm(inp["c"][b]))
    m["cctx_fm"] = f(fm(inp["c_ctx"]))
    m["mod_w"] = f(inp["mod_w"])
    m["modb"] = f(fm(inp["mod_b"]).transpose(1, 0, 2))
    g = np.stack([fm(inp["norm1_g"]), fm(inp["norm2_g"])], axis=1)
    g = np.repeat(g[..., None], 2, axis=-1).reshape(2, 2, 128, 16)
    m["gdup"] = f(g.transpose(2, 0, 1, 3))
    m["ab_w_in"] = f(inp["ab_w_in"][0])
    m["ab_w_out"] = f(inp["ab_w_out"][0])
    m["gqa_norms"] = f(np.stack([inp["gqa_q_norm"][0], inp["gqa_k_norm"][0]], axis=1))
    m["lru_convw_fm"] = f(fm(inp["lru_conv_w"][0]).transpose(1, 2, 0))
    m["lru_convb_fm"] = f(fm(inp["lru_conv_b"][0]))
    lb = np.stack([fm(inp["lru_b_a"][0].reshape(2, 512)), fm(inp["lru_b_x"][0].reshape(2, 512))], axis=0)
    m["lru_b_fm"] = f(lb.transpose(2, 0, 1, 3))
    m["lru_lam_fm"] = f(fm(inp["lru_lambda"][0]).transpose(1, 0, 2))
    m["lru_w_a"] = f(inp["lru_w_a"][0])
    m["lru_w_x"] = f(inp["lru_w_x"][0])
    m["ffn_w1"] = f(inp["ffn_w1"][0])
    m["ffn_w3"] = f(inp["ffn_w3"][0])
    m["ffn_w2"] = f(inp["ffn_w2"][0])
    m["cd_w_in"] = f(inp["cd_w_in"][0])
    m["cd_w_out"] = f(inp["cd_w_out"][0])
    mg = np.zeros((128, 5), np.float32)
    mg[:, 0:2] = fm(inp["mla_q_a_norm"][0])
    mg[:, 2] = inp["mla_kv_a_norm"][0]
    mg[0:96, 3] = inp["mla_q_norm"][0]
    mg[0:96, 4] = inp["mla_k_norm"][0]
    m["mla_gains"] = mg
    m["mla_q_b"] = f(inp["mla_q_b"][0])
    m["mla_kv_b"] = f(inp["mla_kv_b"][0])
    m["hy_p"] = f(np.stack([inp["hy_filt_b1"][0], inp["hy_filt_b2"][0], inp["hy_sin_freq"][0]], axis=1))
    m["hy_filt_w1"] = f(inp["hy_filt_w1"][0])
    m["hy_filt_w2"] = f(inp["hy_filt_w2"][0])
    m["hy_filt_w3"] = f(inp["hy_filt_w3"][0])
    m["hy_conv_w"] = f(inp["hy_conv_w"][0])
    m["hy_conv_b"] = f(inp["hy_conv_b"][0])
    m["hy_skip"] = f(inp["hy_skip"][0])
    m["moe_router"] = f(inp["moe_router"][0])
    m["moe_w1"] = f(inp["moe_w1"][0])
    m["moe_w3"] = f(inp["moe_w3"][0])
    m["moe_w2"] = f(inp["moe_w2"][0])
    return m


def shapes_of(m):
    return {k_: (tuple(v.shape), str(v.dtype)) for k_, v in m.items()}


def kernel(**inputs):
    maps = [prep_inputs(inputs, b) for b in range(8)]
    shapes = shapes_of(maps[0])
    key = tuple(sorted(shapes.items()))
    if key not in _NC_CACHE:
        _NC_CACHE[key] = build(shapes)
    nc = _NC_CACHE[key]
    res = run_bass_kernel_spmd(nc, maps, core_ids=list(range(8)))
    return np.stack([np.asarray(r["out"], dtype=np.float32) for r in res.results], axis=0)


def ps_bf(k, i, rows, c0, c1):
    b = k.banks[i]
    return V(b[0:rows, :].bitcast(BF16)[:, c0:c1], [(PSUM_PAGE0 + i, PSUM_PAGE0 + i + 1)])


def layer1_mixer(k, dr, xs, hbf, ycat, ident_b, ones_b, M_, project_out, qk_norm_rope, attend, ALLB, LATB, SUB):
    dm = k.dram
    SW = "pool"
    win = dr["cd_w_in"].rearrange("(kc p) n -> p kc n", p=128)
    cosT = k.tile([96, T], F32, "cosM")
    sinT = k.tile([96, T], F32, "sinM")
    Rm = k.tile([96, 96], BF16, "RmM")
    gn = k.tile([128, 5], F32, "mla_g")
    k.dma("sp", cosT.all, dm(dr["rope_m_cos"]))
    k.dma("sp", sinT.all, dm(dr["rope_m_sin"]))
    k.dma(SW, Rm.all, dm(dr["rope_m_R"]))
    k.dma("sp", gn.all, dm(dr["mla_gains"]))
    wqa = k.tile([128, 8, 256], BF16, "wqa")
    wkva = k.tile([128, 8, 128], BF16, "wkva")
    wrp = k.tile([128, 8, 96], BF16, "wrope_pad")
    qb = k.tile([128, 2, 768], BF16, "q_b")
    kvbn = k.tile([128, 8, 96], BF16, "kvb_nope_pad")
    kvbv = k.tile([128, 8, 64], BF16, "kvb_v")
    k.dma(SW, wqa.all, dm(win[:, :, 1536:1792]))
    k.dma(SW, wkva.all, dm(win[:, :, 1792:1920]))
    k.memset("dve", wrp.all, 0.0)
    k.dma(SW, wrp[:, :, 64:96], dm(win[:, :, 1920:1952]))
    k.dma(SW, qb.all, dm(dr["mla_q_b"].rearrange("(kc p) n -> p kc n", p=128)))
    kvb3 = dr["mla_kv_b"].rearrange("k (h c) -> k h c", h=8)
    k.memset("dve", kvbn.all, 0.0)
    k.dma(SW, kvbn[:, :, 0:64], dm(kvb3[:, :, 0:64]))
    k.dma(SW, kvbv.all, dm(kvb3[:, :, 64:128]))
    qa = k.tile([128, 2, NLAT], BF16, "qa")
    kva = k.tile([128, T], BF16, "kva")
    tl = (k.tile([128, 512], BF16, "n_sq"), k.tile([128, 512], F32, "n_rstd"), k.tile([128, 512], F32, "n_qn"),
          k.tile([128, 512], F32, "n_t1"))
    sq2 = k.tile([128, 2, 512], BF16, "sq2")
    raw = k.tile([128, 2, 512], F32, "qa_raw")
    for bi, (c0, ncl, s) in enumerate(LATB):
        for m in range(2):
            b = 4 + m
            for kk in range(8):
                k.mm(k.ps(b, c1=ncl), wqa[:, kk, m * 128:(m + 1) * 128], hbf[:, kk, c0:c0 + ncl],
                     start=(kk == 0), stop=(kk == 7))
            k.act(sq2[:, m, 0:ncl], k.ps(b, c1=ncl), AF.Square)
            k.copy("act", raw[:, m, 0:ncl], k.ps(b, c1=ncl))
        for m in range(2):
            k.mm(k.ps(6, c1=ncl), ones_b.all, sq2[:, m, 0:ncl], start=(m == 0), stop=(m == 1))
        rstd = tl[1]
        k.act(rstd[:, 0:ncl], k.ps(6, c1=ncl), AF.Sqrt, bias=EPS, scale=1.0 / 256)
        k.recip(rstd[:, 0:ncl], rstd[:, 0:ncl])
        for m in range(2):
            k.stt("dve", qa[:, m, c0 - NCTX:c0 - NCTX + ncl], raw[:, m, 0:ncl], gn[:, m:m + 1], rstd[:, 0:ncl],
                  ALU.mult, ALU.mult)
    for bi, (c0, ncl, s) in enumerate(ALLB):
        b = 4 + bi % 2
        for kk in range(8):
            k.mm(k.ps(b, c1=ncl), wkva[:, kk, :], hbf[:, kk, c0:c0 + ncl], start=(kk == 0), stop=(kk == 7))
        sq, rstd = tl[0], tl[1]
        k.act(sq[:, 0:ncl], k.ps(b, c1=ncl), AF.Square)
        k.mm(k.ps(6, c1=ncl), ones_b.all, sq[:, 0:ncl])
        k.act(rstd[:, 0:ncl], k.ps(6, c1=ncl), AF.Sqrt, bias=EPS, scale=1.0 / 128)
        k.recip(rstd[:, 0:ncl], rstd[:, 0:ncl])
        k.stt("dve", kva[:, c0:c0 + ncl], k.ps(b, c1=ncl), gn[:, 2:3], rstd[:, 0:ncl], ALU.mult, ALU.mult)
    k.free(sq2, raw)
    kh = k.tile([96, T], BF16, "kh")
    qh = k.tile([96, T], BF16, "qh")
    vaug = k.tile([128, 18, 192], BF16, "vaug")
    etl = [k.tile([128, 512], BF16, f"e{i}") for i in range(4)]
    rdt = [(k.tile([128, 512], F32, "rd"), k.tile([64, 512], F32, "rs"))]
    k.memset("pool", vaug.all, 1.0)
    onesv = ones_b[0:96, 0:96]
    scale = 96 ** -0.5
    bi = 0
    for h in range(8):
        for (c0, ncl, s) in ALLB:
            b = 4 + bi % 2
            bi += 1
            for kk in range(8):
                k.mm(k.ps(b, rows=96, c1=ncl), wrp[:, kk, :], hbf[:, kk, c0:c0 + ncl], start=(kk == 0), stop=False)
            k.mm(k.ps(b, rows=96, c1=ncl), kvbn[:, h, :], kva[:, c0:c0 + ncl], start=False, stop=True)
            qk_norm_rope(k.ps(b, rows=96, c1=ncl), 96, gn[0:96, 4:5], onesv, cosT, sinT, Rm.all,
                         kh[0:96, c0:c0 + ncl], c0, ncl, tl, bi, (6, 7))
        for (c0, ncl, s) in LATB:
            b = 4 + bi % 2
            bi += 1
            for m in range(2):
                k.mm(k.ps(b, rows=96, c1=ncl), qb[:, m, h * 96:(h + 1) * 96], qa[:, m, c0 - NCTX:c0 - NCTX + ncl],
                     start=(m == 0), stop=(m == 1))
            qk_norm_rope(k.ps(b, rows=96, c1=ncl), 96, gn[0:96, 3:4], onesv, cosT, sinT, Rm.all,
                         qh[0:96, c0:c0 + ncl], c0, ncl, tl, bi, (6, 7))
        for tc in range(18):
            b = 4 + tc % 2
            k.mm(k.ps(b, c1=64), kva[:, tc * 128:(tc + 1) * 128], kvbv[:, h, :])
            k.copy("dve", vaug[:, tc, 64:128], k.ps(b, c1=64))
        attend(qh, kh, vaug, 96, [(NCTX + 512 * i, 512) for i in range(4)], 18, scale, ycat,
               (h % 2) * 64, h // 2, etl, rdt)
    k.free(cosT, sinT, Rm, gn, wqa, wkva, wrp, qb, kvbn, kvbv, qa, kva, *tl, kh, qh, vaug, *etl, *rdt[0])
    project_out(1, ycat, dr["cd_w_out"], 512, LATB)
    if SUB == 1:
        return

    hp = k.tile([64, 3], F32, "hy_p")
    k.dma("sp", hp.all, dm(dr["hy_p"]))
    zT = k.tile([33, NLAT], BF16, "zT")
    k.dma(SW, zT.all, dm(dr["hy_zT"]))
    fw1 = k.tile([33, 64], BF16, "fw1")
    fw2 = k.tile([64, 64], BF16, "fw2")
    k.dma(SW, fw1.all, dm(dr["hy_filt_w1"]))
    k.dma(SW, fw2.all, dm(dr["hy_filt_w2"]))
    h1 = k.tile([64, NLAT], BF16, "h1T")
    h2 = k.tile([64, NLAT], BF16, "h2T")
    ft = [k.tile([64, 512], F32, f"ft{i}") for i in range(2)]
    MAGIC = 12582912.0
    TWO_PI = 2.0 * math.pi

    def sin_layer(dst, lhsT, src, bcol):
        for i in range(4):
            cs = slice(i * 512, (i + 1) * 512)
            b = 4 + i % 2
            k.mm(k.ps(b, rows=64), lhsT, src[:, cs])
            y, t_ = ft
            k.ts("dve", y.all, k.ps(b, rows=64), hp[:, bcol:bcol + 1], hp[:, 2:3], ALU.add, ALU.mult)
            k.ts("dve", t_.all, y.all, 1.0 / TWO_PI, MAGIC, ALU.mult, ALU.add)
            k.ts("dve", t_.all, t_.all, -MAGIC, -TWO_PI, ALU.add, ALU.mult)
            k.tt("dve", y.all, y.all, t_.all, ALU.add)
            k.act(dst[:, cs], y.all, AF.Sin)

    sin_layer(h1, fw1.all, zT, 0)
    sin_layer(h2, fw2.all, h1, 1)
    k.free(zT, fw1, fw2, h1, *ft, hp)

    CH = 256
    vt = k.tile([128, 16, 512], BF16, "hy_v")
    x1 = k.tile([128, 16, 512], BF16, "hy_x1")
    x2 = k.tile([128, 16, 512], BF16, "hy_x2")
    hpadL = k.tile([128, 8, 129], BF16, "hpadL")
    hpadR = k.tile([128, 8, 129], BF16, "hpadR")
    k.memset("dve", hpadL.all, 0.0)
    k.memset("dve", hpadR.all, 0.0)
    k.copy("pool", hpadL[:, :, 1:129], hbf[:, :, NCTX:NCTX + 128])
    k.copy("pool", hpadR[:, :, 0:127], hbf[:, :, T - 127:T])
    wf = k.tile([128, 8, CH], BF16, "hy_wf")
    wj = [k.tile([128, 8, CH], BF16, f"hy_wj{j}") for j in range(3)]
    cwb = k.tile([128, 3, CH], F32, "hy_cwb")
    bb = k.tile([128, CH], F32, "hy_bias")
    convw = dr["hy_conv_w"]
    for half in range(2):
        hs = slice(half * CH, (half + 1) * CH)
        for part, dst in enumerate((vt, x1, x2)):
            cbase = part * 512 + half * CH
            k.dma(SW, wf.all, dm(win[:, :, cbase:cbase + CH]))
            k.dma("sp", cwb.all, dm(convw[:, cbase:cbase + CH].partition_broadcast(128)))
            k.dma("sp", bb.all, dm(dr["hy_conv_b"][cbase:cbase + CH].partition_broadcast(128)))
            for j in range(3):
                for kk in range(8):
                    k.tt("dve" if kk % 2 else "pool", wj[j][:, kk, :], wf[:, kk, :], cwb[:, j, :], ALU.mult)
            for tcn in range(16):
                b = 4 + tcn % 2
                n_mm = 0
                for j in range(3):
                    for kk in range(8):
                        sh = j - 1
                        if tcn == 0 and sh == -1:
                            lhs = hpadL[:, kk, 0:128]
                        elif tcn == 15 and sh == 1:
                            lhs = hpadR[:, kk, 0:128]
                        else:
                            c0 = NCTX + tcn * 128 + sh
                            lhs = hbf[:, kk, c0:c0 + 128]
                        k.mm(k.ps(b, c1=CH), lhs, wj[j][:, kk, :], start=(n_mm == 0), stop=(n_mm == 23))
                        n_mm += 1
                k.tt("dve", dst[:, tcn, hs], k.ps(b, c1=CH), bb.all, ALU.add)
    k.free(hpadL, hpadR, wf, *wj, cwb, bb)
    carve = [hbf.off]

    def ctile(shape, dt, name):
        t_ = Tile(k, carve[0], shape, dt, name)
        carve[0] += ((t_.nbytes + PAGE - 1) // PAGE) * PAGE
        assert carve[0] <= hbf.off + hbf.alloc
        return t_

    hsum = ctile([128, 16, CH], BF16, "hsum")
    hdif = ctile([128, 16, CH], BF16, "hdif")
    Yre = ctile([128, 16, CH], BF16, "Yre")
    Yim = ctile([128, 16, CH], BF16, "Yim")
    tmps = [ctile([128, 512], F32, "hyt0"), ctile([128, 512], F32, "hyt1"),
            k.tile([128, 512], F32, "hyt2"), k.tile([128, 512], F32, "hyt3")]
    dftA = [k.tile([128, 16, 128], BF16, f"dftA{i}") for i in range(2)]
    dftB = [k.tile([128, 16, 128], BF16, f"dftB{i}") for i in range(2)]
    w3 = k.tile([64, 512], BF16, "fw3")
    skb = k.tile([128, CH], F32, "hy_skipb")
    winr = [k.tile([128, CH], F32, f"hywin{i}") for i in range(2)]
    for half in range(2):
        hs = slice(half * CH, (half + 1) * CH)
        for o in range(2):
            xo = x1 if o == 0 else x2
            for dr_ in range(2):
                cb_ = o * 1024 + dr_ * 512 + half * CH
                k.dma(SW, w3[:, dr_ * CH:(dr_ + 1) * CH], dm(dr["hy_filt_w3"][:, cb_:cb_ + CH]))
            k.dma("sp", skb.all, dm(dr["hy_skip"][o, half * CH:(half + 1) * CH].partition_broadcast(128)))
            for tcn in range(16):
                b = 4 + tcn % 2
                wn = winr[tcn % 2]
                k.dma("sp", wn.all, dm(dr["hy_window"][tcn * 128:(tcn + 1) * 128, half * CH:(half + 1) * CH]))
                k.mm(k.ps(b), h2[0:64, tcn * 128:(tcn + 1) * 128], w3.all)
                hf_, hb_ = tmps[0], tmps[1]
                k.tt("dve", hf_[:, 0:CH], k.ps(b, c0=0, c1=CH), wn.all, ALU.mult)
                k.tt("dve", hb_[:, 0:CH], k.ps(b, c0=CH, c1=2 * CH), wn.all, ALU.mult)
                k.tt("pool", hsum[:, tcn, :], hf_[:, 0:CH], hb_[:, 0:CH], ALU.add)
                k.tt("pool", hdif[:, tcn, :], hb_[:, 0:CH], hf_[:, 0:CH], ALU.subtract)
                if tcn == 0:
                    k.copy("pool", hsum[0:1, 0, :], hf_[0:1, 0:CH])
            for fc in range(16):
                ca, sa = dftA[fc % 2], dftB[fc % 2]
                k.dma("sp", V(ca.ap.rearrange("p a b -> p (a b)"), ca.all.pg), dm(dr["dft_cf"][fc]))
                k.dma("sp", V(sa.ap.rearrange("p a b -> p (a b)"), sa.all.pg), dm(dr["dft_sf"][fc]))
                bx, by = 4 + (fc % 2) * 2, 5 + (fc % 2) * 2
                for tcn in range(16):
                    k.mm(k.ps(bx, c0=0, c1=CH), ca[:, tcn, :], vt[:, tcn, hs], start=(tcn == 0), stop=(tcn == 15))
                for tcn in range(16):
                    k.mm(k.ps(bx, c0=CH, c1=2 * CH), sa[:, tcn, :], vt[:, tcn, hs], start=(tcn == 0), stop=(tcn == 15))
                for tcn in range(16):
                    k.mm(k.ps(by, c0=0, c1=CH), ca[:, tcn, :], hsum[:, tcn, :], start=(tcn == 0), stop=(tcn == 15))
                for tcn in range(16):
                    k.mm(k.ps(by, c0=CH, c1=2 * CH), sa[:, tcn, :], hdif[:, tcn, :], start=(tcn == 0), stop=(tcn == 15))
                kt, t1, t2 = tmps[2], tmps[0], tmps[1]
                k.copy("act", kt.all, k.ps(by))
                A_ = k.ps(bx, c0=0, c1=CH)
                B_ = k.ps(bx, c0=CH, c1=2 * CH)
                k.tt("dve", t1[:, 0:CH], A_, kt[:, 0:CH], ALU.mult)
                k.tt("dve", t1[:, CH:2 * CH], B_, kt[:, CH:2 * CH], ALU.mult)
                k.tt("pool", Yre[:, fc, :], t1[:, 0:CH], t1[:, CH:2 * CH], ALU.add)
                k.tt("dve", t2[:, 0:CH], A_, kt[:, CH:2 * CH], ALU.mult)
                k.tt("dve", t2[:, CH:2 * CH], B_, kt[:, 0:CH], ALU.mult)
                k.tt("pool", Yim[:, fc, :], t2[:, 0:CH], t2[:, CH:2 * CH], ALU.subtract)
            for tcn in range(16):
                ca, sa = dftA[tcn % 2], dftB[tcn % 2]
                k.dma("sp", V(ca.ap.rearrange("p a b -> p (a b)"), ca.all.pg), dm(dr["dft_ci"][tcn]))
                k.dma("sp", V(sa.ap.rearrange("p a b -> p (a b)"), sa.all.pg), dm(dr["dft_si"][tcn]))
                b = 4 + tcn % 2
                for fc in range(16):
                    k.mm(k.ps(b, c1=CH), ca[:, fc, :], Yre[:, fc, :], start=(fc == 0), stop=False)
                for fc in range(16):
                    k.mm(k.ps(b, c1=CH), sa[:, fc, :], Yim[:, fc, :], start=False, stop=(fc == 15))
                t1 = tmps[3]
                k.tt("pool", t1[:, 0:CH], vt[:, tcn, hs], skb.all, ALU.mult)
                k.tt("dve", t1[:, 0:CH], k.ps(b, c1=CH), t1[:, 0:CH], ALU.add)
                k.tt("dve", vt[:, tcn, hs], t1[:, 0:CH], xo[:, tcn, hs], ALU.mult)
    for tcn in range(16):
        b = 4 + tcn % 2
        for cc in range(4):
            k.transpose(ps_bf(k, b, 128, cc * 128, (cc + 1) * 128), vt[:, tcn, cc * 128:(cc + 1) * 128], ident_b.all)
        for cc in range(4):
            k.copy("dve", ycat[:, cc, NCTX + tcn * 128:NCTX + (tcn + 1) * 128],
                   ps_bf(k, b, 128, cc * 128, (cc + 1) * 128))
    k.free(vt, x1, x2, *dftA, *dftB, w3, skb, tmps[2], tmps[3], *winr, h2)
    project_out(1, ycat, dr["cd_w_out"], 0, LATB)


def moe_layer(k, dr, xs, hbf, ycat, ident_f, ones_b, M_, A_, norm_mod, ffn, LATB):
    dm = k.dram
    AX = mybir.AxisListType.X
    rw = k.tile([128, 8, 8], F32, "rw")
    r_hi = k.tile([128, 8, 8], BF16, "r_hi")
    r_lo = k.tile([128, 8, 8], BF16, "r_lo")
    k.dma("sp", rw.all, dm(dr["moe_router"].rearrange("(kc p) e -> p kc e", p=128)))
    k.copy("dve", r_hi.all, rw.all)
    k.tt("dve", r_lo.all, rw.all, r_hi.all, ALU.subtract)
    logits = k.tile([128, 16, 8], F32, "logits")
    gates = k.tile([128, 16, 8], F32, "gates")
    hlo = k.tile([128, 8, 512], BF16, "hlo")
    hf32 = [k.tile([128, 512], F32, f"hf32_{i}") for i in range(2)]

    def hook(bi, c0, ncl, kk, tmp, s):
        hf = hf32[kk % 2]
        k.act(hf[:, 0:ncl], tmp[:, 0:ncl], AF.Identity, bias=M_(1, 3, kk, s), scale=A_(1, 1, kk, s))
        k.tt("pool", hlo[:, kk, 0:ncl], hf[:, 0:ncl], hbf[:, kk, c0:c0 + ncl], ALU.subtract)
        if kk == 7:
            for q in range(ncl // 128):
                tcn = (c0 - NCTX) // 128 + q
                b = 6 + tcn % 2
                n = 0
                for which in range(3):
                    for k2 in range(8):
                        if which == 1:
                            lhs = hlo[:, k2, q * 128:(q + 1) * 128]
                        else:
                            lhs = hbf[:, k2, c0 + q * 128:c0 + (q + 1) * 128]
                        rr = r_lo if which == 2 else r_hi
                        k.mm(k.ps(b, c1=8), lhs, rr[:, k2, :], start=(n == 0), stop=(n == 23))
                        n += 1
                k.copy("dve", logits[:, tcn, :], k.ps(b, c1=8))

    norm_mod(1, 1, LATB, f32_hook=hook)
    k.free(hlo, *hf32, rw, r_hi, r_lo)
    mx = k.tile([128, 8], F32, "mx")
    sel = k.tile([128, 8], F32, "sel")
    ex = k.tile([128, 8], F32, "ex")
    sm = k.tile([128, 4], F32, "sm")
    for tcn in range(16):
        lg = logits[:, tcn, :]
        k.op("dve", (lambda o_, i_: (lambda e_: e_.max(out=o_, in_=i_)))(mx.ap, lg.ap), R=[lg], W=[mx.all])
        k.ts("dve", sel.all, lg, mx[:, 1:2], None, ALU.is_ge)
        k.ts("dve", sm[:, 0:1], mx[:, 0:1], -1.0, None, ALU.mult)
        k.act(ex.all, lg, AF.Exp, bias=sm[:, 0:1])
        k.tt("dve", ex.all, ex.all, sel.all, ALU.mult)
        k.op("dve", (lambda o_, i_: (lambda e_: e_.reduce_sum(out=o_, in_=i_, axis=AX)))(sm[:, 1:2].ap, ex.ap),
             R=[ex.all], W=[sm[:, 1:2]])
        k.recip(sm[:, 2:3], sm[:, 1:2])
        k.ts("dve", gates[:, tcn, :], ex.all, sm[:, 2:3], None, ALU.mult)
    k.free(mx, sel, ex, sm)
    Gbs = [k.tile([128, NLAT], F32, f"Gb{i}") for i in range(2)]
    Dt = k.tile([128, 128], F32, "Dt")
    Dhi = k.tile([128, 128], BF16, "Dhi")
    Dlo = k.tile([128, 128], BF16, "Dlo")
    for e in range(8):
        Gb = Gbs[e % 2]
        for tcn in range(16):
            b = 6 + tcn % 2
            k.ts("dve", Dt.all, ident_f.all, gates[:, tcn, e:e + 1], None, ALU.mult)
            k.copy("dve", Dhi.all, Dt.all)
            k.tt("dve", Dlo.all, Dt.all, Dhi.all, ALU.subtract)
            k.mm(k.ps(b, c1=128), ones_b.all, Dhi.all, start=True, stop=False)
            k.mm(k.ps(b, c1=128), ones_b.all, Dlo.all, start=False, stop=True)
            k.copy("act", Gb[:, tcn * 128:(tcn + 1) * 128], k.ps(b, c1=128))
        ffn(1, dr["moe_w1"][e], dr["moe_w3"][e], dr["moe_w2"][e], 3584, LATB, ycat, gate_bc=Gb)
    k.free(*Gbs, Dt, Dhi, Dlo, logits, gates)
```

```python
import contextlib
import math
import numpy as np
import ml_dtypes
import concourse.bass as bass
import concourse.mybir as mybir
from concourse.bass_utils import run_bass_kernel_spmd

F32 = mybir.dt.float32
BF16 = mybir.dt.bfloat16
U8 = mybir.dt.uint8
AF = mybir.ActivationFunctionType
ALU = mybir.AluOpType

ENGS = ("pe", "act", "dve", "pool", "sp")
N_DMA_SEMS = 16
DMA_POOLS = {"sp": list(range(0, 7)), "pool": list(range(7, 14)), "act": [14, 15]}
PAGE = 256
ARENA = 207 * 1024
PSUM_PAGE0 = 100000
DRAM_PAGE0 = 200000

D = 1024
T = 2304
NCTX = 256
NLAT = 2048
EPS = 1e-6


class Op:
    __slots__ = ("eng", "fn", "dma", "preds", "sig", "sigval", "dsem", "dval", "idx")


class Prog:
    def __init__(self, nc):
        self.nc = nc
        self.ops = []
        self.pw = {}
        self.pr = {}

    def add(self, eng, fn, reads=(), writes=(), dma=False):
        op = Op()
        op.eng = eng
        op.fn = fn
        op.dma = dma
        op.idx = len(self.ops)
        op.sig = False
        preds = set()
        pw, pr = self.pw, self.pr
        for (lo, hi) in reads:
            for p in range(lo, hi):
                w = pw.get(p)
                if w is not None:
                    preds.add(w)
        for (lo, hi) in writes:
            for p in range(lo, hi):
                w = pw.get(p)
                if w is not None:
                    preds.add(w)
                r = pr.get(p)
                if r:
                    preds.update(r.values())
        rkey = ("d", op.idx) if dma else eng
        for (lo, hi) in reads:
            for p in range(lo, hi):
                r = pr.get(p)
                if r is None:
                    pr[p] = {rkey: op.idx}
                else:
                    r[rkey] = op.idx
        for (lo, hi) in writes:
            for p in range(lo, hi):
                pw[p] = op.idx
                pr[p] = None
        preds.discard(op.idx)
        op.preds = preds
        self.ops.append(op)
        return op

    def emit(self):
        nc = self.nc
        ops = self.ops
        with contextlib.ExitStack() as es:
            esem = {e: es.enter_context(nc.semaphore(f"s_{e}")) for e in ENGS}
            dsems = [es.enter_context(nc.semaphore(f"s_dma{i}")) for i in range(N_DMA_SEMS)]
            waits = [None] * len(ops)
            for op in ops:
                per_eng = {}
                dma_w = []
                for p in op.preds:
                    po = ops[p]
                    if po.dma:
                        dma_w.append(p)
                    else:
                        if po.eng == "pe" and op.eng == "pe" and not op.dma:
                            continue
                        if po.eng not in per_eng or per_eng[po.eng] < p:
                            per_eng[po.eng] = p
                for p in per_eng.values():
                    ops[p].sig = True
                waits[op.idx] = (list(per_eng.values()), dma_w)
            cnt = {e: 0 for e in ENGS}
            dcnt = [0] * N_DMA_SEMS
            dlast = [None] * N_DMA_SEMS
            dpos = {e: 0 for e in ENGS}
            dma_prev = {}
            for op in ops:
                if op.dma:
                    rng = DMA_POOLS[op.eng]
                    k = rng[dpos[op.eng] % len(rng)]
                    dpos[op.eng] += 1
                    dcnt[k] += 16
                    op.dsem = k
                    op.dval = dcnt[k]
                    dma_prev[op.idx] = dlast[k]
                    dlast[k] = op.idx
                elif op.sig:
                    cnt[op.eng] += 1
                    op.sigval = cnt[op.eng]
            streams = {e: [o for o in ops if o.eng == e] for e in ENGS}
            self.stats = {e: len(streams[e]) for e in ENGS}
            self.stats["sig"] = dict(cnt)

            def run_stream(e, eng):
                known = {}

                def wait(key, sem, val):
                    if known.get(key, 0) >= val:
                        return
                    known[key] = val
                    eng.wait_ge(sem, val)

                for op in streams[e]:
                    cw, dw = waits[op.idx]
                    for p in cw:
                        po = ops[p]
                        wait(po.eng, esem[po.eng], po.sigval)
                    for p in dw:
                        po = ops[p]
                        wait(("d", po.dsem), dsems[po.dsem], po.dval)
                    if op.dma:
                        pp = dma_prev[op.idx]
                        if pp is not None:
                            po = ops[pp]
                            wait(("d", po.dsem), dsems[po.dsem], po.dval)
                        ins = op.fn(eng)
                        ins.then_inc(dsems[op.dsem], 16)
                    else:
                        ins = op.fn(eng)
                        if op.sig:
                            ins.then_inc(esem[e], 1)
                if e == "sp":
                    for k in range(N_DMA_SEMS):
                        if dcnt[k]:
                            eng.wait_ge(dsems[k], dcnt[k])

            with nc.Block() as block:
                @block.tensor
                def _(eng):
                    run_stream("pe", eng)

                @block.scalar
                def _(eng):
                    run_stream("act", eng)

                @block.vector
                def _(eng):
                    run_stream("dve", eng)

                @block.gpsimd
                def _(eng):
                    run_stream("pool", eng)

                @block.sync
                def _(eng):
                    run_stream("sp", eng)


class V:
    __slots__ = ("ap", "pg")

    def __init__(self, ap, pg):
        self.ap = ap
        self.pg = pg

    def m(self, f):
        return V(f(self.ap), self.pg)


class Tile:
    def __init__(self, k, off, shape, dt, name):
        self.k = k
        self.off = off
        self.shape = list(shape)
        self.dt = dt
        self.es = 4 if dt == F32 else (2 if dt == BF16 else 1)
        n = 1
        for s in shape[1:]:
            n *= s
        self.n = n
        self.nbytes = n * self.es
        base = k.arena[0:shape[0], off:off + self.nbytes]
        if dt != U8:
            base = base.bitcast(dt)
        if len(shape) == 3:
            base = base.rearrange("p (a b) -> p a b", a=shape[1])
        elif len(shape) == 4:
            base = base.rearrange("p (a b c) -> p a b c", a=shape[1], b=shape[2])
        self.ap = base
        self.name = name

    def _pages(self, lo_e, hi_e):
        lo = self.off + lo_e * self.es
        hi = self.off + hi_e * self.es
        return (lo // PAGE, (hi + PAGE - 1) // PAGE)

    @property
    def all(self):
        return V(self.ap, [self._pages(0, self.n)])

    def __getitem__(self, idx):
        if not isinstance(idx, tuple):
            idx = (idx,)
        ap = self.ap[idx]
        fidx = list(idx[1:]) + [slice(None)] * (len(self.shape) - len(idx))
        dims = self.shape[1:]
        rng = []
        for dim, ix in zip(dims, fidx):
            if isinstance(ix, int):
                rng.append((ix, ix + 1))
            else:
                a = 0 if ix.start is None else ix.start
                b = dim if ix.stop is None else ix.stop
                rng.append((a, b))
        strides = []
        st = 1
        for dim in reversed(dims):
            strides.insert(0, st)
            st *= dim
        lead = rng[:-1]
        cnt = 1
        for a, b in lead:
            cnt *= (b - a)
        pages = []
        if cnt <= 64:
            import itertools
            for combo in itertools.product(*[range(a, b) for a, b in lead]):
                base = sum(c * s_ for c, s_ in zip(combo, strides[:-1]))
                pages.append(self._pages(base + rng[-1][0], base + rng[-1][1]))
        else:
            lo = sum(a * s_ for (a, b), s_ in zip(rng, strides))
            hi = sum((b - 1) * s_ for (a, b), s_ in zip(rng, strides)) + 1
            pages.append(self._pages(lo, hi))
        return V(ap, pages)


def pgs(vs):
    out = []
    for v in vs:
        out.extend(v.pg)
    return out


class K:
    def __init__(self, nc, es):
        self.nc = nc
        self.P = Prog(nc)
        self.arena_t = es.enter_context(nc.sbuf_tensor("arena", [128, ARENA], U8))
        self.arena = self.arena_t
        self.free_list = [(0, ARENA)]
        self.banks = [es.enter_context(nc.psum_tensor(f"bank{i}", [128, 512], F32)) for i in range(8)]
        self.bank_i = 0
        self.dram_pg = DRAM_PAGE0

    def tile(self, shape, dt, name="t"):
        es_ = 4 if dt == F32 else (2 if dt == BF16 else 1)
        n = 1
        for s in shape[1:]:
            n *= s
        nb = ((n * es_ + PAGE - 1) // PAGE) * PAGE
        for i, (o, sz) in enumerate(self.free_list):
            if sz >= nb:
                if sz == nb:
                    self.free_list.pop(i)
                else:
                    self.free_list[i] = (o + nb, sz - nb)
                t = Tile(self, o, shape, dt, name)
                t.alloc = nb
                return t
        raise RuntimeError(f"SBUF arena full allocating {name} {shape} ({nb}B); free={self.free_list}")

    def free(self, *tiles):
        for t in tiles:
            self.free_list.append((t.off, t.alloc))
        self.free_list.sort()
        merged = []
        for o, sz in self.free_list:
            if merged and merged[-1][0] + merged[-1][1] == o:
                merged[-1] = (merged[-1][0], merged[-1][1] + sz)
            else:
                merged.append((o, sz))
        self.free_list = merged

    def ps(self, i, rows=128, c0=0, c1=512, r0=0):
        b = self.banks[i]
        return V(b[r0:r0 + rows, c0:c1], [(PSUM_PAGE0 + i, PSUM_PAGE0 + i + 1)])

    def ps3(self, i, a, rows=128):
        b = self.banks[i]
        return V(b[0:rows, :].rearrange("p (a b) -> p a b", a=a), [(PSUM_PAGE0 + i, PSUM_PAGE0 + i + 1)])

    def dram(self, ap):
        self.dram_pg += 1
        return V(ap, [(self.dram_pg, self.dram_pg + 1)])

    def op(self, eng, fn, R=(), W=()):
        self.P.add(eng, fn, pgs(R), pgs(W))

    def dma(self, eng, out, in_):
        self.P.add(eng, lambda e: e.dma_start(out=out.ap, in_=in_.ap), pgs([in_]), pgs([out]), dma=True)

    def mm(self, out, lhsT, rhs, start=True, stop=True):
        self.P.add("pe", lambda e: e.matmul(out.ap, lhsT=lhsT.ap, rhs=rhs.ap, start=start, stop=stop),
                   pgs([lhsT, rhs]), pgs([out]))

    def transpose(self, out, in_, ident):
        self.P.add("pe", lambda e: e.transpose(out.ap, in_.ap, ident.ap), pgs([in_, ident]), pgs([out]))

    def act(self, out, in_, func, bias=None, scale=None):
        R = [in_]
        kw = {}
        if bias is not None:
            if isinstance(bias, V):
                R.append(bias)
                kw["bias"] = bias.ap
            else:
                kw["bias"] = float(bias)
        if scale is not None:
            if isinstance(scale, V):
                R.append(scale)
                kw["scale"] = scale.ap
            else:
                kw["scale"] = float(scale)
        self.P.add("act", lambda e: e.activation(out=out.ap, in_=in_.ap, func=func, **kw),
                   pgs(R), pgs([out]))

    def copy(self, eng, out, in_):
        if eng == "act":
            self.P.add("act", lambda e: e.copy(out=out.ap, in_=in_.ap), pgs([in_]), pgs([out]))
        else:
            self.P.add(eng, lambda e: e.tensor_copy(out=out.ap, in_=in_.ap), pgs([in_]), pgs([out]))

    def tt(self, eng, out, in0, in1, op):
        self.P.add(eng, lambda e: e.tensor_tensor(out=out.ap, in0=in0.ap, in1=in1.ap, op=op),
                   pgs([in0, in1]), pgs([out]))

    def ts(self, eng, out, in0, s1, s2, op0, op1=None):
        R = [in0]
        a1 = s1.ap if isinstance(s1, V) else float(s1)
        if isinstance(s1, V):
            R.append(s1)
        if s2 is None:
            self.P.add(eng, lambda e: e.tensor_scalar(out=out.ap, in0=in0.ap, scalar1=a1, scalar2=None, op0=op0),
                       pgs(R), pgs([out]))
            return
        a2 = s2.ap if isinstance(s2, V) else float(s2)
        if isinstance(s2, V):
            R.append(s2)
        self.P.add(eng, lambda e: e.tensor_scalar(out=out.ap, in0=in0.ap, scalar1=a1, scalar2=a2, op0=op0, op1=op1),
                   pgs(R), pgs([out]))

    def stt(self, eng, out, in0, scalar, in1, op0, op1):
        R = [in0, in1]
        a = scalar.ap if isinstance(scalar, V) else float(scalar)
        if isinstance(scalar, V):
            R.append(scalar)
        self.P.add(eng, lambda e: e.scalar_tensor_tensor(out=out.ap, in0=in0.ap, scalar=a, in1=in1.ap, op0=op0, op1=op1),
                   pgs(R), pgs([out]))

    def memset(self, eng, out, val):
        self.P.add(eng, lambda e: e.memset(out.ap, val), [], pgs([out]))

    def recip(self, out, in_):
        self.P.add("dve", lambda e: e.reciprocal(out=out.ap, in_=in_.ap), pgs([in_]), pgs([out]))

    def scan(self, out, d0, d1, initial):
        R = [d0, d1]
        a = initial.ap if isinstance(initial, V) else float(initial)
        if isinstance(initial, V):
            R.append(initial)
        self.P.add("dve", lambda e: e.tensor_tensor_scan(out=out.ap, data0=d0.ap, data1=d1.ap, initial=a,
                                                       op0=ALU.mult, op1=ALU.add),
                   pgs(R), pgs([out]))


def col_blocks(ctx=True, lat=True):
    b = []
    if ctx:
        b.append((0, NCTX, 1))
    if lat:
        for i in range(4):
            b.append((NCTX + 512 * i, 512, 0))
    return b


def fm(v):
    v = np.asarray(v)
    n = v.shape[-1] // 128
    return np.ascontiguousarray(v.reshape(v.shape[:-1] + (n, 128)).swapaxes(-1, -2))


def rope_tables(rot_dim, nfeat, rot_off):
    n_freq = rot_dim // 4
    inv_freq = (10000.0 ** (-np.arange(n_freq, dtype=np.float32) / n_freq)).astype(np.float32)
    t = np.arange(NLAT)
    row = (t // 64).astype(np.float32)
    col = (t % 64).astype(np.float32)
    ang = np.concatenate([row[:, None] * inv_freq, col[:, None] * inv_freq], axis=-1).astype(np.float32)
    cos = np.cos(ang).astype(np.float32)
    sin = np.sin(ang).astype(np.float32)
    C = np.ones((nfeat, T), np.float32)
    S = np.zeros((nfeat, T), np.float32)
    half = rot_dim // 2
    for f in range(rot_off, nfeat):
        i = (f - rot_off) % half
        C[f, NCTX:] = cos[:, i]
        S[f, NCTX:] = sin[:, i]
    R = np.zeros((nfeat, nfeat), np.float32)
    for i in range(half):
        R[rot_off + i + half, rot_off + i] = -1.0
        R[rot_off + i, rot_off + i + half] = 1.0
    return C, S, R


def host_consts():
    c = {}
    c["ident"] = np.eye(128, dtype=np.float32)
    cg, sg, rg = rope_tables(64, 64, 0)
    c["rope_g_cos"], c["rope_g_sin"], c["rope_g_R"] = cg, sg, rg
    cm, sm, rm = rope_tables(32, 96, 64)
    c["rope_m_cos"], c["rope_m_sin"], c["rope_m_R"] = cm, sm, rm
    L = NLAT
    N = 2 * L
    s = np.arange(L, dtype=np.float64)[:, None]
    f = np.arange(L, dtype=np.float64)[None, :]
    th = np.pi * (2 * f + 1) * s / N
    Cm = np.cos(th)
    Sm = np.sin(th)
    bf = ml_dtypes.bfloat16
    def tile_(M):
        return np.ascontiguousarray(M.reshape(16, 128, 16, 128).transpose(2, 1, 0, 3).reshape(16, 128, 2048)).astype(bf)
    c["dft_cf"] = tile_(Cm)
    c["dft_sf"] = tile_(Sm)
    c["dft_ci"] = tile_(Cm.T * (2.0 / N))
    c["dft_si"] = tile_(-Sm.T * (2.0 / N))
    t = np.arange(L, dtype=np.float32)[:, None]
    t_norm = t / max(L - 1, 1)
    bands = np.linspace(1e-4, 16 - 1, 16, dtype=np.float32)
    ang = (2.0 * math.pi * t * bands / L).astype(np.float32)
    z = np.concatenate([t_norm, np.cos(ang), -np.sin(ang)], axis=-1).astype(np.float32)
    c["hy_zT"] = np.ascontiguousarray(z.T)
    HY_MIN = math.log(1e-2) / 1.5
    HY_MAX = math.log(1e-2) / 0.3
    deltas = np.abs(np.linspace(HY_MIN, HY_MAX, 512, dtype=np.float32))
    window = (np.exp(-t_norm * deltas) + 0.05).astype(np.float32)
    c["hy_window"] = window
    return c


def build(shapes, stage=99):
    nc = bass.Bass("TRN2", target_bir_lowering=False)
    dr = {}
    for name, shp in shapes.items():
        dt_ = F32
        if isinstance(shp, tuple) and len(shp) == 2 and isinstance(shp[1], str):
            shp, dts = shp
            dt_ = BF16 if dts == "bfloat16" else F32
        dr[name] = nc.dram_tensor(name, list(shp), dt_, kind="ExternalInput").ap()
    out_d = nc.dram_tensor("out", [NLAT, D], F32, kind="ExternalOutput").ap()
    outc_d = nc.dram_tensor("outc", [NCTX, D], F32, kind="ExternalOutput").ap()
    with contextlib.ExitStack() as es:
        k = K(nc, es)
        _emit_program(k, dr, out_d, outc_d, stage)
        k.P.emit()
    return nc


def _emit_program(k, dr, out_d, outc_d, stage):
    dm = k.dram
    SW = "pool"

    xs = k.tile([128, 8, T], F32, "xs")
    hbf = k.tile([128, 8, T], BF16, "hbf")
    ident_f = k.tile([128, 128], F32, "ident_f")
    ident_b = k.tile([128, 128], BF16, "ident_b")
    ones_b = k.tile([128, 128], BF16, "ones_b")
    mods = k.tile([128, 2, 48, 2], F32, "mods")
    affA = k.tile([128, 2, 2, 16], F32, "affA")
    gdup = k.tile([128, 2, 2, 16], F32, "gdup")
    modb = k.tile([128, 2, 48], F32, "modb")

    k.dma("sp", ident_f.all, dm(dr["ident"]))
    k.copy("dve", ident_b.all, ident_f.all)
    k.memset("dve", ones_b.all, 1.0)
    k.dma("sp", gdup.all, dm(dr["gdup"]))
    k.dma("sp", modb.all, dm(dr["modb"]))

    xts = [k.tile([128, 1024], F32, f"xt{i}") for i in range(2)]
    for tc in range(18):
        src = dr["ctx"][tc * 128:(tc + 1) * 128, :] if tc < 2 else dr["x"][(tc - 2) * 128:(tc - 1) * 128, :]
        xt = xts[tc % 2]
        k.dma("sp", xt.all, dm(src))
        for half in range(2):
            b = (tc * 2 + half) % 4
            for kk in range(4):
                kf = half * 4 + kk
                k.transpose(k.ps(b, c0=kk * 128, c1=(kk + 1) * 128), xt[:, kf * 128:(kf + 1) * 128], ident_f.all)
            k.copy("dve" if half == 0 else "act", xs[:, half * 4:(half + 1) * 4, tc * 128:(tc + 1) * 128], k.ps3(b, 4))
    k.free(*xts)

    cl = k.tile([128, 8], F32, "cl")
    cc = k.tile([128, 8], F32, "cc")
    s_bf = k.tile([128, 8, 2], BF16, "s_bf")
    k.dma("sp", cl.all, dm(dr["c_fm"]))
    k.dma("sp", cc.all, dm(dr["cctx_fm"]))
    k.act(s_bf[:, :, 0], cl.all, AF.Silu)
    k.act(s_bf[:, :, 1], cc.all, AF.Silu)
    mwt = [k.tile([128, 8, 512], BF16, f"mw{i}") for i in range(2)]
    gi = 0
    for l in range(2):
        mw = dr["mod_w"][l].rearrange("(kc p) n -> p kc n", p=128)
        for g in range(12):
            wt = mwt[gi % 2]
            k.dma(SW, wt.all, dm(mw[:, :, g * 512:(g + 1) * 512]))
            b = 4 + gi % 2
            for j in range(4):
                for kk in range(8):
                    k.mm(k.ps(b, c0=2 * j, c1=2 * j + 2), wt[:, kk, j * 128:(j + 1) * 128], s_bf[:, kk, :],
                         start=(kk == 0), stop=(kk == 7))
            for j in range(4):
                ch = g * 4 + j
                k.ts("dve", mods[:, l, ch, :], k.ps(b, c0=2 * j, c1=2 * j + 2), modb[:, l, ch:ch + 1], None, ALU.add)
            gi += 1
    k.free(*mwt, cl, cc, s_bf)
    for l in range(2):
        for n, mi in ((0, 1), (1, 4)):
            src = V(mods.ap[:, l, mi * 8:(mi + 1) * 8, :].rearrange("p a b -> p (a b)"), mods[:, l, mi * 8:(mi + 1) * 8, :].pg)
            k.stt("dve", affA[:, l, n, :], src, 1.0, gdup[:, l, n, :], ALU.add, ALU.mult)

    def A_(l, n, kk, s):
        return affA[:, l, n, kk * 2 + s:kk * 2 + s + 1]

    def M_(l, mi, kk, s):
        return mods[:, l, mi * 8 + kk, s:s + 1]

    def norm_mod(l, n, blocks, f32_hook=None):
        sqs = [k.tile([128, 8, 512], BF16, f"sq{i}") for i in range(2)]
        rstds = [k.tile([128, 512], F32, f"rstd{i}") for i in range(2)]
        tmps = [k.tile([128, 512], F32, f"nt{i}") for i in range(3)]
        ti = 0
        for bi, (c0, ncl, s) in enumerate(blocks):
            sq = sqs[bi % 2]
            rstd = rstds[bi % 2]
            for kk in range(8):
                k.act(sq[:, kk, 0:ncl], xs[:, kk, c0:c0 + ncl], AF.Square)
            b = 4 + bi % 2
            for kk in range(8):
                k.mm(k.ps(b, c1=ncl), ones_b.all, sq[:, kk, 0:ncl], start=(kk == 0), stop=(kk == 7))
            k.act(rstd[:, 0:ncl], k.ps(b, c1=ncl), AF.Sqrt, bias=EPS, scale=1.0 / D)
            k.recip(rstd[:, 0:ncl], rstd[:, 0:ncl])
            for kk in range(8):
                tmp = tmps[ti % 3]
                ti += 1
                k.tt("dve" if kk % 2 == 0 else "pool", tmp[:, 0:ncl], xs[:, kk, c0:c0 + ncl], rstd[:, 0:ncl], ALU.mult)
                k.act(hbf[:, kk, c0:c0 + ncl], tmp[:, 0:ncl], AF.Identity,
                      bias=M_(l, 0 if n == 0 else 3, kk, s), scale=A_(l, n, kk, s))
                if f32_hook is not None:
                    f32_hook(bi, c0, ncl, kk, tmp, s)
        k.free(*sqs, *rstds, *tmps)

    def write_out():
        ots = [k.tile([128, 1024], F32, f"ot{i}") for i in range(2)]
        for tc in range(18):
            ot = ots[tc % 2]
            for half in range(2):
                b = (tc * 2 + half) % 4
                for kk in range(4):
                    kf = half * 4 + kk
                    k.transpose(k.ps(b, c0=kk * 128, c1=(kk + 1) * 128), xs[:, kf, tc * 128:(tc + 1) * 128], ident_f.all)
                k.copy("dve" if half == 0 else "act", ot[:, half * 512:(half + 1) * 512], k.ps(b))
            dst = outc_d[tc * 128:(tc + 1) * 128, :] if tc < 2 else out_d[(tc - 2) * 128:(tc - 1) * 128, :]
            k.dma("sp", dm(dst), ot.all)
        k.free(*ots)

    if stage == 0:
        write_out()
        return

    def qk_norm_rope(psv, nrows, gain, onesv, cosT, sinT, Rm, outv, c0, ncl, tl, bi, banks):
        sq, rstd, qn, t1 = tl
        k.act(sq[0:nrows, 0:ncl], psv, AF.Square)
        k.act(qn[0:nrows, 0:ncl], psv, AF.Identity, scale=gain)
        b1, b2 = banks
        k.mm(k.ps(b1, rows=nrows, c1=ncl), onesv, sq[0:nrows, 0:ncl])
        k.act(rstd[0:nrows, 0:ncl], k.ps(b1, rows=nrows, c1=ncl), AF.Sqrt, bias=EPS, scale=1.0 / nrows)
        k.copy("pool", sq[0:nrows, 0:ncl], qn[0:nrows, 0:ncl])
        k.mm(k.ps(b2, rows=nrows, c1=ncl), Rm, sq[0:nrows, 0:ncl])
        k.recip(rstd[0:nrows, 0:ncl], rstd[0:nrows, 0:ncl])
        k.tt("pool", t1[0:nrows, 0:ncl], qn[0:nrows, 0:ncl], cosT[0:nrows, c0:c0 + ncl], ALU.mult)
        k.tt("dve", qn[0:nrows, 0:ncl], k.ps(b2, rows=nrows, c1=ncl), sinT[0:nrows, c0:c0 + ncl], ALU.mult)
        k.tt("pool", t1[0:nrows, 0:ncl], t1[0:nrows, 0:ncl], qn[0:nrows, 0:ncl], ALU.add)
        k.tt("dve", outv, t1[0:nrows, 0:ncl], rstd[0:nrows, 0:ncl], ALU.mult)

    def attend(qh, kh, vaug, Dh, q_blocks, n_kc, scale, ycat, yrow0, ychunk, etl, rdt):
        for qi, (c0, ncl) in enumerate(q_blocks):
            ob = qi % 2
            LA = 4
            for it in range(n_kc + LA):
                if it < n_kc:
                    kc = it
                    sb = 2 + kc % 5
                    e = etl[kc % len(etl)]
                    k.mm(k.ps(sb, c1=ncl), kh[0:Dh, kc * 128:(kc + 1) * 128], qh[0:Dh, c0:c0 + ncl])
                    k.act(e[:, 0:ncl], k.ps(sb, c1=ncl), AF.Exp, scale=scale)
                if it >= LA:
                    kc = it - LA
                    e = etl[kc % len(etl)]
                    k.mm(k.ps(ob, c1=ncl), vaug[:, kc, 64:192], e[:, 0:ncl], start=(kc == 0), stop=(kc == n_kc - 1))
            rd, rs = rdt[qi % len(rdt)]
            k.recip(rd[64:128, 0:ncl], k.ps(ob, rows=64, r0=64, c1=ncl))
            k.copy("pool", rs[0:64, 0:ncl], rd[64:128, 0:ncl])
            k.tt("dve", ycat[yrow0:yrow0 + 64, ychunk, c0:c0 + ncl], k.ps(ob, rows=64, c1=ncl), rs[0:64, 0:ncl], ALU.mult)

    def project_out(l, ycat, w_d, krow0, blocks):
        wo = k.tile([128, 4, 1024], BF16, "wo")
        k.dma(SW, wo.all, dm(w_d[krow0:krow0 + 512, :].rearrange("(kc p) n -> p kc n", p=128)))
        i = 0
        for (c0, ncl, s) in blocks:
            for d in range(8):
                b = 4 + i % 4
                i += 1
                for kk in range(4):
                    k.mm(k.ps(b, c1=ncl), wo[:, kk, d * 128:(d + 1) * 128], ycat[:, kk, c0:c0 + ncl],
                         start=(kk == 0), stop=(kk == 3))
                k.stt("dve", xs[:, d, c0:c0 + ncl], k.ps(b, c1=ncl), M_(l, 2, d, s),
                      xs[:, d, c0:c0 + ncl], ALU.mult, ALU.add)
        k.free(wo)

    def ffn(l, w1_d, w3_d, w2_d, dff, blocks, hid, gate_bc=None):
        ngroups = (dff + 511) // 512
        w1t = [k.tile([128, 8, 512], BF16, f"w1_{i}") for i in range(2)]
        w3t = [k.tile([128, 8, 512], BF16, f"w3_{i}") for i in range(2)]
        w2t = [k.tile([128, 4, 1024], BF16, f"w2_{i}") for i in range(2)]
        sts = [k.tile([128, 512], F32, f"st{i}") for i in range(3)]
        w1v = w1_d.rearrange("(kc p) n -> p kc n", p=128)
        w3v = w3_d.rearrange("(kc p) n -> p kc n", p=128)
        si = 0
        ai = 0
        for g in range(ngroups):
            f0 = g * 512
            fw = min(512, dff - f0)
            nch = fw // 128
            w1, w3, w2 = w1t[g % 2], w3t[g % 2], w2t[g % 2]
            k.dma(SW, w1[:, :, 0:fw], dm(w1v[:, :, f0:f0 + fw]))
            k.dma(SW, w3[:, :, 0:fw], dm(w3v[:, :, f0:f0 + fw]))
            k.dma(SW, w2[:, 0:nch, :], dm(w2_d[f0:f0 + fw, :].rearrange("(kc p) n -> p kc n", p=128)))
            for m in range(nch):
                for (c0, ncl, s) in blocks:
                    bg = si % 2
                    bu = 2 + si % 2
                    st = sts[si % 3]
                    si += 1
                    for kk in range(8):
                        k.mm(k.ps(bg, c1=ncl), w1[:, kk, m * 128:(m + 1) * 128], hbf[:, kk, c0:c0 + ncl],
                             start=(kk == 0), stop=(kk == 7))
                    for kk in range(8):
                        k.mm(k.ps(bu, c1=ncl), w3[:, kk, m * 128:(m + 1) * 128], hbf[:, kk, c0:c0 + ncl],
                             start=(kk == 0), stop=(kk == 7))
                    k.act(st[:, 0:ncl], k.ps(bg, c1=ncl), AF.Silu)
                    if gate_bc is None:
                        k.tt("dve", hid[:, m, c0:c0 + ncl], st[:, 0:ncl], k.ps(bu, c1=ncl), ALU.mult)
                    else:
                        k.tt("dve", st[:, 0:ncl], st[:, 0:ncl], k.ps(bu, c1=ncl), ALU.mult)
                        k.tt("pool", hid[:, m, c0:c0 + ncl], st[:, 0:ncl], gate_bc[:, c0 - NCTX:c0 - NCTX + ncl], ALU.mult)
            for (c0, ncl, s) in blocks:
                for d in range(8):
                    b = 4 + ai % 4
                    ai += 1
                    for kk in range(nch):
                        k.mm(k.ps(b, c1=ncl), w2[:, kk, d * 128:(d + 1) * 128], hid[:, kk, c0:c0 + ncl],
                             start=(kk == 0), stop=(kk == nch - 1))
                    k.stt("dve", xs[:, d, c0:c0 + ncl], k.ps(b, c1=ncl), M_(l, 5, d, s),
                          xs[:, d, c0:c0 + ncl], ALU.mult, ALU.add)
        k.free(*w1t, *w3t, *w2t, *sts)

    ALLB = col_blocks(True, True)
    LATB = col_blocks(False, True)

    norm_mod(0, 0, ALLB)
    ycat = k.tile([128, 4, T], BF16, "ycat")
    layer0_mixer(k, dr, xs, hbf, ycat, ident_f, ones_b, M_, project_out, qk_norm_rope, attend, ALLB)
    if stage == 1:
        write_out()
        return
    norm_mod(0, 1, ALLB)
    ffn(0, dr["ffn_w1"], dr["ffn_w3"], dr["ffn_w2"], 2816, ALLB, ycat)
    if stage == 2:
        write_out()
        return
    import os
    SUB = int(os.environ.get("SUB1", "99"))
    norm_mod(1, 0, ALLB)
    layer1_mixer(k, dr, xs, hbf, ycat, ident_b, ones_b, M_, project_out, qk_norm_rope, attend, ALLB, LATB, SUB)
    if stage == 3:
        write_out()
        return
    moe_layer(k, dr, xs, hbf, ycat, ident_f, ones_b, M_, A_, norm_mod, ffn, LATB)
    write_out()


def layer0_mixer(k, dr, xs, hbf, ycat, ident_f, ones_b, M_, project_out, qk_norm_rope, attend, ALLB):
    import os
    SUB = int(os.environ.get("SUBSTAGE", "99"))
    dm = k.dram
    SW = "pool"
    win = dr["ab_w_in"].rearrange("(kc p) n -> p kc n", p=128)
    cosT = k.tile([64, T], F32, "cosT")
    sinT = k.tile([64, T], F32, "sinT")
    Rm = k.tile([64, 64], BF16, "Rm")
    gq = k.tile([64, 2], F32, "gqk")
    k.dma("sp", cosT.all, dm(dr["rope_g_cos"]))
    k.dma("sp", sinT.all, dm(dr["rope_g_sin"]))
    k.dma(SW, Rm.all, dm(dr["rope_g_R"]))
    k.dma("sp", gq.all, dm(dr["gqa_norms"]))
    wq_ = k.tile([128, 8, 512], BF16, "wq")
    wkv_ = k.tile([128, 8, 256], BF16, "wkv")
    k.dma(SW, wkv_.all, dm(win[:, :, 1536:1792]))
    k.dma(SW, wq_.all, dm(win[:, :, 1024:1536]))
    kh = [k.tile([64, T], BF16, f"kh{i}") for i in range(2)]
    vaug = [k.tile([128, 18, 192], BF16, f"vaug{i}") for i in range(2)]
    tl = (k.tile([64, 512], BF16, "n_sq"), k.tile([64, 512], F32, "n_rstd"), k.tile([64, 512], F32, "n_qn"),
          k.tile([64, 512], F32, "n_t1"))
    onesv = ones_b[0:64, 0:64]
    bi = 0
    for h in range(2):
        for (c0, ncl, s) in ALLB:
            b = 4 + bi % 2
            bi += 1
            for kk in range(8):
                k.mm(k.ps(b, rows=64, c1=ncl), wkv_[:, kk, h * 64:(h + 1) * 64], hbf[:, kk, c0:c0 + ncl],
                     start=(kk == 0), stop=(kk == 7))
            qk_norm_rope(k.ps(b, rows=64, c1=ncl), 64, gq[:, 1:2], onesv, cosT, sinT, Rm.all,
                         kh[h][0:64, c0:c0 + ncl], c0, ncl, tl, bi, (6, 7))
    if SUB == 1:
        return
    BIS = os.environ.get("BIS", "")
    for h in range(2):
        if "m" not in BIS:
            k.memset("pool", vaug[h].all, 1.0)
    for tc in range(18 if "t" not in BIS else 1):
        b = 4 + tc % 2
        if "x" not in BIS:
            for kk in range(8):
                k.mm(k.ps(b, c1=128), hbf[:, kk, tc * 128:(tc + 1) * 128], wkv_[:, kk, 128:256],
                     start=(kk == 0), stop=(kk == 7))
        if "c" not in BIS:
            for h in range(2):
                ce = "dve"
                if "d" in BIS:
                    ce = "dve"
                if "a" in BIS:
                    ce = "act"
                k.copy(ce, vaug[h][:, tc, 64:128], k.ps(b, c0=h * 64, c1=(h + 1) * 64))
    if SUB == 2:
        return
    k.free(wkv_)
    qhs = [k.tile([64, T], BF16, f"qh{i}") for i in range(2)]
    etl = [k.tile([128, 512], BF16, f"e{i}") for i in range(6)]
    rdt = [(k.tile([128, 512], F32, f"rd{i}"), k.tile([64, 512], F32, f"rs{i}")) for i in range(1)]
    scale = 64 ** -0.5
    for h in range(8 if SUB != 3 else 1):
        qh = qhs[h % 2]
        for (c0, ncl, s) in ALLB:
            b = 4 + bi % 2
            bi += 1
            for kk in range(8):
                k.mm(k.ps(b, rows=64, c1=ncl), wq_[:, kk, h * 64:(h + 1) * 64], hbf[:, kk, c0:c0 + ncl],
                     start=(kk == 0), stop=(kk == 7))
            qk_norm_rope(k.ps(b, rows=64, c1=ncl), 64, gq[:, 0:1], onesv, cosT, sinT, Rm.all,
                         qh[0:64, c0:c0 + ncl], c0, ncl, tl, bi, (6, 7))
        kv = h // 4
        attend(qh, kh[kv], vaug[kv], 64, [(0, NCTX)], 2, scale, ycat, (h % 2) * 64, h // 2, etl, rdt)
        attend(qh, kh[kv], vaug[kv], 64, [(NCTX + 512 * i, 512) for i in range(4)], 18, scale, ycat,
               (h % 2) * 64, h // 2, etl, rdt)
    k.free(cosT, sinT, Rm, gq, wq_, *kh, *vaug, *tl, *qhs, *etl, *[t for p in rdt for t in p])
    if SUB == 3:
        return
    project_out(0, ycat, dr["ab_w_out"], 512, ALLB)
    if SUB == 4:
        return

    cw = k.tile([128, 4, 4], F32, "convw")
    cb = k.tile([128, 4], F32, "convb")
    lb = k.tile([128, 2, 2, 4], F32, "lru_b")
    lam = k.tile([128, 2, 4], F32, "lam")
    cneg = k.tile([128, 2, 4], F32, "cneg")
    k.dma("sp", cw.all, dm(dr["lru_convw_fm"]))
    k.dma("sp", cb.all, dm(dr["lru_convb_fm"]))
    k.dma("sp", lb.all, dm(dr["lru_b_fm"]))
    k.dma("sp", lam.all, dm(dr["lru_lam_fm"]))
    lamf = V(lam.ap.rearrange("p a b -> p (a b)"), lam.all.pg)
    cnegf = V(cneg.ap.rearrange("p a b -> p (a b)"), cneg.all.pg)
    k.act(cnegf, lamf, AF.Exp, scale=-1.0)
    k.act(cnegf, cnegf, AF.Ln, bias=1.0)
    k.ts("dve", cnegf, cnegf, -8.0, None, ALU.mult)
    xr = k.tile([128, T], F32, "xr")
    u = k.tile([128, T], F32, "u")
    ubf = k.tile([128, T], BF16, "ubf")
    rec = k.tile([128, T], F32, "rec")
    at = k.tile([128, T], F32, "a")
    bt = k.tile([128, T], F32, "b")
    ht = k.tile([128, T], F32, "h")
    wx = k.tile([128, 8, 256], BF16, "wxg")
    bd = [k.tile([128, 128], BF16, f"bd{i}") for i in range(4)]
    tmp = [k.tile([128, 512], F32, f"lt{i}") for i in range(4)]
    seqs = [(0, NCTX), (NCTX, NLAT)]
    for j in range(4):
        k.dma(SW, wx[:, :, 0:128], dm(k_win_slice(dr, j * 128)))
        k.dma(SW, wx[:, :, 128:256], dm(k_win_slice(dr, 512 + j * 128)))
        for d_ in range(2):
            for gi_, nm in enumerate(("lru_w_a", "lru_w_x")):
                t_ = bd[d_ * 2 + gi_]
                k.memset("pool", t_.all, 0.0)
                for hb_ in range(2):
                    k.dma(SW, t_[hb_ * 64:(hb_ + 1) * 64, hb_ * 64:(hb_ + 1) * 64], dm(dr[nm][d_, 2 * j + hb_]))
        for bi_, (c0, ncl, s) in enumerate(ALLB):
            b = 4 + bi_ % 2
            for kk in range(8):
                k.mm(k.ps(b, c1=ncl), wx[:, kk, 0:128], hbf[:, kk, c0:c0 + ncl], start=(kk == 0), stop=(kk == 7))
            k.copy("act", xr[:, c0:c0 + ncl], k.ps(b, c1=ncl))
        k.ts("dve", u.all, xr.all, cw[:, j, 2:3], cb[:, j:j + 1], ALU.mult, ALU.add)
        for tap in (0, 1, 3):
            sh = tap - 2
            for (s0, L_) in seqs:
                lo = s0 + max(0, -sh)
                hi = s0 + L_ - max(0, sh)
                k.stt("dve", u[:, lo:hi], xr[:, lo + sh:hi + sh], cw[:, j, tap:tap + 1], u[:, lo:hi], ALU.mult, ALU.add)
        k.copy("pool", ubf.all, u.all)
        for d_ in range(2):
            for bi_, (c0, ncl, s) in enumerate(ALLB):
                ba, bx = 4 + bi_ % 2, 6 + bi_ % 2
                k.mm(k.ps(ba, c1=ncl), bd[d_ * 2].all, ubf[:, c0:c0 + ncl])
                k.mm(k.ps(bx, c1=ncl), bd[d_ * 2 + 1].all, ubf[:, c0:c0 + ncl])
                r_, i_, q_, _ = tmp
                k.act(r_[:, 0:ncl], k.ps(ba, c1=ncl), AF.Sigmoid, bias=lb[:, 0, d_, j:j + 1])
                k.act(i_[:, 0:ncl], k.ps(bx, c1=ncl), AF.Sigmoid, bias=lb[:, 1, d_, j:j + 1])
                k.act(at[:, c0:c0 + ncl], r_[:, 0:ncl], AF.Exp, scale=cneg[:, d_, j:j + 1])
                k.tt("pool", q_[:, 0:ncl], at[:, c0:c0 + ncl], at[:, c0:c0 + ncl], ALU.mult)
                k.act(q_[:, 0:ncl], q_[:, 0:ncl], AF.Sqrt, bias=1.0, scale=-1.0)
                k.tt("dve", i_[:, 0:ncl], i_[:, 0:ncl], u[:, c0:c0 + ncl], ALU.mult)
                k.tt("dve", bt[:, c0:c0 + ncl], q_[:, 0:ncl], i_[:, 0:ncl], ALU.mult)
            if d_ == 0:
                k.scan(rec.all, at.all, bt.all, 0.0)
            else:
                rv = lambda v: v.m(lambda ap: ap[:, ::-1])
                k.scan(rv(ht[:, 0:NCTX]), rv(at[:, 0:NCTX]), rv(bt[:, 0:NCTX]), 0.0)
                k.scan(rv(ht[:, NCTX:T]), rv(at[:, NCTX:T]), rv(bt[:, NCTX:T]), ht[:, 0:1])
                k.tt("pool", rec.all, rec.all, ht.all, ALU.add)
        for bi_, (c0, ncl, s) in enumerate(ALLB):
            b = 4 + bi_ % 2
            for kk in range(8):
                k.mm(k.ps(b, c1=ncl), wx[:, kk, 128:256], hbf[:, kk, c0:c0 + ncl], start=(kk == 0), stop=(kk == 7))
            g_, t_, w_, _ = tmp
            k.copy("act", g_[:, 0:ncl], k.ps(b, c1=ncl))
            k.tt("pool", t_[:, 0:ncl], g_[:, 0:ncl], g_[:, 0:ncl], ALU.mult)
            k.ts("dve", t_[:, 0:ncl], t_[:, 0:ncl], 0.044715, 1.0, ALU.mult, ALU.add)
            k.tt("dve", t_[:, 0:ncl], t_[:, 0:ncl], g_[:, 0:ncl], ALU.mult)
            k.act(t_[:, 0:ncl], t_[:, 0:ncl], AF.Sigmoid, scale=2.0 * math.sqrt(2.0 / math.pi))
            k.tt("pool", t_[:, 0:ncl], t_[:, 0:ncl], g_[:, 0:ncl], ALU.mult)
            k.tt("dve", ycat[:, j, c0:c0 + ncl], t_[:, 0:ncl], rec[:, c0:c0 + ncl], ALU.mult)
    k.free(cw, cb, lb, lam, cneg, xr, u, ubf, rec, at, bt, ht, wx, *bd, *tmp)
    project_out(0, ycat, dr["ab_w_out"], 0, ALLB)


def k_win_slice(dr, c0):
    return dr["ab_w_in"].rearrange("(kc p) n -> p kc n", p=128)[:, :, c0:c0 + 128]


_CONSTS = None
_NC_CACHE = {}


def prep_inputs(inp, b):
    global _CONSTS
    if _CONSTS is None:
        _CONSTS = host_consts()
    f = lambda a: np.ascontiguousarray(np.asarray(a, dtype=np.float32))
    m = dict(_CONSTS)
    m.pop("rope_g_R"); m.pop("rope_m_R")
    m["rope_g_R"] = _CONSTS["rope_g_R"]; m["rope_m_R"] = _CONSTS["rope_m_R"]
    m["x"] = f(inp["x"][b])
    m["ctx"] = f(inp["ctx"][b])
    m["c_fm"] = f(fm(inp["c"][b]))
    m["cctx_fm"] = f(fm(inp["c_ctx"]))
    m["mod_w"] = f(inp["mod_w"])
    m["modb"] = f(fm(inp["mod_b"]).transpose(1, 0, 2))
    g = np.stack([fm(inp["norm1_g"]), fm(inp["norm2_g"])], axis=1)
    g = np.repeat(g[..., None], 2, axis=-1).reshape(2, 2, 128, 16)
    m["gdup"] = f(g.transpose(2, 0, 1, 3))
    m["ab_w_in"] = f(inp["ab_w_in"][0])
    m["ab_w_out"] = f(inp["ab_w_out"][0])
    m["gqa_norms"] = f(np.stack([inp["gqa_q_norm"][0], inp["gqa_k_norm"][0]], axis=1))
    m["lru_convw_fm"] = f(fm(inp["lru_conv_w"][0]).transpose(1, 2, 0))
    m["lru_convb_fm"] = f(fm(inp["lru_conv_b"][0]))
    lb = np.stack([fm(inp["lru_b_a"][0].reshape(2, 512)), fm(inp["lru_b_x"][0].reshape(2, 512))], axis=0)
    m["lru_b_fm"] = f(lb.transpose(2, 0, 1, 3))
    m["lru_lam_fm"] = f(fm(inp["lru_lambda"][0]).transpose(1, 0, 2))
    m["lru_w_a"] = f(inp["lru_w_a"][0])
    m["lru_w_x"] = f(inp["lru_w_x"][0])
    m["ffn_w1"] = f(inp["ffn_w1"][0])
    m["ffn_w3"] = f(inp["ffn_w3"][0])
    m["ffn_w2"] = f(inp["ffn_w2"][0])
    m["cd_w_in"] = f(inp["cd_w_in"][0])
    m["cd_w_out"] = f(inp["cd_w_out"][0])
    mg = np.zeros((128, 5), np.float32)
    mg[:, 0:2] = fm(inp["mla_q_a_norm"][0])
    mg[:, 2] = inp["mla_kv_a_norm"][0]
    mg[0:96, 3] = inp["mla_q_norm"][0]
    mg[0:96, 4] = inp["mla_k_norm"][0]
    m["mla_gains"] = mg
    m["mla_q_b"] = f(inp["mla_q_b"][0])
    m["mla_kv_b"] = f(inp["mla_kv_b"][0])
    m["hy_p"] = f(np.stack([inp["hy_filt_b1"][0], inp["hy_filt_b2"][0], inp["hy_sin_freq"][0]], axis=1))
    m["hy_filt_w1"] = f(inp["hy_filt_w1"][0])
    m["hy_filt_w2"] = f(inp["hy_filt_w2"][0])
    m["hy_filt_w3"] = f(inp["hy_filt_w3"][0])
    m["hy_conv_w"] = f(inp["hy_conv_w"][0])
    m["hy_conv_b"] = f(inp["hy_conv_b"][0])
    m["hy_skip"] = f(inp["hy_skip"][0])
    m["moe_router"] = f(inp["moe_router"][0])
    m["moe_w1"] = f(inp["moe_w1"][0])
    m["moe_w3"] = f(inp["moe_w3"][0])
    m["moe_w2"] = f(inp["moe_w2"][0])
    return m


def shapes_of(m):
    return {k_: (tuple(v.shape), str(v.dtype)) for k_, v in m.items()}


def kernel(**inputs):
    maps = [prep_inputs(inputs, b) for b in range(8)]
    shapes = shapes_of(maps[0])
    key = tuple(sorted(shapes.items()))
    if key not in _NC_CACHE:
        _NC_CACHE[key] = build(shapes)
    nc = _NC_CACHE[key]
    res = run_bass_kernel_spmd(nc, maps, core_ids=list(range(8)))
    return np.stack([np.asarray(r["out"], dtype=np.float32) for r in res.results], axis=0)


def ps_bf(k, i, rows, c0, c1):
    b = k.banks[i]
    return V(b[0:rows, :].bitcast(BF16)[:, c0:c1], [(PSUM_PAGE0 + i, PSUM_PAGE0 + i + 1)])


def layer1_mixer(k, dr, xs, hbf, ycat, ident_b, ones_b, M_, project_out, qk_norm_rope, attend, ALLB, LATB, SUB):
    dm = k.dram
    SW = "pool"
    win = dr["cd_w_in"].rearrange("(kc p) n -> p kc n", p=128)
    cosT = k.tile([96, T], F32, "cosM")
    sinT = k.tile([96, T], F32, "sinM")
    Rm = k.tile([96, 96], BF16, "RmM")
    gn = k.tile([128, 5], F32, "mla_g")
    k.dma("sp", cosT.all, dm(dr["rope_m_cos"]))
    k.dma("sp", sinT.all, dm(dr["rope_m_sin"]))
    k.dma(SW, Rm.all, dm(dr["rope_m_R"]))
    k.dma("sp", gn.all, dm(dr["mla_gains"]))
    wqa = k.tile([128, 8, 256], BF16, "wqa")
    wkva = k.tile([128, 8, 128], BF16, "wkva")
    wrp = k.tile([128, 8, 96], BF16, "wrope_pad")
    qb = k.tile([128, 2, 768], BF16, "q_b")
    kvbn = k.tile([128, 8, 96], BF16, "kvb_nope_pad")
    kvbv = k.tile([128, 8, 64], BF16, "kvb_v")
    k.dma(SW, wqa.all, dm(win[:, :, 1536:1792]))
    k.dma(SW, wkva.all, dm(win[:, :, 1792:1920]))
    k.memset("dve", wrp.all, 0.0)
    k.dma(SW, wrp[:, :, 64:96], dm(win[:, :, 1920:1952]))
    k.dma(SW, qb.all, dm(dr["mla_q_b"].rearrange("(kc p) n -> p kc n", p=128)))
    kvb3 = dr["mla_kv_b"].rearrange("k (h c) -> k h c", h=8)
    k.memset("dve", kvbn.all, 0.0)
    k.dma(SW, kvbn[:, :, 0:64], dm(kvb3[:, :, 0:64]))
    k.dma(SW, kvbv.all, dm(kvb3[:, :, 64:128]))
    qa = k.tile([128, 2, NLAT], BF16, "qa")
    kva = k.tile([128, T], BF16, "kva")
    tl = (k.tile([128, 512], BF16, "n_sq"), k.tile([128, 512], F32, "n_rstd"), k.tile([128, 512], F32, "n_qn"),
          k.tile([128, 512], F32, "n_t1"))
    sq2 = k.tile([128, 2, 512], BF16, "sq2")
    raw = k.tile([128, 2, 512], F32, "qa_raw")
    for bi, (c0, ncl, s) in enumerate(LATB):
        for m in range(2):
            b = 4 + m
            for kk in range(8):
                k.mm(k.ps(b, c1=ncl), wqa[:, kk, m * 128:(m + 1) * 128], hbf[:, kk, c0:c0 + ncl],
                     start=(kk == 0), stop=(kk == 7))
            k.act(sq2[:, m, 0:ncl], k.ps(b, c1=ncl), AF.Square)
            k.copy("act", raw[:, m, 0:ncl], k.ps(b, c1=ncl))
        for m in range(2):
            k.mm(k.ps(6, c1=ncl), ones_b.all, sq2[:, m, 0:ncl], start=(m == 0), stop=(m == 1))
        rstd = tl[1]
        k.act(rstd[:, 0:ncl], k.ps(6, c1=ncl), AF.Sqrt, bias=EPS, scale=1.0 / 256)
        k.recip(rstd[:, 0:ncl], rstd[:, 0:ncl])
        for m in range(2):
            k.stt("dve", qa[:, m, c0 - NCTX:c0 - NCTX + ncl], raw[:, m, 0:ncl], gn[:, m:m + 1], rstd[:, 0:ncl],
                  ALU.mult, ALU.mult)
    for bi, (c0, ncl, s) in enumerate(ALLB):
        b = 4 + bi % 2
        for kk in range(8):
            k.mm(k.ps(b, c1=ncl), wkva[:, kk, :], hbf[:, kk, c0:c0 + ncl], start=(kk == 0), stop=(kk == 7))
        sq, rstd = tl[0], tl[1]
        k.act(sq[:, 0:ncl], k.ps(b, c1=ncl), AF.Square)
        k.mm(k.ps(6, c1=ncl), ones_b.all, sq[:, 0:ncl])
        k.act(rstd[:, 0:ncl], k.ps(6, c1=ncl), AF.Sqrt, bias=EPS, scale=1.0 / 128)
        k.recip(rstd[:, 0:ncl], rstd[:, 0:ncl])
        k.stt("dve", kva[:, c0:c0 + ncl], k.ps(b, c1=ncl), gn[:, 2:3], rstd[:, 0:ncl], ALU.mult, ALU.mult)
    k.free(sq2, raw, wqa, wkva)
    kh = k.tile([96, T], BF16, "kh")
    qh = k.tile([96, T], BF16, "qh")
    vaug = k.tile([128, 18, 192], BF16, "vaug")
    etl = [k.tile([128, 512], BF16, f"e{i}") for i in range(6)]
    rdt = [(k.tile([128, 512], F32, "rd"), k.tile([64, 512], F32, "rs"))]
    k.memset("pool", vaug.all, 1.0)
    onesv = ones_b[0:96, 0:96]
    scale = 96 ** -0.5
    bi = 0
    for h in range(8):
        for (c0, ncl, s) in ALLB:
            b = 4 + bi % 2
            bi += 1
            for kk in range(8):
                k.mm(k.ps(b, rows=96, c1=ncl), wrp[:, kk, :], hbf[:, kk, c0:c0 + ncl], start=(kk == 0), stop=False)
            k.mm(k.ps(b, rows=96, c1=ncl), kvbn[:, h, :], kva[:, c0:c0 + ncl], start=False, stop=True)
            qk_norm_rope(k.ps(b, rows=96, c1=ncl), 96, gn[0:96, 4:5], onesv, cosT, sinT, Rm.all,
                         kh[0:96, c0:c0 + ncl], c0, ncl, tl, bi, (6, 7))
        for (c0, ncl, s) in LATB:
            b = 4 + bi % 2
            bi += 1
            for m in range(2):
                k.mm(k.ps(b, rows=96, c1=ncl), qb[:, m, h * 96:(h + 1) * 96], qa[:, m, c0 - NCTX:c0 - NCTX + ncl],
                     start=(m == 0), stop=(m == 1))
            qk_norm_rope(k.ps(b, rows=96, c1=ncl), 96, gn[0:96, 3:4], onesv, cosT, sinT, Rm.all,
                         qh[0:96, c0:c0 + ncl], c0, ncl, tl, bi, (6, 7))
        for tc in range(18):
            b = 4 + tc % 2
            k.mm(k.ps(b, c1=64), kva[:, tc * 128:(tc + 1) * 128], kvbv[:, h, :])
            k.copy("dve", vaug[:, tc, 64:128], k.ps(b, c1=64))
        attend(qh, kh, vaug, 96, [(NCTX + 512 * i, 512) for i in range(4)], 18, scale, ycat,
               (h % 2) * 64, h // 2, etl, rdt)
    k.free(cosT, sinT, Rm, gn, wrp, qb, kvbn, kvbv, qa, kva, *tl, kh, qh, vaug, *etl, *rdt[0])
    project_out(1, ycat, dr["cd_w_out"], 512, LATB)
    if SUB == 1:
        return

    hp = k.tile([64, 3], F32, "hy_p")
    k.dma("sp", hp.all, dm(dr["hy_p"]))
    zT = k.tile([33, NLAT], BF16, "zT")
    k.dma(SW, zT.all, dm(dr["hy_zT"]))
    fw1 = k.tile([33, 64], BF16, "fw1")
    fw2 = k.tile([64, 64], BF16, "fw2")
    k.dma(SW, fw1.all, dm(dr["hy_filt_w1"]))
    k.dma(SW, fw2.all, dm(dr["hy_filt_w2"]))
    h1 = k.tile([64, NLAT], BF16, "h1T")
    h2 = k.tile([64, NLAT], BF16, "h2T")
    ft = [k.tile([64, 512], F32, f"ft{i}") for i in range(2)]
    MAGIC = 12582912.0
    TWO_PI = 2.0 * math.pi

    def sin_layer(dst, lhsT, src, bcol):
        for i in range(4):
            cs = slice(i * 512, (i + 1) * 512)
            b = 4 + i % 2
            k.mm(k.ps(b, rows=64), lhsT, src[:, cs])
            y, t_ = ft
            k.ts("dve", y.all, k.ps(b, rows=64), hp[:, bcol:bcol + 1], hp[:, 2:3], ALU.add, ALU.mult)
            k.ts("dve", t_.all, y.all, 1.0 / TWO_PI, MAGIC, ALU.mult, ALU.add)
            k.ts("dve", t_.all, t_.all, -MAGIC, -TWO_PI, ALU.add, ALU.mult)
            k.tt("dve", y.all, y.all, t_.all, ALU.add)
            k.act(dst[:, cs], y.all, AF.Sin)

    sin_layer(h1, fw1.all, zT, 0)
    sin_layer(h2, fw2.all, h1, 1)
    k.free(zT, fw1, fw2, h1, *ft, hp)

    CH = 256
    vt = k.tile([128, 16, 512], BF16, "hy_v")
    x1 = k.tile([128, 16, 512], BF16, "hy_x1")
    x2 = k.tile([128, 16, 512], BF16, "hy_x2")
    hpadL = k.tile([128, 8, 129], BF16, "hpadL")
    hpadR = k.tile([128, 8, 129], BF16, "hpadR")
    k.memset("dve", hpadL.all, 0.0)
    k.memset("dve", hpadR.all, 0.0)
    k.copy("pool", hpadL[:, :, 1:129], hbf[:, :, NCTX:NCTX + 128])
    k.copy("pool", hpadR[:, :, 0:127], hbf[:, :, T - 127:T])
    wf = k.tile([128, 8, CH], BF16, "hy_wf")
    wj = [k.tile([128, 8, CH], BF16, f"hy_wj{j}") for j in range(3)]
    cwb = k.tile([128, 3, CH], F32, "hy_cwb")
    bb = k.tile([128, CH], F32, "hy_bias")
    convw = dr["hy_conv_w"]
    for half in range(2):
        hs = slice(half * CH, (half + 1) * CH)
        for part, dst in enumerate((vt, x1, x2)):
            cbase = part * 512 + half * CH
            k.dma(SW, wf.all, dm(win[:, :, cbase:cbase + CH]))
            k.dma("sp", cwb.all, dm(convw[:, cbase:cbase + CH].partition_broadcast(128)))
            k.dma("sp", bb.all, dm(dr["hy_conv_b"][cbase:cbase + CH].partition_broadcast(128)))
            for j in range(3):
                for kk in range(8):
                    k.tt("dve" if kk % 2 else "pool", wj[j][:, kk, :], wf[:, kk, :], cwb[:, j, :], ALU.mult)
            for tcn in range(16):
                b = 4 + tcn % 2
                n_mm = 0
                for j in range(3):
                    for kk in range(8):
                        sh = j - 1
                        if tcn == 0 and sh == -1:
                            lhs = hpadL[:, kk, 0:128]
                        elif tcn == 15 and sh == 1:
                            lhs = hpadR[:, kk, 0:128]
                        else:
                            c0 = NCTX + tcn * 128 + sh
                            lhs = hbf[:, kk, c0:c0 + 128]
                        k.mm(k.ps(b, c1=CH), lhs, wj[j][:, kk, :], start=(n_mm == 0), stop=(n_mm == 23))
                        n_mm += 1
                k.tt("dve", dst[:, tcn, hs], k.ps(b, c1=CH), bb.all, ALU.add)
    k.free(hpadL, hpadR, wf, *wj, cwb, bb)
    carve = [hbf.off]

    def ctile(shape, dt, name):
        t_ = Tile(k, carve[0], shape, dt, name)
        carve[0] += ((t_.nbytes + PAGE - 1) // PAGE) * PAGE
        assert carve[0] <= hbf.off + hbf.alloc
        return t_

    hsum = ctile([128, 16, CH], BF16, "hsum")
    hdif = ctile([128, 16, CH], BF16, "hdif")
    Yre = ctile([128, 16, CH], BF16, "Yre")
    Yim = ctile([128, 16, CH], BF16, "Yim")
    tmps = [ctile([128, 512], F32, "hyt0"), ctile([128, 512], F32, "hyt1"),
            k.tile([128, 512], F32, "hyt2"), k.tile([128, 512], F32, "hyt3")]
    dftA = [k.tile([128, 16, 128], BF16, f"dftA{i}") for i in range(2)]
    dftB = [k.tile([128, 16, 128], BF16, f"dftB{i}") for i in range(2)]
    w3 = k.tile([64, 512], BF16, "fw3")
    skb = k.tile([128, CH], F32, "hy_skipb")
    winr = [k.tile([128, CH], F32, f"hywin{i}") for i in range(2)]
    for half in range(2):
        hs = slice(half * CH, (half + 1) * CH)
        for o in range(2):
            xo = x1 if o == 0 else x2
            for dr_ in range(2):
                cb_ = o * 1024 + dr_ * 512 + half * CH
                k.dma(SW, w3[:, dr_ * CH:(dr_ + 1) * CH], dm(dr["hy_filt_w3"][:, cb_:cb_ + CH]))
            k.dma("sp", skb.all, dm(dr["hy_skip"][o, half * CH:(half + 1) * CH].partition_broadcast(128)))
            for tcn in range(16):
                b = 4 + tcn % 2
                wn = winr[tcn % 2]
                k.dma("sp", wn.all, dm(dr["hy_window"][tcn * 128:(tcn + 1) * 128, half * CH:(half + 1) * CH]))
                k.mm(k.ps(b), h2[0:64, tcn * 128:(tcn + 1) * 128], w3.all)
                hf_, hb_ = tmps[0], tmps[1]
                k.tt("dve", hf_[:, 0:CH], k.ps(b, c0=0, c1=CH), wn.all, ALU.mult)
                k.tt("dve", hb_[:, 0:CH], k.ps(b, c0=CH, c1=2 * CH), wn.all, ALU.mult)
                k.tt("pool", hsum[:, tcn, :], hf_[:, 0:CH], hb_[:, 0:CH], ALU.add)
                k.tt("pool", hdif[:, tcn, :], hb_[:, 0:CH], hf_[:, 0:CH], ALU.subtract)
                if tcn == 0:
                    k.copy("pool", hsum[0:1, 0, :], hf_[0:1, 0:CH])
            for fc in range(16):
                ca, sa = dftA[fc % 2], dftB[fc % 2]
                k.dma("sp", V(ca.ap.rearrange("p a b -> p (a b)"), ca.all.pg), dm(dr["dft_cf"][fc]))
                k.dma("sp", V(sa.ap.rearrange("p a b -> p (a b)"), sa.all.pg), dm(dr["dft_sf"][fc]))
                bx, by = 4 + (fc % 2) * 2, 5 + (fc % 2) * 2
                for tcn in range(16):
                    k.mm(k.ps(bx, c0=0, c1=CH), ca[:, tcn, :], vt[:, tcn, hs], start=(tcn == 0), stop=(tcn == 15))
                for tcn in range(16):
                    k.mm(k.ps(bx, c0=CH, c1=2 * CH), sa[:, tcn, :], vt[:, tcn, hs], start=(tcn == 0), stop=(tcn == 15))
                for tcn in range(16):
                    k.mm(k.ps(by, c0=0, c1=CH), ca[:, tcn, :], hsum[:, tcn, :], start=(tcn == 0), stop=(tcn == 15))
                for tcn in range(16):
                    k.mm(k.ps(by, c0=CH, c1=2 * CH), sa[:, tcn, :], hdif[:, tcn, :], start=(tcn == 0), stop=(tcn == 15))
                kt, t1, t2 = tmps[2], tmps[0], tmps[1]
                k.copy("act", kt.all, k.ps(by))
                A_ = k.ps(bx, c0=0, c1=CH)
                B_ = k.ps(bx, c0=CH, c1=2 * CH)
                k.tt("dve", t1[:, 0:CH], A_, kt[:, 0:CH], ALU.mult)
                k.tt("dve", t1[:, CH:2 * CH], B_, kt[:, CH:2 * CH], ALU.mult)
                k.tt("pool", Yre[:, fc, :], t1[:, 0:CH], t1[:, CH:2 * CH], ALU.add)
                k.tt("dve", t2[:, 0:CH], A_, kt[:, CH:2 * CH], ALU.mult)
                k.tt("dve", t2[:, CH:2 * CH], B_, kt[:, 0:CH], ALU.mult)
                k.tt("pool", Yim[:, fc, :], t2[:, 0:CH], t2[:, CH:2 * CH], ALU.subtract)
            for tcn in range(16):
                ca, sa = dftA[tcn % 2], dftB[tcn % 2]
                k.dma("sp", V(ca.ap.rearrange("p a b -> p (a b)"), ca.all.pg), dm(dr["dft_ci"][tcn]))
                k.dma("sp", V(sa.ap.rearrange("p a b -> p (a b)"), sa.all.pg), dm(dr["dft_si"][tcn]))
                b = 4 + tcn % 2
                for fc in range(16):
                    k.mm(k.ps(b, c1=CH), ca[:, fc, :], Yre[:, fc, :], start=(fc == 0), stop=False)
                for fc in range(16):
                    k.mm(k.ps(b, c1=CH), sa[:, fc, :], Yim[:, fc, :], start=False, stop=(fc == 15))
                t1 = tmps[3]
                k.tt("pool", t1[:, 0:CH], vt[:, tcn, hs], skb.all, ALU.mult)
                k.tt("dve", t1[:, 0:CH], k.ps(b, c1=CH), t1[:, 0:CH], ALU.add)
                k.tt("dve", vt[:, tcn, hs], t1[:, 0:CH], xo[:, tcn, hs], ALU.mult)
    for tcn in range(16):
        b = 4 + tcn % 2
        for cc in range(4):
            k.transpose(ps_bf(k, b, 128, cc * 128, (cc + 1) * 128), vt[:, tcn, cc * 128:(cc + 1) * 128], ident_b.all)
        for cc in range(4):
            k.copy("dve", ycat[:, cc, NCTX + tcn * 128:NCTX + (tcn + 1) * 128],
                   ps_bf(k, b, 128, cc * 128, (cc + 1) * 128))
    k.free(vt, x1, x2, *dftA, *dftB, w3, skb, tmps[2], tmps[3], *winr, h2)
    project_out(1, ycat, dr["cd_w_out"], 0, LATB)


def moe_layer(k, dr, xs, hbf, ycat, ident_f, ones_b, M_, A_, norm_mod, ffn, LATB):
    dm = k.dram
    AX = mybir.AxisListType.X
    rw = k.tile([128, 8, 8], F32, "rw")
    r_hi = k.tile([128, 8, 8], BF16, "r_hi")
    r_lo = k.tile([128, 8, 8], BF16, "r_lo")
    k.dma("sp", rw.all, dm(dr["moe_router"].rearrange("(kc p) e -> p kc e", p=128)))
    k.copy("dve", r_hi.all, rw.all)
    k.tt("dve", r_lo.all, rw.all, r_hi.all, ALU.subtract)
    logits = k.tile([128, 16, 8], F32, "logits")
    gates = k.tile([128, 16, 8], F32, "gates")
    hlo = k.tile([128, 8, 512], BF16, "hlo")
    hf32 = [k.tile([128, 512], F32, f"hf32_{i}") for i in range(2)]

    def hook(bi, c0, ncl, kk, tmp, s):
        hf = hf32[kk % 2]
        k.act(hf[:, 0:ncl], tmp[:, 0:ncl], AF.Identity, bias=M_(1, 3, kk, s), scale=A_(1, 1, kk, s))
        k.tt("pool", hlo[:, kk, 0:ncl], hf[:, 0:ncl], hbf[:, kk, c0:c0 + ncl], ALU.subtract)
        if kk == 7:
            for q in range(ncl // 128):
                tcn = (c0 - NCTX) // 128 + q
                b = 6 + tcn % 2
                n = 0
                for which in range(3):
                    for k2 in range(8):
                        if which == 1:
                            lhs = hlo[:, k2, q * 128:(q + 1) * 128]
                        else:
                            lhs = hbf[:, k2, c0 + q * 128:c0 + (q + 1) * 128]
                        rr = r_lo if which == 2 else r_hi
                        k.mm(k.ps(b, c1=8), lhs, rr[:, k2, :], start=(n == 0), stop=(n == 23))
                        n += 1
                k.copy("dve", logits[:, tcn, :], k.ps(b, c1=8))

    norm_mod(1, 1, LATB, f32_hook=hook)
    k.free(hlo, *hf32, rw, r_hi, r_lo)
    mx = k.tile([128, 8], F32, "mx")
    sel = k.tile([128, 8], F32, "sel")
    ex = k.tile([128, 8], F32, "ex")
    sm = k.tile([128, 4], F32, "sm")
    for tcn in range(16):
        lg = logits[:, tcn, :]
        k.op("dve", (lambda o_, i_: (lambda e_: e_.max(out=o_, in_=i_)))(mx.ap, lg.ap), R=[lg], W=[mx.all])
        k.ts("dve", sel.all, lg, mx[:, 1:2], None, ALU.is_ge)
        k.ts("dve", sm[:, 0:1], mx[:, 0:1], -1.0, None, ALU.mult)
        k.act(ex.all, lg, AF.Exp, bias=sm[:, 0:1])
        k.tt("dve", ex.all, ex.all, sel.all, ALU.mult)
        k.op("dve", (lambda o_, i_: (lambda e_: e_.reduce_sum(out=o_, in_=i_, axis=AX)))(sm[:, 1:2].ap, ex.ap),
             R=[ex.all], W=[sm[:, 1:2]])
        k.recip(sm[:, 2:3], sm[:, 1:2])
        k.ts("dve", gates[:, tcn, :], ex.all, sm[:, 2:3], None, ALU.mult)
    k.free(mx, sel, ex, sm)
    Gbs = [k.tile([128, NLAT], F32, f"Gb{i}") for i in range(2)]
    Dt = k.tile([128, 128], F32, "Dt")
    Dhi = k.tile([128, 128], BF16, "Dhi")
    Dlo = k.tile([128, 128], BF16, "Dlo")
    for e in range(8):
        Gb = Gbs[e % 2]
        for tcn in range(16):
            b = 6 + tcn % 2
            k.ts("dve", Dt.all, ident_f.all, gates[:, tcn, e:e + 1], None, ALU.mult)
            k.copy("dve", Dhi.all, Dt.all)
            k.tt("dve", Dlo.all, Dt.all, Dhi.all, ALU.subtract)
            k.mm(k.ps(b, c1=128), ones_b.all, Dhi.all, start=True, stop=False)
            k.mm(k.ps(b, c1=128), ones_b.all, Dlo.all, start=False, stop=True)
            k.copy("act", Gb[:, tcn * 128:(tcn + 1) * 128], k.ps(b, c1=128))
        ffn(1, dr["moe_w1"][e], dr["moe_w3"][e], dr["moe_w2"][e], 3584, LATB, ycat, gate_bc=Gb)
    k.free(*Gbs, Dt, Dhi, Dlo, logits, gates)
```

```python
import contextlib
import math
import numpy as np
import ml_dtypes
import concourse.bass as bass
import concourse.mybir as mybir
from concourse.bass_utils import run_bass_kernel_spmd

F32 = mybir.dt.float32
BF16 = mybir.dt.bfloat16
U8 = mybir.dt.uint8
AF = mybir.ActivationFunctionType
ALU = mybir.AluOpType

ENGS = ("pe", "act", "dve", "pool", "sp")
N_DMA_SEMS = 16
DMA_POOLS = {"sp": list(range(0, 7)), "pool": list(range(7, 14)), "act": [14, 15]}
PAGE = 256
ARENA = 207 * 1024
PSUM_PAGE0 = 100000
DRAM_PAGE0 = 200000

D = 1024
T = 2304
NCTX = 256
NLAT = 2048
EPS = 1e-6


class Op:
    __slots__ = ("eng", "fn", "dma", "preds", "sig", "sigval", "dsem", "dval", "idx")


class Prog:
    def __init__(self, nc):
        self.nc = nc
        self.ops = []
        self.pw = {}
        self.pr = {}

    def add(self, eng, fn, reads=(), writes=(), dma=False):
        op = Op()
        op.eng = eng
        op.fn = fn
        op.dma = dma
        op.idx = len(self.ops)
        op.sig = False
        preds = set()
        pw, pr = self.pw, self.pr
        for (lo, hi) in reads:
            for p in range(lo, hi):
                w = pw.get(p)
                if w is not None:
                    preds.add(w)
        for (lo, hi) in writes:
            for p in range(lo, hi):
                w = pw.get(p)
                if w is not None:
                    preds.add(w)
                r = pr.get(p)
                if r:
                    preds.update(r.values())
        rkey = ("d", op.idx) if dma else eng
        for (lo, hi) in reads:
            for p in range(lo, hi):
                r = pr.get(p)
                if r is None:
                    pr[p] = {rkey: op.idx}
                else:
                    r[rkey] = op.idx
        for (lo, hi) in writes:
            for p in range(lo, hi):
                pw[p] = op.idx
                pr[p] = None
        preds.discard(op.idx)
        op.preds = preds
        self.ops.append(op)
        return op

    def emit(self):
        nc = self.nc
        ops = self.ops
        with contextlib.ExitStack() as es:
            esem = {e: es.enter_context(nc.semaphore(f"s_{e}")) for e in ENGS}
            dsems = [es.enter_context(nc.semaphore(f"s_dma{i}")) for i in range(N_DMA_SEMS)]
            waits = [None] * len(ops)
            for op in ops:
                per_eng = {}
                dma_w = []
                for p in op.preds:
                    po = ops[p]
                    if po.dma:
                        dma_w.append(p)
                    else:
                        if po.eng == "pe" and op.eng == "pe" and not op.dma:
                            continue
                        if po.eng not in per_eng or per_eng[po.eng] < p:
                            per_eng[po.eng] = p
                for p in per_eng.values():
                    ops[p].sig = True
                waits[op.idx] = (list(per_eng.values()), dma_w)
            cnt = {e: 0 for e in ENGS}
            dcnt = [0] * N_DMA_SEMS
            dlast = [None] * N_DMA_SEMS
            dpos = {e: 0 for e in ENGS}
            dma_prev = {}
            for op in ops:
                if op.dma:
                    rng = DMA_POOLS[op.eng]
                    k = rng[dpos[op.eng] % len(rng)]
                    dpos[op.eng] += 1
                    dcnt[k] += 16
                    op.dsem = k
                    op.dval = dcnt[k]
                    dma_prev[op.idx] = dlast[k]
                    dlast[k] = op.idx
                elif op.sig:
                    cnt[op.eng] += 1
                    op.sigval = cnt[op.eng]
            streams = {e: [o for o in ops if o.eng == e] for e in ENGS}
            self.stats = {e: len(streams[e]) for e in ENGS}
            self.stats["sig"] = dict(cnt)

            def run_stream(e, eng):
                known = {}

                def wait(key, sem, val):
                    if known.get(key, 0) >= val:
                        return
                    known[key] = val
                    eng.wait_ge(sem, val)

                for op in streams[e]:
                    cw, dw = waits[op.idx]
                    for p in cw:
                        po = ops[p]
                        wait(po.eng, esem[po.eng], po.sigval)
                    for p in dw:
                        po = ops[p]
                        wait(("d", po.dsem), dsems[po.dsem], po.dval)
                    if op.dma:
                        pp = dma_prev[op.idx]
                        if pp is not None:
                            po = ops[pp]
                            wait(("d", po.dsem), dsems[po.dsem], po.dval)
                        ins = op.fn(eng)
                        ins.then_inc(dsems[op.dsem], 16)
                    else:
                        ins = op.fn(eng)
                        if op.sig:
                            ins.then_inc(esem[e], 1)
                if e == "sp":
                    for k in range(N_DMA_SEMS):
                        if dcnt[k]:
                            eng.wait_ge(dsems[k], dcnt[k])

            with nc.Block() as block:
                @block.tensor
                def _(eng):
                    run_stream("pe", eng)

                @block.scalar
                def _(eng):
                    run_stream("act", eng)

                @block.vector
                def _(eng):
                    run_stream("dve", eng)

                @block.gpsimd
                def _(eng):
                    run_stream("pool", eng)

                @block.sync
                def _(eng):
                    run_stream("sp", eng)


class V:
    __slots__ = ("ap", "pg")

    def __init__(self, ap, pg):
        self.ap = ap
        self.pg = pg

    def m(self, f):
        return V(f(self.ap), self.pg)


class Tile:
    def __init__(self, k, off, shape, dt, name):
        self.k = k
        self.off = off
        self.shape = list(shape)
        self.dt = dt
        self.es = 4 if dt == F32 else (2 if dt == BF16 else 1)
        n = 1
        for s in shape[1:]:
            n *= s
        self.n = n
        self.nbytes = n * self.es
        base = k.arena[0:shape[0], off:off + self.nbytes]
        if dt != U8:
            base = base.bitcast(dt)
        if len(shape) == 3:
            base = base.rearrange("p (a b) -> p a b", a=shape[1])
        elif len(shape) == 4:
            base = base.rearrange("p (a b c) -> p a b c", a=shape[1], b=shape[2])
        self.ap = base
        self.name = name

    def _pages(self, lo_e, hi_e):
        lo = self.off + lo_e * self.es
        hi = self.off + hi_e * self.es
        return (lo // PAGE, (hi + PAGE - 1) // PAGE)

    @property
    def all(self):
        return V(self.ap, [self._pages(0, self.n)])

    def __getitem__(self, idx):
        if not isinstance(idx, tuple):
            idx = (idx,)
        ap = self.ap[idx]
        fidx = list(idx[1:]) + [slice(None)] * (len(self.shape) - len(idx))
        dims = self.shape[1:]
        rng = []
        for dim, ix in zip(dims, fidx):
            if isinstance(ix, int):
                rng.append((ix, ix + 1))
            else:
                a = 0 if ix.start is None else ix.start
                b = dim if ix.stop is None else ix.stop
                rng.append((a, b))
        strides = []
        st = 1
        for dim in reversed(dims):
            strides.insert(0, st)
            st *= dim
        lead = rng[:-1]
        cnt = 1
        for a, b in lead:
            cnt *= (b - a)
        pages = []
        if cnt <= 64:
            import itertools
            for combo in itertools.product(*[range(a, b) for a, b in lead]):
                base = sum(c * s_ for c, s_ in zip(combo, strides[:-1]))
                pages.append(self._pages(base + rng[-1][0], base + rng[-1][1]))
        else:
            lo = sum(a * s_ for (a, b), s_ in zip(rng, strides))
            hi = sum((b - 1) * s_ for (a, b), s_ in zip(rng, strides)) + 1
            pages.append(self._pages(lo, hi))
        return V(ap, pages)


def pgs(vs):
    out = []
    for v in vs:
        out.extend(v.pg)
    return out


class K:
    def __init__(self, nc, es):
        self.nc = nc
        self.P = Prog(nc)
        self.arena_t = es.enter_context(nc.sbuf_tensor("arena", [128, ARENA], U8))
        self.arena = self.arena_t
        self.free_list = [(0, ARENA)]
        self.banks = [es.enter_context(nc.psum_tensor(f"bank{i}", [128, 512], F32)) for i in range(8)]
        self.bank_i = 0
        self.dram_pg = DRAM_PAGE0

    def tile(self, shape, dt, name="t"):
        es_ = 4 if dt == F32 else (2 if dt == BF16 else 1)
        n = 1
        for s in shape[1:]:
            n *= s
        nb = ((n * es_ + PAGE - 1) // PAGE) * PAGE
        for i, (o, sz) in enumerate(self.free_list):
            if sz >= nb:
                if sz == nb:
                    self.free_list.pop(i)
                else:
                    self.free_list[i] = (o + nb, sz - nb)
                t = Tile(self, o, shape, dt, name)
                t.alloc = nb
                return t
        raise RuntimeError(f"SBUF arena full allocating {name} {shape} ({nb}B); free={self.free_list}")

    def free(self, *tiles):
        for t in tiles:
            self.free_list.append((t.off, t.alloc))
        self.free_list.sort()
        merged = []
        for o, sz in self.free_list:
            if merged and merged[-1][0] + merged[-1][1] == o:
                merged[-1] = (merged[-1][0], merged[-1][1] + sz)
            else:
                merged.append((o, sz))
        self.free_list = merged

    def ps(self, i, rows=128, c0=0, c1=512, r0=0):
        b = self.banks[i]
        return V(b[r0:r0 + rows, c0:c1], [(PSUM_PAGE0 + i, PSUM_PAGE0 + i + 1)])

    def ps3(self, i, a, rows=128):
        b = self.banks[i]
        return V(b[0:rows, :].rearrange("p (a b) -> p a b", a=a), [(PSUM_PAGE0 + i, PSUM_PAGE0 + i + 1)])

    def dram(self, ap):
        self.dram_pg += 1
        return V(ap, [(self.dram_pg, self.dram_pg + 1)])

    def op(self, eng, fn, R=(), W=()):
        self.P.add(eng, fn, pgs(R), pgs(W))

    def dma(self, eng, out, in_):
        self.P.add(eng, lambda e: e.dma_start(out=out.ap, in_=in_.ap), pgs([in_]), pgs([out]), dma=True)

    def mm(self, out, lhsT, rhs, start=True, stop=True):
        self.P.add("pe", lambda e: e.matmul(out.ap, lhsT=lhsT.ap, rhs=rhs.ap, start=start, stop=stop),
                   pgs([lhsT, rhs]), pgs([out]))

    def transpose(self, out, in_, ident):
        self.P.add("pe", lambda e: e.transpose(out.ap, in_.ap, ident.ap), pgs([in_, ident]), pgs([out]))

    def act(self, out, in_, func, bias=None, scale=None):
        R = [in_]
        kw = {}
        if bias is not None:
            if isinstance(bias, V):
                R.append(bias)
                kw["bias"] = bias.ap
            else:
                kw["bias"] = float(bias)
        if scale is not None:
            if isinstance(scale, V):
                R.append(scale)
                kw["scale"] = scale.ap
            else:
                kw["scale"] = float(scale)
        self.P.add("act", lambda e: e.activation(out=out.ap, in_=in_.ap, func=func, **kw),
                   pgs(R), pgs([out]))

    def copy(self, eng, out, in_):
        if eng == "act":
            self.P.add("act", lambda e: e.copy(out=out.ap, in_=in_.ap), pgs([in_]), pgs([out]))
        else:
            self.P.add(eng, lambda e: e.tensor_copy(out=out.ap, in_=in_.ap), pgs([in_]), pgs([out]))

    def tt(self, eng, out, in0, in1, op):
        self.P.add(eng, lambda e: e.tensor_tensor(out=out.ap, in0=in0.ap, in1=in1.ap, op=op),
                   pgs([in0, in1]), pgs([out]))

    def ts(self, eng, out, in0, s1, s2, op0, op1=None):
        R = [in0]
        a1 = s1.ap if isinstance(s1, V) else float(s1)
        if isinstance(s1, V):
            R.append(s1)
        if s2 is None:
            self.P.add(eng, lambda e: e.tensor_scalar(out=out.ap, in0=in0.ap, scalar1=a1, scalar2=None, op0=op0),
                       pgs(R), pgs([out]))
            return
        a2 = s2.ap if isinstance(s2, V) else float(s2)
        if isinstance(s2, V):
            R.append(s2)
        self.P.add(eng, lambda e: e.tensor_scalar(out=out.ap, in0=in0.ap, scalar1=a1, scalar2=a2, op0=op0, op1=op1),
                   pgs(R), pgs([out]))

    def stt(self, eng, out, in0, scalar, in1, op0, op1):
        R = [in0, in1]
        a = scalar.ap if isinstance(scalar, V) else float(scalar)
        if isinstance(scalar, V):
            R.append(scalar)
        self.P.add(eng, lambda e: e.scalar_tensor_tensor(out=out.ap, in0=in0.ap, scalar=a, in1=in1.ap, op0=op0, op1=op1),
                   pgs(R), pgs([out]))

    def memset(self, eng, out, val):
        self.P.add(eng, lambda e: e.memset(out.ap, val), [], pgs([out]))

    def recip(self, out, in_):
        self.P.add("dve", lambda e: e.reciprocal(out=out.ap, in_=in_.ap), pgs([in_]), pgs([out]))

    def scan(self, out, d0, d1, initial):
        R = [d0, d1]
        a = initial.ap if isinstance(initial, V) else float(initial)
        if isinstance(initial, V):
            R.append(initial)
        self.P.add("dve", lambda e: e.tensor_tensor_scan(out=out.ap, data0=d0.ap, data1=d1.ap, initial=a,
                                                       op0=ALU.mult, op1=ALU.add),
                   pgs(R), pgs([out]))


def col_blocks(ctx=True, lat=True):
    b = []
    if ctx:
        b.append((0, NCTX, 1))
    if lat:
        for i in range(4):
            b.append((NCTX + 512 * i, 512, 0))
    return b


def fm(v):
    v = np.asarray(v)
    n = v.shape[-1] // 128
    return np.ascontiguousarray(v.reshape(v.shape[:-1] + (n, 128)).swapaxes(-1, -2))


def rope_tables(rot_dim, nfeat, rot_off):
    n_freq = rot_dim // 4
    inv_freq = (10000.0 ** (-np.arange(n_freq, dtype=np.float32) / n_freq)).astype(np.float32)
    t = np.arange(NLAT)
    row = (t // 64).astype(np.float32)
    col = (t % 64).astype(np.float32)
    ang = np.concatenate([row[:, None] * inv_freq, col[:, None] * inv_freq], axis=-1).astype(np.float32)
    cos = np.cos(ang).astype(np.float32)
    sin = np.sin(ang).astype(np.float32)
    C = np.ones((nfeat, T), np.float32)
    S = np.zeros((nfeat, T), np.float32)
    half = rot_dim // 2
    for f in range(rot_off, nfeat):
        i = (f - rot_off) % half
        C[f, NCTX:] = cos[:, i]
        S[f, NCTX:] = sin[:, i]
    R = np.zeros((nfeat, nfeat), np.float32)
    for i in range(half):
        R[rot_off + i + half, rot_off + i] = -1.0
        R[rot_off + i, rot_off + i + half] = 1.0
    return C, S, R


def host_consts():
    c = {}
    c["ident"] = np.eye(128, dtype=np.float32)
    cg, sg, rg = rope_tables(64, 64, 0)
    c["rope_g_cos"], c["rope_g_sin"], c["rope_g_R"] = cg, sg, rg
    cm, sm, rm = rope_tables(32, 96, 64)
    c["rope_m_cos"], c["rope_m_sin"], c["rope_m_R"] = cm, sm, rm
    L = NLAT
    N = 2 * L
    s = np.arange(L, dtype=np.float64)[:, None]
    f = np.arange(L, dtype=np.float64)[None, :]
    th = np.pi * (2 * f + 1) * s / N
    Cm = np.cos(th)
    Sm = np.sin(th)
    bf = ml_dtypes.bfloat16
    def tile_(M):
        return np.ascontiguousarray(M.reshape(16, 128, 16, 128).transpose(2, 1, 0, 3).reshape(16, 128, 2048)).astype(bf)
    c["dft_cf"] = tile_(Cm)
    c["dft_sf"] = tile_(Sm)
    c["dft_ci"] = tile_(Cm.T * (2.0 / N))
    c["dft_si"] = tile_(-Sm.T * (2.0 / N))
    t = np.arange(L, dtype=np.float32)[:, None]
    t_norm = t / max(L - 1, 1)
    bands = np.linspace(1e-4, 16 - 1, 16, dtype=np.float32)
    ang = (2.0 * math.pi * t * bands / L).astype(np.float32)
    z = np.concatenate([t_norm, np.cos(ang), -np.sin(ang)], axis=-1).astype(np.float32)
    c["hy_zT"] = np.ascontiguousarray(z.T)
    HY_MIN = math.log(1e-2) / 1.5
    HY_MAX = math.log(1e-2) / 0.3
    deltas = np.abs(np.linspace(HY_MIN, HY_MAX, 512, dtype=np.float32))
    window = (np.exp(-t_norm * deltas) + 0.05).astype(np.float32)
    c["hy_window"] = window
    return c


def build(shapes, stage=99):
    nc = bass.Bass("TRN2", target_bir_lowering=False)
    dr = {}
    for name, shp in shapes.items():
        dt_ = F32
        if isinstance(shp, tuple) and len(shp) == 2 and isinstance(shp[1], str):
            shp, dts = shp
            dt_ = BF16 if dts == "bfloat16" else F32
        dr[name] = nc.dram_tensor(name, list(shp), dt_, kind="ExternalInput").ap()
    out_d = nc.dram_tensor("out", [NLAT, D], F32, kind="ExternalOutput").ap()
    outc_d = nc.dram_tensor("outc", [NCTX, D], F32, kind="ExternalOutput").ap()
    with contextlib.ExitStack() as es:
        k = K(nc, es)
        _emit_program(k, dr, out_d, outc_d, stage)
        k.P.emit()
    return nc


def _emit_program(k, dr, out_d, outc_d, stage):
    dm = k.dram
    SW = "pool"

    xs = k.tile([128, 8, T], F32, "xs")
    hbf = k.tile([128, 8, T], BF16, "hbf")
    ident_f = k.tile([128, 128], F32, "ident_f")
    ident_b = k.tile([128, 128], BF16, "ident_b")
    ones_b = k.tile([128, 128], BF16, "ones_b")
    mods = k.tile([128, 2, 48, 2], F32, "mods")
    affA = k.tile([128, 2, 2, 16], F32, "affA")
    gdup = k.tile([128, 2, 2, 16], F32, "gdup")
    modb = k.tile([128, 2, 48], F32, "modb")

    k.dma("sp", ident_f.all, dm(dr["ident"]))
    k.copy("dve", ident_b.all, ident_f.all)
    k.memset("dve", ones_b.all, 1.0)
    k.dma("sp", gdup.all, dm(dr["gdup"]))
    k.dma("sp", modb.all, dm(dr["modb"]))

    xts = [k.tile([128, 1024], F32, f"xt{i}") for i in range(2)]
    for tc in range(18):
        src = dr["ctx"][tc * 128:(tc + 1) * 128, :] if tc < 2 else dr["x"][(tc - 2) * 128:(tc - 1) * 128, :]
        xt = xts[tc % 2]
        k.dma("sp", xt.all, dm(src))
        for half in range(2):
            b = (tc * 2 + half) % 4
            for kk in range(4):
                kf = half * 4 + kk
                k.transpose(k.ps(b, c0=kk * 128, c1=(kk + 1) * 128), xt[:, kf * 128:(kf + 1) * 128], ident_f.all)
            k.copy("dve" if half == 0 else "act", xs[:, half * 4:(half + 1) * 4, tc * 128:(tc + 1) * 128], k.ps3(b, 4))
    k.free(*xts)

    cl = k.tile([128, 8], F32, "cl")
    cc = k.tile([128, 8], F32, "cc")
    s_bf = k.tile([128, 8, 2], BF16, "s_bf")
    k.dma("sp", cl.all, dm(dr["c_fm"]))
    k.dma("sp", cc.all, dm(dr["cctx_fm"]))
    k.act(s_bf[:, :, 0], cl.all, AF.Silu)
    k.act(s_bf[:, :, 1], cc.all, AF.Silu)
    mwt = [k.tile([128, 8, 512], BF16, f"mw{i}") for i in range(2)]
    gi = 0
    for l in range(2):
        mw = dr["mod_w"][l].rearrange("(kc p) n -> p kc n", p=128)
        for g in range(12):
            wt = mwt[gi % 2]
            k.dma(SW, wt.all, dm(mw[:, :, g * 512:(g + 1) * 512]))
            b = 4 + gi % 2
            for j in range(4):
                for kk in range(8):
                    k.mm(k.ps(b, c0=2 * j, c1=2 * j + 2), wt[:, kk, j * 128:(j + 1) * 128], s_bf[:, kk, :],
                         start=(kk == 0), stop=(kk == 7))
            for j in range(4):
                ch = g * 4 + j
                k.ts("dve", mods[:, l, ch, :], k.ps(b, c0=2 * j, c1=2 * j + 2), modb[:, l, ch:ch + 1], None, ALU.add)
            gi += 1
    k.free(*mwt, cl, cc, s_bf)
    for l in range(2):
        for n, mi in ((0, 1), (1, 4)):
            src = V(mods.ap[:, l, mi * 8:(mi + 1) * 8, :].rearrange("p a b -> p (a b)"), mods[:, l, mi * 8:(mi + 1) * 8, :].pg)
            k.stt("dve", affA[:, l, n, :], src, 1.0, gdup[:, l, n, :], ALU.add, ALU.mult)

    def A_(l, n, kk, s):
        return affA[:, l, n, kk * 2 + s:kk * 2 + s + 1]

    def M_(l, mi, kk, s):
        return mods[:, l, mi * 8 + kk, s:s + 1]

    def norm_mod(l, n, blocks, f32_hook=None):
        sqs = [k.tile([128, 8, 512], BF16, f"sq{i}") for i in range(2)]
        rstds = [k.tile([128, 512], F32, f"rstd{i}") for i in range(2)]
        tmps = [k.tile([128, 512], F32, f"nt{i}") for i in range(3)]
        ti = 0
        for bi, (c0, ncl, s) in enumerate(blocks):
            sq = sqs[bi % 2]
            rstd = rstds[bi % 2]
            for kk in range(8):
                k.act(sq[:, kk, 0:ncl], xs[:, kk, c0:c0 + ncl], AF.Square)
            b = 4 + bi % 2
            for kk in range(8):
                k.mm(k.ps(b, c1=ncl), ones_b.all, sq[:, kk, 0:ncl], start=(kk == 0), stop=(kk == 7))
            k.act(rstd[:, 0:ncl], k.ps(b, c1=ncl), AF.Sqrt, bias=EPS, scale=1.0 / D)
            k.recip(rstd[:, 0:ncl], rstd[:, 0:ncl])
            for kk in range(8):
                tmp = tmps[ti % 3]
                ti += 1
                k.tt("dve" if kk % 2 == 0 else "pool", tmp[:, 0:ncl], xs[:, kk, c0:c0 + ncl], rstd[:, 0:ncl], ALU.mult)
                k.act(hbf[:, kk, c0:c0 + ncl], tmp[:, 0:ncl], AF.Identity,
                      bias=M_(l, 0 if n == 0 else 3, kk, s), scale=A_(l, n, kk, s))
                if f32_hook is not None:
                    f32_hook(bi, c0, ncl, kk, tmp, s)
        k.free(*sqs, *rstds, *tmps)

    def write_out():
        ots = [k.tile([128, 1024], F32, f"ot{i}") for i in range(2)]
        for tc in range(18):
            ot = ots[tc % 2]
            for half in range(2):
                b = (tc * 2 + half) % 4
                for kk in range(4):
                    kf = half * 4 + kk
                    k.transpose(k.ps(b, c0=kk * 128, c1=(kk + 1) * 128), xs[:, kf, tc * 128:(tc + 1) * 128], ident_f.all)
                k.copy("dve" if half == 0 else "act", ot[:, half * 512:(half + 1) * 512], k.ps(b))
            dst = outc_d[tc * 128:(tc + 1) * 128, :] if tc < 2 else out_d[(tc - 2) * 128:(tc - 1) * 128, :]
            k.dma("sp", dm(dst), ot.all)
        k.free(*ots)

    if stage == 0:
        write_out()
        return

    def qk_norm_rope(psv, nrows, gain, onesv, cosT, sinT, Rm, outv, c0, ncl, tl, bi, banks):
        sq, rstd, qn, t1 = tl
        k.act(sq[0:nrows, 0:ncl], psv, AF.Square)
        k.act(qn[0:nrows, 0:ncl], psv, AF.Identity, scale=gain)
        b1, b2 = banks
        k.mm(k.ps(b1, rows=nrows, c1=ncl), onesv, sq[0:nrows, 0:ncl])
        k.act(rstd[0:nrows, 0:ncl], k.ps(b1, rows=nrows, c1=ncl), AF.Sqrt, bias=EPS, scale=1.0 / nrows)
        k.copy("pool", sq[0:nrows, 0:ncl], qn[0:nrows, 0:ncl])
        k.mm(k.ps(b2, rows=nrows, c1=ncl), Rm, sq[0:nrows, 0:ncl])
        k.recip(rstd[0:nrows, 0:ncl], rstd[0:nrows, 0:ncl])
        k.tt("pool", t1[0:nrows, 0:ncl], qn[0:nrows, 0:ncl], cosT[0:nrows, c0:c0 + ncl], ALU.mult)
        k.tt("dve", qn[0:nrows, 0:ncl], k.ps(b2, rows=nrows, c1=ncl), sinT[0:nrows, c0:c0 + ncl], ALU.mult)
        k.tt("pool", t1[0:nrows, 0:ncl], t1[0:nrows, 0:ncl], qn[0:nrows, 0:ncl], ALU.add)
        k.tt("dve", outv, t1[0:nrows, 0:ncl], rstd[0:nrows, 0:ncl], ALU.mult)

    def attend(qh, kh, vaug, Dh, q_blocks, n_kc, scale, ycat, yrow0, ychunk, etl, rdt):
        for qi, (c0, ncl) in enumerate(q_blocks):
            ob = qi % 2
            LA = 4
            for it in range(n_kc + LA):
                if it < n_kc:
                    kc = it
                    sb = 2 + kc % 5
                    e = etl[kc % len(etl)]
                    k.mm(k.ps(sb, c1=ncl), kh[0:Dh, kc * 128:(kc + 1) * 128], qh[0:Dh, c0:c0 + ncl])
                    k.act(e[:, 0:ncl], k.ps(sb, c1=ncl), AF.Exp, scale=scale)
                if it >= LA:
                    kc = it - LA
                    e = etl[kc % len(etl)]
                    k.mm(k.ps(ob, c1=ncl), vaug[:, kc, 64:192], e[:, 0:ncl], start=(kc == 0), stop=(kc == n_kc - 1))
            rd, rs = rdt[qi % len(rdt)]
            k.recip(rd[64:128, 0:ncl], k.ps(ob, rows=64, r0=64, c1=ncl))
            k.copy("pool", rs[0:64, 0:ncl], rd[64:128, 0:ncl])
            k.tt("dve", ycat[yrow0:yrow0 + 64, ychunk, c0:c0 + ncl], k.ps(ob, rows=64, c1=ncl), rs[0:64, 0:ncl], ALU.mult)

    def project_out(l, ycat, w_d, krow0, blocks):
        wo = k.tile([128, 4, 1024], BF16, "wo")
        k.dma(SW, wo.all, dm(w_d[krow0:krow0 + 512, :].rearrange("(kc p) n -> p kc n", p=128)))
        i = 0
        for (c0, ncl, s) in blocks:
            for d in range(8):
                b = 4 + i % 4
                i += 1
                for kk in range(4):
                    k.mm(k.ps(b, c1=ncl), wo[:, kk, d * 128:(d + 1) * 128], ycat[:, kk, c0:c0 + ncl],
                         start=(kk == 0), stop=(kk == 3))
                k.stt("dve", xs[:, d, c0:c0 + ncl], k.ps(b, c1=ncl), M_(l, 2, d, s),
                      xs[:, d, c0:c0 + ncl], ALU.mult, ALU.add)
        k.free(wo)

    def ffn(l, w1_d, w3_d, w2_d, dff, blocks, hid, gate_bc=None):
        ngroups = (dff + 511) // 512
        w1t = [k.tile([128, 8, 512], BF16, f"w1_{i}") for i in range(2)]
        w3t = [k.tile([128, 8, 512], BF16, f"w3_{i}") for i in range(2)]
        w2t = [k.tile([128, 4, 1024], BF16, f"w2_{i}") for i in range(2)]
        sts = [k.tile([128, 512], F32, f"st{i}") for i in range(3)]
        w1v = w1_d.rearrange("(kc p) n -> p kc n", p=128)
        w3v = w3_d.rearrange("(kc p) n -> p kc n", p=128)
        si = 0
        ai = 0
        for g in range(ngroups):
            f0 = g * 512
            fw = min(512, dff - f0)
            nch = fw // 128
            w1, w3, w2 = w1t[g % 2], w3t[g % 2], w2t[g % 2]
            k.dma(SW, w1[:, :, 0:fw], dm(w1v[:, :, f0:f0 + fw]))
            k.dma(SW, w3[:, :, 0:fw], dm(w3v[:, :, f0:f0 + fw]))
            k.dma(SW, w2[:, 0:nch, :], dm(w2_d[f0:f0 + fw, :].rearrange("(kc p) n -> p kc n", p=128)))
            for m in range(nch):
                for (c0, ncl, s) in blocks:
                    bg = si % 2
                    bu = 2 + si % 2
                    st = sts[si % 3]
                    si += 1
                    for kk in range(8):
                        k.mm(k.ps(bg, c1=ncl), w1[:, kk, m * 128:(m + 1) * 128], hbf[:, kk, c0:c0 + ncl],
                             start=(kk == 0), stop=(kk == 7))
                    for kk in range(8):
                        k.mm(k.ps(bu, c1=ncl), w3[:, kk, m * 128:(m + 1) * 128], hbf[:, kk, c0:c0 + ncl],
                             start=(kk == 0), stop=(kk == 7))
                    k.act(st[:, 0:ncl], k.ps(bg, c1=ncl), AF.Silu)
                    if gate_bc is None:
                        k.tt("dve", hid[:, m, c0:c0 + ncl], st[:, 0:ncl], k.ps(bu, c1=ncl), ALU.mult)
                    else:
                        k.tt("dve", st[:, 0:ncl], st[:, 0:ncl], k.ps(bu, c1=ncl), ALU.mult)
                        k.tt("pool", hid[:, m, c0:c0 + ncl], st[:, 0:ncl], gate_bc[:, c0 - NCTX:c0 - NCTX + ncl], ALU.mult)
            for (c0, ncl, s) in blocks:
                for d in range(8):
                    b = 4 + ai % 4
                    ai += 1
                    for kk in range(nch):
                        k.mm(k.ps(b, c1=ncl), w2[:, kk, d * 128:(d + 1) * 128], hid[:, kk, c0:c0 + ncl],
                             start=(kk == 0), stop=(kk == nch - 1))
                    k.stt("dve", xs[:, d, c0:c0 + ncl], k.ps(b, c1=ncl), M_(l, 5, d, s),
                          xs[:, d, c0:c0 + ncl], ALU.mult, ALU.add)
        k.free(*w1t, *w3t, *w2t, *sts)

    ALLB = col_blocks(True, True)
    LATB = col_blocks(False, True)

    norm_mod(0, 0, ALLB)
    ycat = k.tile([128, 4, T], BF16, "ycat")
    layer0_mixer(k, dr, xs, hbf, ycat, ident_f, ones_b, M_, project_out, qk_norm_rope, attend, ALLB)
    if stage == 1:
        write_out()
        return
    norm_mod(0, 1, ALLB)
    ffn(0, dr["ffn_w1"], dr["ffn_w3"], dr["ffn_w2"], 2816, ALLB, ycat)
    if stage == 2:
        write_out()
        return
    import os
    SUB = int(os.environ.get("SUB1", "99"))
    norm_mod(1, 0, ALLB)
    layer1_mixer(k, dr, xs, hbf, ycat, ident_b, ones_b, M_, project_out, qk_norm_rope, attend, ALLB, LATB, SUB)
    if stage == 3:
        write_out()
        return
    moe_layer(k, dr, xs, hbf, ycat, ident_f, ones_b, M_, A_, norm_mod, ffn, LATB)
    write_out()


def layer0_mixer(k, dr, xs, hbf, ycat, ident_f, ones_b, M_, project_out, qk_norm_rope, attend, ALLB):
    import os
    SUB = int(os.environ.get("SUBSTAGE", "99"))
    dm = k.dram
    SW = "pool"
    win = dr["ab_w_in"].rearrange("(kc p) n -> p kc n", p=128)
    cosT = k.tile([64, T], F32, "cosT")
    sinT = k.tile([64, T], F32, "sinT")
    Rm = k.tile([64, 64], BF16, "Rm")
    gq = k.tile([64, 2], F32, "gqk")
    k.dma("sp", cosT.all, dm(dr["rope_g_cos"]))
    k.dma("sp", sinT.all, dm(dr["rope_g_sin"]))
    k.dma(SW, Rm.all, dm(dr["rope_g_R"]))
    k.dma("sp", gq.all, dm(dr["gqa_norms"]))
    wq_ = k.tile([128, 8, 512], BF16, "wq")
    wkv_ = k.tile([128, 8, 256], BF16, "wkv")
    k.dma(SW, wkv_.all, dm(win[:, :, 1536:1792]))
    k.dma(SW, wq_.all, dm(win[:, :, 1024:1536]))
    kh = [k.tile([64, T], BF16, f"kh{i}") for i in range(2)]
    vaug = [k.tile([128, 18, 192], BF16, f"vaug{i}") for i in range(2)]
    tl = (k.tile([64, 512], BF16, "n_sq"), k.tile([64, 512], F32, "n_rstd"), k.tile([64, 512], F32, "n_qn"),
          k.tile([64, 512], F32, "n_t1"))
    onesv = ones_b[0:64, 0:64]
    bi = 0
    for h in range(2):
        for (c0, ncl, s) in ALLB:
            b = 4 + bi % 2
            bi += 1
            for kk in range(8):
                k.mm(k.ps(b, rows=64, c1=ncl), wkv_[:, kk, h * 64:(h + 1) * 64], hbf[:, kk, c0:c0 + ncl],
                     start=(kk == 0), stop=(kk == 7))
            qk_norm_rope(k.ps(b, rows=64, c1=ncl), 64, gq[:, 1:2], onesv, cosT, sinT, Rm.all,
                         kh[h][0:64, c0:c0 + ncl], c0, ncl, tl, bi, (6, 7))
    if SUB == 1:
        return
    BIS = os.environ.get("BIS", "")
    for h in range(2):
        if "m" not in BIS:
            k.memset("pool", vaug[h].all, 1.0)
    for tc in range(18 if "t" not in BIS else 1):
        b = 4 + tc % 2
        if "x" not in BIS:
            for kk in range(8):
                k.mm(k.ps(b, c1=128), hbf[:, kk, tc * 128:(tc + 1) * 128], wkv_[:, kk, 128:256],
                     start=(kk == 0), stop=(kk == 7))
        if "c" not in BIS:
            for h in range(2):
                ce = "dve"
                if "d" in BIS:
                    ce = "dve"
                if "a" in BIS:
                    ce = "act"
                k.copy(ce, vaug[h][:, tc, 64:128], k.ps(b, c0=h * 64, c1=(h + 1) * 64))
    if SUB == 2:
        return
    k.free(wkv_)
    qhs = [k.tile([64, T], BF16, f"qh{i}") for i in range(2)]
    etl = [k.tile([128, 512], BF16, f"e{i}") for i in range(6)]
    rdt = [(k.tile([128, 512], F32, f"rd{i}"), k.tile([64, 512], F32, f"rs{i}")) for i in range(1)]
    scale = 64 ** -0.5
    for h in range(8 if SUB != 3 else 1):
        qh = qhs[h % 2]
        for (c0, ncl, s) in ALLB:
            b = 4 + bi % 2
            bi += 1
            for kk in range(8):
                k.mm(k.ps(b, rows=64, c1=ncl), wq_[:, kk, h * 64:(h + 1) * 64], hbf[:, kk, c0:c0 + ncl],
                     start=(kk == 0), stop=(kk == 7))
            qk_norm_rope(k.ps(b, rows=64, c1=ncl), 64, gq[:, 0:1], onesv, cosT, sinT, Rm.all,
                         qh[0:64, c0:c0 + ncl], c0, ncl, tl, bi, (6, 7))
        kv = h // 4
        attend(qh, kh[kv], vaug[kv], 64, [(0, NCTX)], 2, scale, ycat, (h % 2) * 64, h // 2, etl, rdt)
        attend(qh, kh[kv], vaug[kv], 64, [(NCTX + 512 * i, 512) for i in range(4)], 18, scale, ycat,
               (h % 2) * 64, h // 2, etl, rdt)
    k.free(cosT, sinT, Rm, gq, wq_, *kh, *vaug, *tl, *qhs, *etl, *[t for p in rdt for t in p])
    if SUB == 3:
        return
    project_out(0, ycat, dr["ab_w_out"], 512, ALLB)
    if SUB == 4:
        return

    cw = k.tile([128, 4, 4], F32, "convw")
    cb = k.tile([128, 4], F32, "convb")
    lb = k.tile([128, 2, 2, 4], F32, "lru_b")
    lam = k.tile([128, 2, 4], F32, "lam")
    cneg = k.tile([128, 2, 4], F32, "cneg")
    k.dma("sp", cw.all, dm(dr["lru_convw_fm"]))
    k.dma("sp", cb.all, dm(dr["lru_convb_fm"]))
    k.dma("sp", lb.all, dm(dr["lru_b_fm"]))
    k.dma("sp", lam.all, dm(dr["lru_lam_fm"]))
    lamf = V(lam.ap.rearrange("p a b -> p (a b)"), lam.all.pg)
    cnegf = V(cneg.ap.rearrange("p a b -> p (a b)"), cneg.all.pg)
    k.act(cnegf, lamf, AF.Exp, scale=-1.0)
    k.act(cnegf, cnegf, AF.Ln, bias=1.0)
    k.ts("dve", cnegf, cnegf, -8.0, None, ALU.mult)
    xr = k.tile([128, T], F32, "xr")
    u = k.tile([128, T], F32, "u")
    ubf = k.tile([128, T], BF16, "ubf")
    rec = k.tile([128, T], F32, "rec")
    at = k.tile([128, T], F32, "a")
    bt = k.tile([128, T], F32, "b")
    ht = k.tile([128, T], F32, "h")
    wx = k.tile([128, 8, 256], BF16, "wxg")
    bd = [k.tile([128, 128], BF16, f"bd{i}") for i in range(4)]
    tmp = [k.tile([128, 512], F32, f"lt{i}") for i in range(4)]
    seqs = [(0, NCTX), (NCTX, NLAT)]
    for j in range(4):
        k.dma(SW, wx[:, :, 0:128], dm(k_win_slice(dr, j * 128)))
        k.dma(SW, wx[:, :, 128:256], dm(k_win_slice(dr, 512 + j * 128)))
        for d_ in range(2):
            for gi_, nm in enumerate(("lru_w_a", "lru_w_x")):
                t_ = bd[d_ * 2 + gi_]
                k.memset("pool", t_.all, 0.0)
                for hb_ in range(2):
                    k.dma(SW, t_[hb_ * 64:(hb_ + 1) * 64, hb_ * 64:(hb_ + 1) * 64], dm(dr[nm][d_, 2 * j + hb_]))
        for bi_, (c0, ncl, s) in enumerate(ALLB):
            b = 4 + bi_ % 2
            for kk in range(8):
                k.mm(k.ps(b, c1=ncl), wx[:, kk, 0:128], hbf[:, kk, c0:c0 + ncl], start=(kk == 0), stop=(kk == 7))
            k.copy("act", xr[:, c0:c0 + ncl], k.ps(b, c1=ncl))
        k.ts("dve", u.all, xr.all, cw[:, j, 2:3], cb[:, j:j + 1], ALU.mult, ALU.add)
        for tap in (0, 1, 3):
            sh = tap - 2
            for (s0, L_) in seqs:
                lo = s0 + max(0, -sh)
                hi = s0 + L_ - max(0, sh)
                k.stt("dve", u[:, lo:hi], xr[:, lo + sh:hi + sh], cw[:, j, tap:tap + 1], u[:, lo:hi], ALU.mult, ALU.add)
        k.copy("pool", ubf.all, u.all)
        for d_ in range(2):
            for bi_, (c0, ncl, s) in enumerate(ALLB):
                ba, bx = 4 + bi_ % 2, 6 + bi_ % 2
                k.mm(k.ps(ba, c1=ncl), bd[d_ * 2].all, ubf[:, c0:c0 + ncl])
                k.mm(k.ps(bx, c1=ncl), bd[d_ * 2 + 1].all, ubf[:, c0:c0 + ncl])
                r_, i_, q_, _ = tmp
                k.act(r_[:, 0:ncl], k.ps(ba, c1=ncl), AF.Sigmoid, bias=lb[:, 0, d_, j:j + 1])
                k.act(i_[:, 0:ncl], k.ps(bx, c1=ncl), AF.Sigmoid, bias=lb[:, 1, d_, j:j + 1])
                k.act(at[:, c0:c0 + ncl], r_[:, 0:ncl], AF.Exp, scale=cneg[:, d_, j:j + 1])
                k.tt("pool", q_[:, 0:ncl], at[:, c0:c0 + ncl], at[:, c0:c0 + ncl], ALU.mult)
                k.act(q_[:, 0:ncl], q_[:, 0:ncl], AF.Sqrt, bias=1.0, scale=-1.0)
                k.tt("dve", i_[:, 0:ncl], i_[:, 0:ncl], u[:, c0:c0 + ncl], ALU.mult)
                k.tt("dve", bt[:, c0:c0 + ncl], q_[:, 0:ncl], i_[:, 0:ncl], ALU.mult)
            if d_ == 0:
                k.scan(rec.all, at.all, bt.all, 0.0)
            else:
                rv = lambda v: v.m(lambda ap: ap[:, ::-1])
                k.scan(rv(ht[:, 0:NCTX]), rv(at[:, 0:NCTX]), rv(bt[:, 0:NCTX]), 0.0)
                k.scan(rv(ht[:, NCTX:T]), rv(at[:, NCTX:T]), rv(bt[:, NCTX:T]), ht[:, 0:1])
                k.tt("pool", rec.all, rec.all, ht.all, ALU.add)
        for bi_, (c0, ncl, s) in enumerate(ALLB):
            b = 4 + bi_ % 2
            for kk in range(8):
                k.mm(k.ps(b, c1=ncl), wx[:, kk, 128:256], hbf[:, kk, c0:c0 + ncl], start=(kk == 0), stop=(kk == 7))
            g_, t_, w_, _ = tmp
            k.copy("act", g_[:, 0:ncl], k.ps(b, c1=ncl))
            k.tt("pool", t_[:, 0:ncl], g_[:, 0:ncl], g_[:, 0:ncl], ALU.mult)
            k.ts("dve", t_[:, 0:ncl], t_[:, 0:ncl], 0.044715, 1.0, ALU.mult, ALU.add)
            k.tt("dve", t_[:, 0:ncl], t_[:, 0:ncl], g_[:, 0:ncl], ALU.mult)
            k.act(t_[:, 0:ncl], t_[:, 0:ncl], AF.Sigmoid, scale=2.0 * math.sqrt(2.0 / math.pi))
            k.tt("pool", t_[:, 0:ncl], t_[:, 0:ncl], g_[:, 0:ncl], ALU.mult)
            k.tt("dve", ycat[:, j, c0:c0 + ncl], t_[:, 0:ncl], rec[:, c0:c0 + ncl], ALU.mult)
    k.free(cw, cb, lb, lam, cneg, xr, u, ubf, rec, at, bt, ht, wx, *bd, *tmp)
    project_out(0, ycat, dr["ab_w_out"], 0, ALLB)


def k_win_slice(dr, c0):
    return dr["ab_w_in"].rearrange("(kc p) n -> p kc n", p=128)[:, :, c0:c0 + 128]


_CONSTS = None
_NC_CACHE = {}


def prep_inputs(inp, b):
    global _CONSTS
    if _CONSTS is None:
        _CONSTS = host_consts()
    f = lambda a: np.ascontiguousarray(np.asarray(a, dtype=np.float32))
    m = dict(_CONSTS)
    m.pop("rope_g_R"); m.pop("rope_m_R")
    m["rope_g_R"] = _CONSTS["rope_g_R"]; m["rope_m_R"] = _CONSTS["rope_m_R"]
    m["x"] = f(inp["x"][b])
    m["ctx"] = f(inp["ctx"][b])
    m["c_fm"] = f(fm(inp["c"][b]))
    m["cctx_fm"] = f(fm(inp["c_ctx"]))
    m["mod_w"] = f(inp["mod_w"])
    m["modb"] = f(fm(inp["mod_b"]).transpose(1, 0, 2))
    g = np.stack([fm(inp["norm1_g"]), fm(inp["norm2_g"])], axis=1)
    g = np.repeat(g[..., None], 2, axis=-1).reshape(2, 2, 128, 16)
    m["gdup"] = f(g.transpose(2, 0, 1, 3))
    m["ab_w_in"] = f(inp["ab_w_in"][0])
    m["ab_w_out"] = f(inp["ab_w_out"][0])
    m["gqa_norms"] = f(np.stack([inp["gqa_q_norm"][0], inp["gqa_k_norm"][0]], axis=1))
    m["lru_convw_fm"] = f(fm(inp["lru_conv_w"][0]).transpose(1, 2, 0))
    m["lru_convb_fm"] = f(fm(inp["lru_conv_b"][0]))
    lb = np.stack([fm(inp["lru_b_a"][0].reshape(2, 512)), fm(inp["lru_b_x"][0].reshape(2, 512))], axis=0)
    m["lru_b_fm"] = f(lb.transpose(2, 0, 1, 3))
    m["lru_lam_fm"] = f(fm(inp["lru_lambda"][0]).transpose(1, 0, 2))
    m["lru_w_a"] = f(inp["lru_w_a"][0])
    m["lru_w_x"] = f(inp["lru_w_x"][0])
    m["ffn_w1"] = f(inp["ffn_w1"][0])
    m["ffn_w3"] = f(inp["ffn_w3"][0])
    m["ffn_w2"] = f(inp["ffn_w2"][0])
    m["cd_w_in"] = f(inp["cd_w_in"][0])
    m["cd_w_out"] = f(inp["cd_w_out"][0])
    mg = np.zeros((128, 5), np.float32)
    mg[:, 0:2] = fm(inp["mla_q_a_norm"][0])
    mg[:, 2] = inp["mla_kv_a_norm"][0]
    mg[0:96, 3] = inp["mla_q_norm"][0]
    mg[0:96, 4] = inp["mla_k_norm"][0]
    m["mla_gains"] = mg
    m["mla_q_b"] = f(inp["mla_q_b"][0])
    m["mla_kv_b"] = f(inp["mla_kv_b"][0])
    m["hy_p"] = f(np.stack([inp["hy_filt_b1"][0], inp["hy_filt_b2"][0], inp["hy_sin_freq"][0]], axis=1))
    m["hy_filt_w1"] = f(inp["hy_filt_w1"][0])
    m["hy_filt_w2"] = f(inp["hy_filt_w2"][0])
    m["hy_filt_w3"] = f(inp["hy_filt_w3"][0])
    m["hy_conv_w"] = f(inp["hy_conv_w"][0])
    m["hy_conv_b"] = f(inp["hy_conv_b"][0])
    m["hy_skip"] = f(inp["hy_skip"][0])
    m["moe_router"] = f(inp["moe_router"][0])
    m["moe_w1"] = f(inp["moe_w1"][0])
    m["moe_w3"] = f(inp["moe_w3"][0])
    m["moe_w2"] = f(inp["moe_w2"][0])
    return m


def shapes_of(m):
    return {k_: (tuple(v.shape), str(v.dtype)) for k_, v in m.items()}


def kernel(**inputs):
    maps = [prep_inputs(inputs, b) for b in range(8)]
    shapes = shapes_of(maps[0])
    key = tuple(sorted(shapes.items()))
    if key not in _NC_CACHE:
        _NC_CACHE[key] = build(shapes)
    nc = _NC_CACHE[key]
    res = run_bass_kernel_spmd(nc, maps, core_ids=list(range(8)))
    return np.stack([np.asarray(r["out"], dtype=np.float32) for r in res.results], axis=0)


def ps_bf(k, i, rows, c0, c1):
    b = k.banks[i]
    return V(b[0:rows, :].bitcast(BF16)[:, c0:c1], [(PSUM_PAGE0 + i, PSUM_PAGE0 + i + 1)])


def layer1_mixer(k, dr, xs, hbf, ycat, ident_b, ones_b, M_, project_out, qk_norm_rope, attend, ALLB, LATB, SUB):
    dm = k.dram
    SW = "pool"
    win = dr["cd_w_in"].rearrange("(kc p) n -> p kc n", p=128)
    cosT = k.tile([96, T], F32, "cosM")
    sinT = k.tile([96, T], F32, "sinM")
    Rm = k.tile([96, 96], BF16, "RmM")
    gn = k.tile([128, 5], F32, "mla_g")
    k.dma("sp", cosT.all, dm(dr["rope_m_cos"]))
    k.dma("sp", sinT.all, dm(dr["rope_m_sin"]))
    k.dma(SW, Rm.all, dm(dr["rope_m_R"]))
    k.dma("sp", gn.all, dm(dr["mla_gains"]))
    wqa = k.tile([128, 8, 256], BF16, "wqa")
    wkva = k.tile([128, 8, 128], BF16, "wkva")
    wrp = k.tile([128, 8, 96], BF16, "wrope_pad")
    qb = k.tile([128, 2, 768], BF16, "q_b")
    kvbn = k.tile([128, 8, 96], BF16, "kvb_nope_pad")
    kvbv = k.tile([128, 8, 64], BF16, "kvb_v")
    k.dma(SW, wqa.all, dm(win[:, :, 1536:1792]))
    k.dma(SW, wkva.all, dm(win[:, :, 1792:1920]))
    k.memset("dve", wrp.all, 0.0)
    k.dma(SW, wrp[:, :, 64:96], dm(win[:, :, 1920:1952]))
    k.dma(SW, qb.all, dm(dr["mla_q_b"].rearrange("(kc p) n -> p kc n", p=128)))
    kvb3 = dr["mla_kv_b"].rearrange("k (h c) -> k h c", h=8)
    k.memset("dve", kvbn.all, 0.0)
    k.dma(SW, kvbn[:, :, 0:64], dm(kvb3[:, :, 0:64]))
    k.dma(SW, kvbv.all, dm(kvb3[:, :, 64:128]))
    qa = k.tile([128, 2, NLAT], BF16, "qa")
    kva = k.tile([128, T], BF16, "kva")
    tl = (k.tile([128, 512], BF16, "n_sq"), k.tile([128, 512], F32, "n_rstd"), k.tile([128, 512], F32, "n_qn"),
          k.tile([128, 512], F32, "n_t1"))
    sq2 = k.tile([128, 2, 512], BF16, "sq2")
    raw = k.tile([128, 2, 512], F32, "qa_raw")
    for bi, (c0, ncl, s) in enumerate(LATB):
        for m in range(2):
            b = 4 + m
            for kk in range(8):
                k.mm(k.ps(b, c1=ncl), wqa[:, kk, m * 128:(m + 1) * 128], hbf[:, kk, c0:c0 + ncl],
                     start=(kk == 0), stop=(kk == 7))
            k.act(sq2[:, m, 0:ncl], k.ps(b, c1=ncl), AF.Square)
            k.copy("act", raw[:, m, 0:ncl], k.ps(b, c1=ncl))
        for m in range(2):
            k.mm(k.ps(6, c1=ncl), ones_b.all, sq2[:, m, 0:ncl], start=(m == 0), stop=(m == 1))
        rstd = tl[1]
        k.act(rstd[:, 0:ncl], k.ps(6, c1=ncl), AF.Sqrt, bias=EPS, scale=1.0 / 256)
        k.recip(rstd[:, 0:ncl], rstd[:, 0:ncl])
        for m in range(2):
            k.stt("dve", qa[:, m, c0 - NCTX:c0 - NCTX + ncl], raw[:, m, 0:ncl], gn[:, m:m + 1], rstd[:, 0:ncl],
                  ALU.mult, ALU.mult)
    for bi, (c0, ncl, s) in enumerate(ALLB):
        b = 4 + bi % 2
        for kk in range(8):
            k.mm(k.ps(b, c1=ncl), wkva[:, kk, :], hbf[:, kk, c0:c0 + ncl], start=(kk == 0), stop=(kk == 7))
        sq, rstd = tl[0], tl[1]
        k.act(sq[:, 0:ncl], k.ps(b, c1=ncl), AF.Square)
        k.mm(k.ps(6, c1=ncl), ones_b.all, sq[:, 0:ncl])
        k.act(rstd[:, 0:ncl], k.ps(6, c1=ncl), AF.Sqrt, bias=EPS, scale=1.0 / 128)
        k.recip(rstd[:, 0:ncl], rstd[:, 0:ncl])
        k.stt("dve", kva[:, c0:c0 + ncl], k.ps(b, c1=ncl), gn[:, 2:3], rstd[:, 0:ncl], ALU.mult, ALU.mult)
    k.free(sq2, raw, wqa, wkva)
    kh = k.tile([96, T], BF16, "kh")
    qh = k.tile([96, T], BF16, "qh")
    vaug = k.tile([128, 18, 192], BF16, "vaug")
    etl = [k.tile([128, 512], BF16, f"e{i}") for i in range(6)]
    rdt = [(k.tile([128, 512], F32, "rd"), k.tile([64, 512], F32, "rs"))]
    k.memset("pool", vaug.all, 1.0)
    onesv = ones_b[0:96, 0:96]
    scale = 96 ** -0.5
    bi = 0
    for h in range(8):
        for (c0, ncl, s) in ALLB:
            b = 4 + bi % 2
            bi += 1
            for kk in range(8):
                k.mm(k.ps(b, rows=96, c1=ncl), wrp[:, kk, :], hbf[:, kk, c0:c0 + ncl], start=(kk == 0), stop=False)
            k.mm(k.ps(b, rows=96, c1=ncl), kvbn[:, h, :], kva[:, c0:c0 + ncl], start=False, stop=True)
            qk_norm_rope(k.ps(b, rows=96, c1=ncl), 96, gn[0:96, 4:5], onesv, cosT, sinT, Rm.all,
                         kh[0:96, c0:c0 + ncl], c0, ncl, tl, bi, (6, 7))
        for (c0, ncl, s) in LATB:
            b = 4 + bi % 2
            bi += 1
            for m in range(2):
                k.mm(k.ps(b, rows=96, c1=ncl), qb[:, m, h * 96:(h + 1) * 96], qa[:, m, c0 - NCTX:c0 - NCTX + ncl],
                     start=(m == 0), stop=(m == 1))
            qk_norm_rope(k.ps(b, rows=96, c1=ncl), 96, gn[0:96, 3:4], onesv, cosT, sinT, Rm.all,
                         qh[0:96, c0:c0 + ncl], c0, ncl, tl, bi, (6, 7))
        for g0 in range(0, 18, 4):
            n_ = min(4, 18 - g0)
            b = 4 + (g0 // 4) % 2
            for q_ in range(n_):
                tc = g0 + q_
                k.mm(k.ps(b, c0=q_ * 64, c1=(q_ + 1) * 64), kva[:, tc * 128:(tc + 1) * 128], kvbv[:, h, :])
            src = V(k.banks[b][:, 0:n_ * 64].rearrange("p (a b) -> p a b", a=n_), [(PSUM_PAGE0 + b, PSUM_PAGE0 + b + 1)])
            k.copy("dve", vaug[:, g0:g0 + n_, 64:128], src)
        attend(qh, kh, vaug, 96, [(NCTX + 512 * i, 512) for i in range(4)], 18, scale, ycat,
               (h % 2) * 64, h // 2, etl, rdt)
    k.free(cosT, sinT, Rm, gn, wrp, qb, kvbn, kvbv, qa, kva, *tl, kh, qh, vaug, *etl, *rdt[0])
    project_out(1, ycat, dr["cd_w_out"], 512, LATB)
    if SUB == 1:
        return

    hp = k.tile([64, 3], F32, "hy_p")
    k.dma("sp", hp.all, dm(dr["hy_p"]))
    zT = k.tile([33, NLAT], BF16, "zT")
    k.dma(SW, zT.all, dm(dr["hy_zT"]))
    fw1 = k.tile([33, 64], BF16, "fw1")
    fw2 = k.tile([64, 64], BF16, "fw2")
    k.dma(SW, fw1.all, dm(dr["hy_filt_w1"]))
    k.dma(SW, fw2.all, dm(dr["hy_filt_w2"]))
    h1 = k.tile([64, NLAT], BF16, "h1T")
    h2 = k.tile([64, NLAT], BF16, "h2T")
    ft = [k.tile([64, 512], F32, f"ft{i}") for i in range(2)]
    MAGIC = 12582912.0
    TWO_PI = 2.0 * math.pi

    def sin_layer(dst, lhsT, src, bcol):
        for i in range(4):
            cs = slice(i * 512, (i + 1) * 512)
            b = 4 + i % 2
            k.mm(k.ps(b, rows=64), lhsT, src[:, cs])
            y, t_ = ft
            k.ts("dve", y.all, k.ps(b, rows=64), hp[:, bcol:bcol + 1], hp[:, 2:3], ALU.add, ALU.mult)
            k.ts("dve", t_.all, y.all, 1.0 / TWO_PI, MAGIC, ALU.mult, ALU.add)
            k.ts("dve", t_.all, t_.all, -MAGIC, -TWO_PI, ALU.add, ALU.mult)
            k.tt("dve", y.all, y.all, t_.all, ALU.add)
            k.act(dst[:, cs], y.all, AF.Sin)

    sin_layer(h1, fw1.all, zT, 0)
    sin_layer(h2, fw2.all, h1, 1)
    k.free(zT, fw1, fw2, h1, *ft, hp)

    CH = 256
    vt = k.tile([128, 16, 512], BF16, "hy_v")
    x1 = k.tile([128, 16, 512], BF16, "hy_x1")
    x2 = k.tile([128, 16, 512], BF16, "hy_x2")
    hpadL = k.tile([128, 8, 129], BF16, "hpadL")
    hpadR = k.tile([128, 8, 129], BF16, "hpadR")
    k.memset("dve", hpadL.all, 0.0)
    k.memset("dve", hpadR.all, 0.0)
    k.copy("pool", hpadL[:, :, 1:129], hbf[:, :, NCTX:NCTX + 128])
    k.copy("pool", hpadR[:, :, 0:127], hbf[:, :, T - 127:T])
    wf = k.tile([128, 8, CH], BF16, "hy_wf")
    wj = [k.tile([128, 8, CH], BF16, f"hy_wj{j}") for j in range(3)]
    cwb = k.tile([128, 3, CH], F32, "hy_cwb")
    bb = k.tile([128, CH], F32, "hy_bias")
    convw = dr["hy_conv_w"]
    for half in range(2):
        hs = slice(half * CH, (half + 1) * CH)
        for part, dst in enumerate((vt, x1, x2)):
            cbase = part * 512 + half * CH
            k.dma(SW, wf.all, dm(win[:, :, cbase:cbase + CH]))
            k.dma("sp", cwb.all, dm(convw[:, cbase:cbase + CH].partition_broadcast(128)))
            k.dma("sp", bb.all, dm(dr["hy_conv_b"][cbase:cbase + CH].partition_broadcast(128)))
            for j in range(3):
                for kk in range(8):
                    k.tt("dve" if kk % 2 else "pool", wj[j][:, kk, :], wf[:, kk, :], cwb[:, j, :], ALU.mult)
            for tcn in range(16):
                b = 4 + tcn % 2
                n_mm = 0
                for j in range(3):
                    for kk in range(8):
                        sh = j - 1
                        if tcn == 0 and sh == -1:
                            lhs = hpadL[:, kk, 0:128]
                        elif tcn == 15 and sh == 1:
                            lhs = hpadR[:, kk, 0:128]
                        else:
                            c0 = NCTX + tcn * 128 + sh
                            lhs = hbf[:, kk, c0:c0 + 128]
                        k.mm(k.ps(b, c1=CH), lhs, wj[j][:, kk, :], start=(n_mm == 0), stop=(n_mm == 23))
                        n_mm += 1
                k.tt("dve", dst[:, tcn, hs], k.ps(b, c1=CH), bb.all, ALU.add)
    k.free(hpadL, hpadR, wf, *wj, cwb, bb)
    carve = [hbf.off]

    def ctile(shape, dt, name):
        t_ = Tile(k, carve[0], shape, dt, name)
        carve[0] += ((t_.nbytes + PAGE - 1) // PAGE) * PAGE
        assert carve[0] <= hbf.off + hbf.alloc
        return t_

    hsum = ctile([128, 16, CH], BF16, "hsum")
    hdif = ctile([128, 16, CH], BF16, "hdif")
    Yre = ctile([128, 16, CH], BF16, "Yre")
    Yim = ctile([128, 16, CH], BF16, "Yim")
    tmps = [ctile([128, 512], F32, "hyt0"), ctile([128, 512], F32, "hyt1"),
            k.tile([128, 512], F32, "hyt2"), k.tile([128, 512], F32, "hyt3")]
    dftA = [k.tile([128, 16, 128], BF16, f"dftA{i}") for i in range(2)]
    dftB = [k.tile([128, 16, 128], BF16, f"dftB{i}") for i in range(2)]
    w3 = k.tile([64, 512], BF16, "fw3")
    skb = k.tile([128, CH], F32, "hy_skipb")
    winr = [k.tile([128, CH], F32, f"hywin{i}") for i in range(2)]
    for half in range(2):
        hs = slice(half * CH, (half + 1) * CH)
        for o in range(2):
            xo = x1 if o == 0 else x2
            for dr_ in range(2):
                cb_ = o * 1024 + dr_ * 512 + half * CH
                k.dma(SW, w3[:, dr_ * CH:(dr_ + 1) * CH], dm(dr["hy_filt_w3"][:, cb_:cb_ + CH]))
            k.dma("sp", skb.all, dm(dr["hy_skip"][o, half * CH:(half + 1) * CH].partition_broadcast(128)))
            for tcn in range(16):
                b = 4 + tcn % 2
                wn = winr[tcn % 2]
                k.dma("sp", wn.all, dm(dr["hy_window"][tcn * 128:(tcn + 1) * 128, half * CH:(half + 1) * CH]))
                k.mm(k.ps(b), h2[0:64, tcn * 128:(tcn + 1) * 128], w3.all)
                hf_, hb_ = tmps[0], tmps[1]
                k.tt("dve", hf_[:, 0:CH], k.ps(b, c0=0, c1=CH), wn.all, ALU.mult)
                k.tt("dve", hb_[:, 0:CH], k.ps(b, c0=CH, c1=2 * CH), wn.all, ALU.mult)
                k.tt("pool", hsum[:, tcn, :], hf_[:, 0:CH], hb_[:, 0:CH], ALU.add)
                k.tt("pool", hdif[:, tcn, :], hb_[:, 0:CH], hf_[:, 0:CH], ALU.subtract)
                if tcn == 0:
                    k.copy("pool", hsum[0:1, 0, :], hf_[0:1, 0:CH])
            for fc in range(16):
                ca, sa = dftA[fc % 2], dftB[fc % 2]
                k.dma("sp", V(ca.ap.rearrange("p a b -> p (a b)"), ca.all.pg), dm(dr["dft_cf"][fc]))
                k.dma("sp", V(sa.ap.rearrange("p a b -> p (a b)"), sa.all.pg), dm(dr["dft_sf"][fc]))
                bx, by = 4 + (fc % 2) * 2, 5 + (fc % 2) * 2
                for tcn in range(16):
                    k.mm(k.ps(bx, c0=0, c1=CH), ca[:, tcn, :], vt[:, tcn, hs], start=(tcn == 0), stop=(tcn == 15))
                for tcn in range(16):
                    k.mm(k.ps(bx, c0=CH, c1=2 * CH), sa[:, tcn, :], vt[:, tcn, hs], start=(tcn == 0), stop=(tcn == 15))
                for tcn in range(16):
                    k.mm(k.ps(by, c0=0, c1=CH), ca[:, tcn, :], hsum[:, tcn, :], start=(tcn == 0), stop=(tcn == 15))
                for tcn in range(16):
                    k.mm(k.ps(by, c0=CH, c1=2 * CH), sa[:, tcn, :], hdif[:, tcn, :], start=(tcn == 0), stop=(tcn == 15))
                kt, t1, t2 = tmps[2], tmps[0], tmps[1]
                k.copy("act", kt.all, k.ps(by))
                A_ = k.ps(bx, c0=0, c1=CH)
                B_ = k.ps(bx, c0=CH, c1=2 * CH)
                k.tt("dve", t1[:, 0:CH], A_, kt[:, 0:CH], ALU.mult)
                k.tt("dve", t1[:, CH:2 * CH], B_, kt[:, CH:2 * CH], ALU.mult)
                k.tt("pool", Yre[:, fc, :], t1[:, 0:CH], t1[:, CH:2 * CH], ALU.add)
                k.tt("dve", t2[:, 0:CH], A_, kt[:, CH:2 * CH], ALU.mult)
                k.tt("dve", t2[:, CH:2 * CH], B_, kt[:, 0:CH], ALU.mult)
                k.tt("pool", Yim[:, fc, :], t2[:, 0:CH], t2[:, CH:2 * CH], ALU.subtract)
            for tcn in range(16):
                ca, sa = dftA[tcn % 2], dftB[tcn % 2]
                k.dma("sp", V(ca.ap.rearrange("p a b -> p (a b)"), ca.all.pg), dm(dr["dft_ci"][tcn]))
                k.dma("sp", V(sa.ap.rearrange("p a b -> p (a b)"), sa.all.pg), dm(dr["dft_si"][tcn]))
                b = 4 + tcn % 2
                for fc in range(16):
                    k.mm(k.ps(b, c1=CH), ca[:, fc, :], Yre[:, fc, :], start=(fc == 0), stop=False)
                for fc in range(16):
                    k.mm(k.ps(b, c1=CH), sa[:, fc, :], Yim[:, fc, :], start=False, stop=(fc == 15))
                t1 = tmps[3]
                k.tt("pool", t1[:, 0:CH], vt[:, tcn, hs], skb.all, ALU.mult)
                k.tt("dve", t1[:, 0:CH], k.ps(b, c1=CH), t1[:, 0:CH], ALU.add)
                k.tt("dve", vt[:, tcn, hs], t1[:, 0:CH], xo[:, tcn, hs], ALU.mult)
    for tcn in range(16):
        b = 4 + tcn % 2
        for cc in range(4):
            k.transpose(ps_bf(k, b, 128, cc * 128, (cc + 1) * 128), vt[:, tcn, cc * 128:(cc + 1) * 128], ident_b.all)
        for cc in range(4):
            k.copy("dve", ycat[:, cc, NCTX + tcn * 128:NCTX + (tcn + 1) * 128],
                   ps_bf(k, b, 128, cc * 128, (cc + 1) * 128))
    k.free(vt, x1, x2, *dftA, *dftB, w3, skb, tmps[2], tmps[3], *winr, h2)
    project_out(1, ycat, dr["cd_w_out"], 0, LATB)


def moe_layer(k, dr, xs, hbf, ycat, ident_f, ones_b, M_, A_, norm_mod, ffn, LATB):
    dm = k.dram
    AX = mybir.AxisListType.X
    rw = k.tile([128, 8, 8], F32, "rw")
    r_hi = k.tile([128, 8, 8], BF16, "r_hi")
    r_lo = k.tile([128, 8, 8], BF16, "r_lo")
    k.dma("sp", rw.all, dm(dr["moe_router"].rearrange("(kc p) e -> p kc e", p=128)))
    k.copy("dve", r_hi.all, rw.all)
    k.tt("dve", r_lo.all, rw.all, r_hi.all, ALU.subtract)
    logits = k.tile([128, 16, 8], F32, "logits")
    gates = k.tile([128, 16, 8], F32, "gates")
    hlo = k.tile([128, 8, 512], BF16, "hlo")
    hf32 = [k.tile([128, 512], F32, f"hf32_{i}") for i in range(2)]

    def hook(bi, c0, ncl, kk, tmp, s):
        hf = hf32[kk % 2]
        k.act(hf[:, 0:ncl], tmp[:, 0:ncl], AF.Identity, bias=M_(1, 3, kk, s), scale=A_(1, 1, kk, s))
        k.tt("pool", hlo[:, kk, 0:ncl], hf[:, 0:ncl], hbf[:, kk, c0:c0 + ncl], ALU.subtract)
        if kk == 7:
            for q in range(ncl // 128):
                tcn = (c0 - NCTX) // 128 + q
                b = 6 + tcn % 2
                n = 0
                for which in range(3):
                    for k2 in range(8):
                        if which == 1:
                            lhs = hlo[:, k2, q * 128:(q + 1) * 128]
                        else:
                            lhs = hbf[:, k2, c0 + q * 128:c0 + (q + 1) * 128]
                        rr = r_lo if which == 2 else r_hi
                        k.mm(k.ps(b, c1=8), lhs, rr[:, k2, :], start=(n == 0), stop=(n == 23))
                        n += 1
                k.copy("dve", logits[:, tcn, :], k.ps(b, c1=8))

    norm_mod(1, 1, LATB, f32_hook=hook)
    k.free(hlo, *hf32, rw, r_hi, r_lo)
    mx = k.tile([128, 8], F32, "mx")
    sel = k.tile([128, 8], F32, "sel")
    ex = k.tile([128, 8], F32, "ex")
    sm = k.tile([128, 4], F32, "sm")
    for tcn in range(16):
        lg = logits[:, tcn, :]
        k.op("dve", (lambda o_, i_: (lambda e_: e_.max(out=o_, in_=i_)))(mx.ap, lg.ap), R=[lg], W=[mx.all])
        k.ts("dve", sel.all, lg, mx[:, 1:2], None, ALU.is_ge)
        k.ts("dve", sm[:, 0:1], mx[:, 0:1], -1.0, None, ALU.mult)
        k.act(ex.all, lg, AF.Exp, bias=sm[:, 0:1])
        k.tt("dve", ex.all, ex.all, sel.all, ALU.mult)
        k.op("dve", (lambda o_, i_: (lambda e_: e_.reduce_sum(out=o_, in_=i_, axis=AX)))(sm[:, 1:2].ap, ex.ap),
             R=[ex.all], W=[sm[:, 1:2]])
        k.recip(sm[:, 2:3], sm[:, 1:2])
        k.ts("dve", gates[:, tcn, :], ex.all, sm[:, 2:3], None, ALU.mult)
    k.free(mx, sel, ex, sm)
    Gbs = [k.tile([128, NLAT], F32, f"Gb{i}") for i in range(2)]
    Dt = k.tile([128, 128], F32, "Dt")
    Dhi = k.tile([128, 128], BF16, "Dhi")
    Dlo = k.tile([128, 128], BF16, "Dlo")
    for e in range(8):
        Gb = Gbs[e % 2]
        for tcn in range(16):
            b = 6 + tcn % 2
            k.ts("dve", Dt.all, ident_f.all, gates[:, tcn, e:e + 1], None, ALU.mult)
            k.copy("dve", Dhi.all, Dt.all)
            k.tt("dve", Dlo.all, Dt.all, Dhi.all, ALU.subtract)
            k.mm(k.ps(b, c1=128), ones_b.all, Dhi.all, start=True, stop=False)
            k.mm(k.ps(b, c1=128), ones_b.all, Dlo.all, start=False, stop=True)
            k.copy("act", Gb[:, tcn * 128:(tcn + 1) * 128], k.ps(b, c1=128))
        ffn(1, dr["moe_w1"][e], dr["moe_w3"][e], dr["moe_w2"][e], 3584, LATB, ycat, gate_bc=Gb)
    k.free(*Gbs, Dt, Dhi, Dlo, logits, gates)
```
